# Optimizing a Trainium2 kernel written in Bass

```python
import jax, jax.numpy as jnp
from jax import lax
import numpy as np

D_MODEL = 1024
BATCH = 32
SEQ = 2048
DEPTH = 1

CTX_LEN = 256
GRID_W = 64
EPS = 1e-6

POOL_WINDOWS = (2, 4, 8, 16)
POOL_WIDTH = D_MODEL // 2
POOL_GROUP = POOL_WIDTH // len(POOL_WINDOWS)

RET_HEADS = 4
RET_DK = 128
RET_DV = 256
RET_CHUNK = 128
ROPE_BASE = 10000.0
QK_WIDTH = RET_HEADS * RET_DK
V_WIDTH = RET_HEADS * RET_DV

OFF_POOL = 0
OFF_Q = OFF_POOL + POOL_WIDTH
OFF_K = OFF_Q + QK_WIDTH
OFF_V = OFF_K + QK_WIDTH
OFF_G = OFF_V + V_WIDTH
OFF_MERGE = OFF_G + V_WIDTH
IN_WIDTH = OFF_MERGE + 2 * D_MODEL

PEER_HEADS = 8
PEER_NKEYS = 128
PEER_EXPERTS = PEER_NKEYS * PEER_NKEYS
PEER_TOPK = 16
PEER_DQ = 256
PEER_BLOCK = 128

kernel_name = "hybrid_pool_retention_peer_dit"


def rmsnorm(x, g):
    xf = x.astype(jnp.float32)
    y = xf * lax.rsqrt(jnp.mean(xf * xf, axis=-1, keepdims=True) + EPS)
    return (y * g.astype(jnp.float32)).astype(x.dtype)


def modulate(h, shift, scale):
    return h * (1.0 + scale) + shift


def heads(a, d):
    b, l, _ = a.shape
    return a.reshape(b, l, -1, d).transpose(0, 2, 1, 3)


def axial_rope(a, row, col):
    quarter = a.shape[-1] // 4
    inv = ROPE_BASE ** (-jnp.arange(quarter, dtype=jnp.float32) / quarter)
    ang = jnp.concatenate([row[:, None] * inv, col[:, None] * inv], axis=-1)
    cos, sin = jnp.cos(ang), jnp.sin(ang)
    a1, a2 = jnp.split(a, 2, axis=-1)
    return jnp.concatenate([a1 * cos - a2 * sin, a1 * sin + a2 * cos], axis=-1).astype(a.dtype)


def multiscale_pool(p, pool_w, pool_scale):
    L = p.shape[1]
    pf = p.astype(jnp.float32)
    cs = jnp.pad(jnp.cumsum(pf, axis=1), ((0, 0), (1, 0), (0, 0)))
    t = jnp.arange(L)
    groups = []
    for gi, w in enumerate(POOL_WINDOWS):
        lo = jnp.clip(t - w // 2, 0, L)
        hi = jnp.clip(t + w - w // 2, 0, L)
        sl = slice(gi * POOL_GROUP, (gi + 1) * POOL_GROUP)
        win_sum = cs[:, hi, sl] - cs[:, lo, sl]
        cnt = (hi - lo).astype(jnp.float32)[None, :, None]
        groups.append(win_sum / cnt - pf[:, :, sl])
    d = jnp.stack(groups, axis=2).astype(p.dtype)
    mixed = jnp.einsum('blgc,gcd->blgd', d, pool_w).reshape(p.shape)
    return mixed * pool_scale


def retention_scan(q, k, v, log_gamma, s0, strict):
    b, h, L, _ = q.shape
    dv = v.shape[-1]
    C = RET_CHUNK
    n = L // C

    def chunks(a):
        return a.reshape(b, h, n, C, a.shape[-1]).transpose(2, 0, 1, 3, 4)

    idx = jnp.arange(C, dtype=jnp.float32)
    rel = idx[:, None] - idx[None, :]
    mask = (rel > 0) if strict else (rel >= 0)
    lg = log_gamma[:, None, None]
    intra = jnp.where(mask, jnp.exp(lg * jnp.where(mask, rel, 0.0)), 0.0)
    q_dec = jnp.exp(log_gamma[:, None] * (idx + 1.0))[..., None]
    k_dec = jnp.exp(log_gamma[:, None] * (C - 1.0 - idx))[..., None]
    blk_dec = jnp.exp(log_gamma * C)[:, None, None]

    def step(s, qkv):
        qi, ki, vi = qkv
        scores = jnp.einsum('bhnd,bhmd->bhnm', qi, ki) * intra
        y = (jnp.einsum('bhnm,bhme->bhne', scores, vi)
             + jnp.einsum('bhnd,bhde->bhne', qi * q_dec, s))
        s = s * blk_dec + jnp.einsum('bhmd,bhme->bhde', ki * k_dec, vi)
        return s, y

    _, ys = lax.scan(step, s0, (chunks(q), chunks(k), chunks(v)))
    return ys.transpose(1, 2, 0, 3, 4).reshape(b, h, L, dv)


def context_states(kc, vc, lg):
    Lc = kc.shape[2]
    m = jnp.arange(Lc, dtype=jnp.float32)
    w_f = jnp.exp(lg[0][:, None] * (Lc - 1.0 - m))
    w_b = jnp.exp(lg[1][:, None] * m)
    kf = kc.astype(jnp.float32)
    vf = vc.astype(jnp.float32)
    s_f = jnp.einsum('hm,bhmd,bhme->bhde', w_f, kf, vf)
    s_b = jnp.einsum('hm,bhmd,bhme->bhde', w_b, kf, vf)
    return s_f, s_b


def bidir_retention(q, k, v, lg, s_f, s_b):
    y_f = retention_scan(q, k, v, lg[0], s_f, False)
    y_b = retention_scan(jnp.flip(q, 2), jnp.flip(k, 2), jnp.flip(v, 2), lg[1], s_b, True)
    return y_f + jnp.flip(y_b, 2)


def retention_out(y, gate, norm_g, w_o):
    b, h, L, dv = y.shape
    yf = y.astype(jnp.float32)
    mu = jnp.mean(yf, axis=-1, keepdims=True)
    var = jnp.mean(jnp.square(yf - mu), axis=-1, keepdims=True)
    yn = ((yf - mu) * lax.rsqrt(var + EPS)).transpose(0, 2, 1, 3).reshape(b, L, h * dv)
    yn = (yn * norm_g.astype(jnp.float32)).astype(gate.dtype)
    return (yn * jax.nn.silu(gate)) @ w_o


def mix_tokens(proj, y_ret, lp):
    pool = multiscale_pool(proj[..., OFF_POOL:OFF_Q], lp['pool_w'], lp['pool_scale']) @ lp['pool_out']
    ret = retention_out(y_ret, proj[..., OFF_G:OFF_MERGE], lp['ret_norm_g'], lp['ret_out'])
    g_pool, g_ret = jnp.split(proj[..., OFF_MERGE:], 2, axis=-1)
    merged = jax.nn.sigmoid(g_pool) * pool + jax.nn.sigmoid(g_ret) * ret
    return merged @ lp['w_out']


def peer_ffn(h, wq, keys, u, v):
    b, L, d = h.shape
    blocks = h.reshape(-1, PEER_BLOCK, d)

    def one_block(hb):
        t = hb.shape[0]
        q = (hb @ wq).reshape(t, PEER_HEADS, 2, PEER_DQ // 2)
        s = jnp.einsum('thpd,hpkd->thpk', q, keys).astype(jnp.float32)
        s1, i1 = lax.top_k(s[:, :, 0], PEER_TOPK)
        s2, i2 = lax.top_k(s[:, :, 1], PEER_TOPK)
        cand = (s1[..., :, None] + s2[..., None, :]).reshape(t, PEER_HEADS, PEER_TOPK * PEER_TOPK)
        cidx = (i1[..., :, None] * PEER_NKEYS + i2[..., None, :]).reshape(t, PEER_HEADS, PEER_TOPK * PEER_TOPK)
        best, pos = lax.top_k(cand, PEER_TOPK)
        eidx = jnp.take_along_axis(cidx, pos, axis=-1)
        g = jax.nn.softmax(best, axis=-1)
        act = jax.nn.gelu(jnp.einsum('thkd,td->thk', u[eidx], hb).astype(jnp.float32), approximate=False)
        return jnp.einsum('thk,thkd->td', (g * act).astype(hb.dtype), v[eidx])

    return lax.map(one_block, blocks).reshape(b, L, d)


def trunk_layer(x, ctx, c, c_ctx, lp, update_ctx):
    b, L, d = x.shape
    mod = jax.nn.silu(c) @ lp['ada_w'] + lp['ada_b']
    mod_c = jax.nn.silu(c_ctx) @ lp['ada_w'] + lp['ada_b']
    sh1, sc1, g1, sh2, sc2, g2 = jnp.split(mod[:, None, :], 6, axis=-1)
    csh1, csc1, cg1, csh2, csc2, cg2 = jnp.split(mod_c, 6, axis=-1)
    lg = jax.nn.log_sigmoid(lp['ret_decay'].astype(jnp.float32))
    k_scale = RET_DK ** -0.5

    rows = L // GRID_W
    row = jnp.repeat(jnp.arange(rows, dtype=jnp.float32), GRID_W)
    col = jnp.tile(jnp.arange(GRID_W, dtype=jnp.float32), rows)

    hc = modulate(rmsnorm(ctx, lp['norm_mix_g']), csh1, csc1)
    if update_ctx:
        projc = hc @ lp['w_in']
        kv_c = projc[..., OFF_K:OFF_G]
    else:
        kv_c = hc @ lp['w_in'][:, OFF_K:OFF_G]
    kc = heads(kv_c[..., :QK_WIDTH], RET_DK) * k_scale
    vc = heads(kv_c[..., QK_WIDTH:], RET_DV)
    s_f, s_b = context_states(kc, vc, lg)

    h = modulate(rmsnorm(x, lp['norm_mix_g']), sh1, sc1)
    proj = h @ lp['w_in']
    q = axial_rope(heads(proj[..., OFF_Q:OFF_K], RET_DK), row, col)
    k = axial_rope(heads(proj[..., OFF_K:OFF_V], RET_DK), row, col) * k_scale
    v = heads(proj[..., OFF_V:OFF_G], RET_DV)
    y = bidir_retention(q, k, v, lg, s_f, s_b)
    x = x + g1 * mix_tokens(proj, y, lp)

    hf = modulate(rmsnorm(x, lp['norm_ffn_g']), sh2, sc2)
    x = x + g2 * peer_ffn(hf, lp['peer_wq'], lp['peer_keys'], lp['peer_u'], lp['peer_v'])

    if update_ctx:
        qc = heads(projc[..., OFF_Q:OFF_K], RET_DK)
        zero = jnp.zeros_like(s_f)
        yc = bidir_retention(qc, kc, vc, lg, zero, zero)
        ctx = ctx + cg1 * mix_tokens(projc, yc, lp)
        hcf = modulate(rmsnorm(ctx, lp['norm_ffn_g']), csh2, csc2)
        ctx = ctx + cg2 * peer_ffn(hcf, lp['peer_wq'], lp['peer_keys'], lp['peer_u'], lp['peer_v'])
    return x, ctx


def setup_inputs(seed: int = 0) -> dict:
    key = jax.random.key(seed)
    ks = jax.random.split(key, 24)
    f32 = jnp.float32
    D = D_MODEL

    def nrm(k, shape, s):
        return jax.random.normal(k, shape, f32) * s

    base_logit = jnp.log(2.0 ** (5.0 + jnp.arange(RET_HEADS, dtype=f32)) - 1.0)
    return {
        'x': nrm(ks[0], (BATCH, SEQ, D), 1.0),
        'c': nrm(ks[1], (BATCH, D), 1.0),
        'ctx': nrm(ks[2], (BATCH, CTX_LEN, D), 1.0),
        'c_ctx': nrm(ks[3], (D,), 1.0),
        'ada_w': nrm(ks[4], (DEPTH, D, 6 * D), 0.5 * D ** -0.5),
        'ada_b': nrm(ks[5], (DEPTH, 6 * D), 0.02),
        'norm_mix_g': 1.0 + nrm(ks[6], (DEPTH, D), 0.02),
        'norm_ffn_g': 1.0 + nrm(ks[7], (DEPTH, D), 0.02),
        'w_in': nrm(ks[8], (DEPTH, D, IN_WIDTH), D ** -0.5),
        'pool_w': nrm(ks[9], (DEPTH, len(POOL_WINDOWS), POOL_GROUP, POOL_GROUP), POOL_GROUP ** -0.5),
        'pool_scale': 1.0 + nrm(ks[10], (DEPTH, POOL_WIDTH), 0.1),
        'pool_out': nrm(ks[11], (DEPTH, POOL_WIDTH, D), POOL_WIDTH ** -0.5),
        'ret_decay': base_logit[None, None, :] + nrm(ks[12], (DEPTH, 2, RET_HEADS), 0.1),
        'ret_norm_g': 1.0 + nrm(ks[13], (DEPTH, V_WIDTH), 0.02),
        'ret_out': nrm(ks[14], (DEPTH, V_WIDTH, D), V_WIDTH ** -0.5),
        'w_out': nrm(ks[15], (DEPTH, D, D), D ** -0.5),
        'peer_wq': nrm(ks[16], (DEPTH, D, PEER_HEADS * PEER_DQ), D ** -0.5),
        'peer_keys': nrm(ks[17], (DEPTH, PEER_HEADS, 2, PEER_NKEYS, PEER_DQ // 2), (PEER_DQ // 2) ** -0.5),
        'peer_u': nrm(ks[18], (DEPTH, PEER_EXPERTS, D), D ** -0.5),
        'peer_v': nrm(ks[19], (DEPTH, PEER_EXPERTS, D), 0.5),
        'final_g': 1.0 + nrm(ks[20], (D,), 0.02),
    }


def reference(x, c, ctx, c_ctx, ada_w, ada_b, norm_mix_g, norm_ffn_g, w_in, pool_w,
              pool_scale, pool_out, ret_decay, ret_norm_g, ret_out, w_out, peer_wq,
              peer_keys, peer_u, peer_v, final_g):
    for layer in range(DEPTH):
        lp = {
            'ada_w': ada_w[layer], 'ada_b': ada_b[layer],
            'norm_mix_g': norm_mix_g[layer], 'norm_ffn_g': norm_ffn_g[layer],
            'w_in': w_in[layer], 'pool_w': pool_w[layer], 'pool_scale': pool_scale[layer],
            'pool_out': pool_out[layer], 'ret_decay': ret_decay[layer],
            'ret_norm_g': ret_norm_g[layer], 'ret_out': ret_out[layer], 'w_out': w_out[layer],
            'peer_wq': peer_wq[layer], 'peer_keys': peer_keys[layer],
            'peer_u': peer_u[layer], 'peer_v': peer_v[layer],
        }
        x, ctx = trunk_layer(x, ctx, c, c_ctx, lp, layer + 1 < DEPTH)
    return rmsnorm(x, final_g)
```

```python
import os, contextlib
import numpy as np
import concourse.bass as bass
import concourse.mybir as mybir
from concourse.bass_utils import run_bass_kernel_spmd

F32 = mybir.dt.float32; BF16 = mybir.dt.bfloat16; U32 = mybir.dt.uint32
ALU = mybir.AluOpType; AF = mybir.ActivationFunctionType; AX = mybir.AxisListType

NCORES = 8
D = 1024; L = 2048; BPC = 4; LC = 256
EPS = 1e-6
NEG = -1e30
TB = 256
K_SCALE = 128 ** -0.5


class Buf:
    __slots__ = ("t", "lw", "rd", "name")

    def __init__(self, t, name=""):
        self.t = t; self.lw = None; self.rd = {}; self.name = name

    def __getitem__(self, k):
        return self.t[k]


class EngW:
    SEM_LIMIT = 30000

    def __init__(self, fw, eng, name, is_pe=False):
        self.fw = fw; self.eng = eng; self.name = name; self.is_pe = is_pe
        self.sem = fw.new_sem(name + "_c0"); self.count = 0; self.nsem = 1
        self.waited = {}
        self.n_instr = 0; self.n_wait = 0

    def wait_tok(self, tok):
        sem, val = tok
        if self.waited.get(id(sem), 0) < val:
            self.eng.wait_ge(sem, val); self.waited[id(sem)] = val; self.n_wait += 1

    def bump(self, ins):
        if self.count >= self.SEM_LIMIT:
            self.sem = self.fw.new_sem(f"{self.name}_c{self.nsem}"); self.nsem += 1; self.count = 0
        self.count += 1; self.n_instr += 1
        ins.then_inc(self.sem, 1)
        return (self.sem, self.count)


class FW:
    def __init__(self, nc):
        self.nc = nc; self.root = contextlib.ExitStack(); self.es = self.root
        self.pe = EngW(self, nc.tensor, "pe", True)
        self.act = EngW(self, nc.scalar, "act")
        self.dve = EngW(self, nc.vector, "dve")
        self.pool = EngW(self, nc.gpsimd, "pool")
        self.sp = EngW(self, nc.sync, "sp")
        self.engs = [self.pe, self.act, self.dve, self.pool, self.sp]
        self.dma_pools = {"sp": [[self.new_sem(f"dmas{i}"), 0] for i in range(32)],
                          "pool": [[self.new_sem(f"dmap{i}"), 0] for i in range(16)]}
        self.dma_rr = {"sp": 0, "pool": 0}
        self.uid = 0

    def new_sem(self, name):
        return self.root.enter_context(self.nc.semaphore(name))

    def sbuf(self, name, shape, dt):
        self.uid += 1
        return Buf(self.es.enter_context(self.nc.sbuf_tensor(f"{name}_{self.uid}", list(shape), dt)), name)

    def psum(self, name, shape, dt=F32):
        return Buf(self.root.enter_context(self.nc.psum_tensor(name, list(shape), dt)), name)

    @contextlib.contextmanager
    def scope(self):
        old = self.es
        self.es = contextlib.ExitStack()
        try:
            yield
        finally:
            self.barrier()
            self.es.close()
            self.es = old

    def barrier(self):
        toks = [(e.sem, e.count) for e in self.engs if e.count > 0]
        toks += [(s, c) for s, c in self.dma_pools["sp"] if c > 0]
        for e in self.engs:
            for tok in toks:
                if tok[0] is e.sem:
                    continue
                e.wait_tok(tok)

    def _deps(self, ew, reads, writes):
        for b in reads:
            if b.lw is not None:
                self._w(ew, b.lw)
        for b in writes:
            if b.lw is not None:
                self._w(ew, b.lw)
            for tok in b.rd.values():
                self._w(ew, tok)

    def _w(self, ew, tok):
        if ew.is_pe and tok[0] is ew.sem:
            return
        ew.wait_tok(tok)

    def _upd(self, tok, reads, writes):
        k = id(tok[0])
        for b in reads:
            b.rd[k] = tok
        for b in writes:
            b.lw = tok; b.rd = {}

    def op(self, ew, fn, reads=(), writes=()):
        self._deps(ew, reads, writes)
        ins = fn(ew.eng)
        tok = ew.bump(ins)
        self._upd(tok, reads, writes)
        return tok

    def dma(self, ew, out, in_, reads=(), writes=()):
        pl = self.dma_pools[ew.name]
        ent = pl[self.dma_rr[ew.name]]; self.dma_rr[ew.name] = (self.dma_rr[ew.name] + 1) % len(pl)
        sem = ent[0]
        self._deps(ew, reads, writes)
        if ent[1] > 0:
            ew.wait_tok((sem, ent[1]))
        ew.eng.dma_start(out=out, in_=in_).then_inc(sem, 16)
        ent[1] += 16
        tok = (sem, ent[1])
        self._upd(tok, reads, writes)
        return tok


def APX(a, dims):
    return bass.AP(a.tensor, a.offset, [list(a.ap[0])] + [list(d) for d in dims])


def build_program(debug=False, nb=BPC):
    nc = bass.Bass("TRN2", target_bir_lowering=False)
    f = FW(nc)
    pe, act, dve, pool, sp = f.pe, f.act, f.dve, f.pool, f.sp

    def din(name, shape, dt=F32):
        return nc.dram_tensor(name, list(shape), dt, kind="ExternalInput")

    xT_d = din("xT", [BPC, 8, 128, L])
    ctxT_d = din("ctxT", [BPC, 8, 128, LC])
    cT_d = din("cT", [128, 8 * 5])
    adab_d = din("ada_b", [128, 48])
    gmix_d = din("g_mix", [128, 8]); gffn_d = din("g_ffn", [128, 8]); gfin_d = din("g_fin", [128, 8])
    pscale_d = din("pool_scale", [128, 4]); rng_d = din("ret_norm_g", [128, 8])
    decay_d = din("ret_decay", [1, 8])
    cos_d = din("ropecos", [128, L]); sin_d = din("ropesin", [128, L])
    icnt_d = din("invcnt", [4, 128, L])
    ada_d = din("ada_w_r", [48 * 128, 1024])
    win_d = din("w_in_r", [52 * 128, 1024])
    poolw_d = din("pool_w", [128, 512])
    poolout_d = din("pool_out_r", [8 * 128, 512])
    retout_d = din("ret_out_r", [8 * 128, 1024])
    wout_d = din("w_out_r", [8 * 128, 1024])
    wq_d = din("wq_r", [16 * 128, 1024])
    keys_d = din("keysT", [128, 2048])
    u_d = din("uT_r", [128 * 128, 1024])
    v_d = din("v", [128 * 128, 1024])
    out_d = nc.dram_tensor("outT", [BPC, 8, 128, L], F32, kind="ExternalOutput")

    dbg_outs = {}

    def dbg(name, buf, ap, shape):
        if not debug:
            return
        t = nc.dram_tensor("dbg_" + name, list(shape), F32, kind="ExternalOutput")
        dbg_outs[name] = f.dma(sp, t.ap(), ap, reads=[buf])

    class Scr:
        def __init__(self, name, src, rpp, order=None, defer=False):
            R, C = src.shape
            self.R = R; self.src = src
            self.t = nc.dram_tensor("scr_" + name, [R, C], BF16, kind="Internal")
            self.rpp = rpp
            n = (R + rpp - 1) // rpp
            self.pieces = [Buf(self.t, name) for _ in range(n)]
            self.order = list(order) if order is not None else list(range(n))
            if not defer:
                self.issue()

        def issue_piece(self, i):
            r0 = i * self.rpp
            r1 = min(self.R, r0 + self.rpp)
            f.dma(pool, self.t.ap()[r0:r1, :], self.src.ap()[r0:r1, :], writes=[self.pieces[i]])

        def issue(self):
            for i in self.order:
                r0 = i * self.rpp
                r1 = min(self.R, r0 + self.rpp)
                f.dma(pool, self.t.ap()[r0:r1, :], self.src.ap()[r0:r1, :], writes=[self.pieces[i]])

        def rows(self, r0, r1):
            return self.t.ap()[r0:r1, :], [self.pieces[i] for i in range(r0 // self.rpp, (r1 - 1) // self.rpp + 1)]

    s_poolw = Scr("poolw", poolw_d, 128)
    _ord = list(range(8, 20))
    for _h in range(4):
        _ord += [4 + _h, 44 + _h, 48 + _h, 20 + 2 * _h, 21 + 2 * _h]
    _ord += [0, 1, 2, 3] + list(range(28, 44))
    assert sorted(_ord) == list(range(52))
    s_win = Scr("win", win_d, 128, order=_ord)
    s_poolout = Scr("poolout", poolout_d, 1024)
    s_retout = Scr("retout", retout_d, 1024)
    s_wout = Scr("wout", wout_d, 1024)
    s_wq = Scr("wq", wq_d, 2048)
    s_keys = Scr("keys", keys_d, 128)
    s_u = Scr("u", u_d, 2048, defer=True)
    s_v = Scr("v", v_d, 2048, defer=True)
    pending_casts = []
    for _i in range(8):
        pending_casts += [(s_u, _i), (s_v, _i)]

    def cast_some(n=1):
        for _ in range(n):
            if pending_casts:
                scr_, i_ = pending_casts.pop(0)
                scr_.issue_piece(i_)

    PS = [f.psum(f"ps{i}", [128, 512], F32) for i in range(8)]

    def psbf(bank, ncols):
        return PS[bank].t[:, 0:ncols // 2].bitcast(BF16)

    iota_f = f.sbuf("iota_f", [128, 128], F32)
    f.op(pool, lambda e: e.iota(iota_f[:], pattern=[[1, 128]], base=0, channel_multiplier=0,
                                 allow_small_or_imprecise_dtypes=True), writes=[iota_f])
    pidx = f.sbuf("pidx", [128, 1], F32)
    f.op(pool, lambda e: e.iota(pidx[:], pattern=[[0, 1]], base=0, channel_multiplier=1,
                                 allow_small_or_imprecise_dtypes=True), writes=[pidx])
    rel = f.sbuf("rel", [128, 128], F32)
    f.op(pool, lambda e: e.iota(rel[:], pattern=[[1, 128]], base=0, channel_multiplier=-1,
                                 allow_small_or_imprecise_dtypes=True), writes=[rel])
    ident_bf = f.sbuf("ident_bf", [128, 128], BF16)
    ident_f = f.sbuf("ident_f", [128, 128], F32)
    ones_f = f.sbuf("ones_f", [128, 128], F32)
    f.op(dve, lambda e: e.tensor_scalar(out=ident_bf[:], in0=iota_f[:], scalar1=pidx[:, 0:1], scalar2=None,
                                        op0=ALU.is_equal), reads=[iota_f, pidx], writes=[ident_bf])
    f.op(dve, lambda e: e.tensor_scalar(out=ident_f[:], in0=iota_f[:], scalar1=pidx[:, 0:1], scalar2=None,
                                        op0=ALU.is_equal), reads=[iota_f, pidx], writes=[ident_f])
    f.op(dve, lambda e: e.memset(ones_f[:], 1.0), writes=[ones_f])
    epsb = f.sbuf("epsb", [128, 1], F32)
    f.op(dve, lambda e: e.memset(epsb[:], EPS), writes=[epsb])
    iota_bf = f.sbuf("iota_bf", [128, 128], BF16)
    f.op(dve, lambda e: e.tensor_copy(out=iota_bf[:], in_=iota_f[:]), reads=[iota_f], writes=[iota_bf])

    def load_const(name, src, shape, dt=F32):
        b = f.sbuf(name, shape, dt)
        f.dma(sp, b[:], src, writes=[b])
        return b

    adab = load_const("adab", adab_d.ap(), [128, 48])
    gmix = load_const("gmix", gmix_d.ap(), [128, 8])
    gffn = load_const("gffn", gffn_d.ap(), [128, 8])
    gfin = load_const("gfin", gfin_d.ap(), [128, 8])
    pscale = load_const("pscale", pscale_d.ap(), [128, 4])
    rng = load_const("rng", rng_d.ap(), [128, 8])
    dec_raw = load_const("dec_raw", bass.AP(decay_d, 0, [[0, 128], [1, 8]]), [128, 8])
    poolw = f.sbuf("poolw", [128, 512], BF16)
    _ap, _rd = s_poolw.rows(0, 128)
    f.dma(sp, poolw[:], _ap, reads=_rd, writes=[poolw])

    lg = f.sbuf("lg", [128, 8], F32)
    tmp8 = f.sbuf("tmp8", [128, 8], F32)
    f.op(act, lambda e: e.activation(out=tmp8[:], in_=dec_raw[:], func=AF.Exp, scale=-1.0), reads=[dec_raw], writes=[tmp8])
    f.op(act, lambda e: e.activation(out=lg[:], in_=tmp8[:], func=AF.Ln, bias=1.0), reads=[tmp8], writes=[lg])
    f.op(dve, lambda e: e.tensor_scalar(out=lg[:], in0=lg[:], scalar1=-1.0, scalar2=None, op0=ALU.mult), reads=[lg], writes=[lg])
    blk = f.sbuf("blk", [128, 8], F32)
    f.op(act, lambda e: e.activation(out=blk[:], in_=lg[:], func=AF.Exp, scale=128.0), reads=[lg], writes=[blk])
    pp = f.sbuf("pp", [128, 6], F32)
    f.op(dve, lambda e: e.tensor_scalar(out=pp[:, 0:1], in0=pidx[:], scalar1=-1.0, scalar2=127.0, op0=ALU.mult, op1=ALU.add), reads=[pidx], writes=[pp])
    f.op(dve, lambda e: e.tensor_copy(out=pp[:, 1:2], in_=pidx[:]), reads=[pidx], writes=[pp])
    f.op(dve, lambda e: e.tensor_scalar(out=pp[:, 2:3], in0=pidx[:], scalar1=-1.0, scalar2=255.0, op0=ALU.mult, op1=ALU.add), reads=[pidx], writes=[pp])
    f.op(dve, lambda e: e.tensor_scalar(out=pp[:, 3:4], in0=pidx[:], scalar1=-1.0, scalar2=127.0, op0=ALU.mult, op1=ALU.add), reads=[pidx], writes=[pp])
    f.op(dve, lambda e: e.tensor_copy(out=pp[:, 4:5], in_=pidx[:]), reads=[pidx], writes=[pp])
    f.op(dve, lambda e: e.tensor_scalar(out=pp[:, 5:6], in0=pidx[:], scalar1=1.0, scalar2=128.0, op0=ALU.mult, op1=ALU.add), reads=[pidx], writes=[pp])
    kdec = f.sbuf("kdec", [128, 8], F32)
    cdec = f.sbuf("cdec", [128, 16], F32)
    for h in range(4):
        f.op(act, lambda e, h=h: e.activation(out=kdec[:, h:h + 1], in_=pp[:, 0:1], func=AF.Exp, scale=lg[:, h:h + 1]), reads=[pp, lg], writes=[kdec])
        f.op(act, lambda e, h=h: e.activation(out=kdec[:, 4 + h:5 + h], in_=pp[:, 1:2], func=AF.Exp, scale=lg[:, 4 + h:5 + h]), reads=[pp, lg], writes=[kdec])
        for t in range(2):
            f.op(act, lambda e, h=h, t=t: e.activation(out=cdec[:, t * 4 + h:t * 4 + h + 1], in_=pp[:, 2 + t:3 + t], func=AF.Exp, scale=lg[:, h:h + 1]), reads=[pp, lg], writes=[cdec])
            f.op(act, lambda e, h=h, t=t: e.activation(out=cdec[:, 8 + t * 4 + h:8 + t * 4 + h + 1], in_=pp[:, 4 + t:5 + t], func=AF.Exp, scale=lg[:, 4 + h:5 + h]), reads=[pp, lg], writes=[cdec])
    f.op(dve, lambda e: e.tensor_scalar(out=kdec[:], in0=kdec[:], scalar1=K_SCALE, scalar2=None, op0=ALU.mult), reads=[kdec], writes=[kdec])
    f.op(dve, lambda e: e.tensor_scalar(out=cdec[:], in0=cdec[:], scalar1=K_SCALE, scalar2=None, op0=ALU.mult), reads=[cdec], writes=[cdec])
    def make_dec_tables():
        qdec = f.sbuf("qdec", [128, 8, 128], F32)
        dmask = f.sbuf("dmask", [128, 4, 128], F32)
        with f.scope():
            np1 = f.sbuf("np1", [128, 128], F32); nm = f.sbuf("nm", [128, 128], F32)
            f.op(dve, lambda e: e.tensor_scalar(out=np1[:], in0=iota_f[:], scalar1=1.0, scalar2=None, op0=ALU.add), reads=[iota_f], writes=[np1])
            f.op(dve, lambda e: e.tensor_scalar(out=nm[:], in0=iota_f[:], scalar1=-1.0, scalar2=128.0, op0=ALU.mult, op1=ALU.add), reads=[iota_f], writes=[nm])
            for h in range(4):
                f.op(act, lambda e, h=h: e.activation(out=qdec[:, h, :], in_=np1[:], func=AF.Exp, scale=lg[:, h:h + 1]), reads=[np1, lg], writes=[qdec])
                f.op(act, lambda e, h=h: e.activation(out=qdec[:, 4 + h, :], in_=nm[:], func=AF.Exp, scale=lg[:, 4 + h:5 + h]), reads=[nm, lg], writes=[qdec])
            relf = f.sbuf("relf", [128, 128], F32); relb = f.sbuf("relb", [128, 128], F32); mge = f.sbuf("mge", [128, 128], F32)
            ef = f.sbuf("ef", [128, 128], F32); eb = f.sbuf("eb", [128, 128], F32)
            f.op(dve, lambda e: e.tensor_scalar(out=relf[:], in0=rel[:], scalar1=0.0, scalar2=None, op0=ALU.max), reads=[rel], writes=[relf])
            f.op(dve, lambda e: e.tensor_scalar(out=relb[:], in0=rel[:], scalar1=-1.0, scalar2=0.0, op0=ALU.mult, op1=ALU.max), reads=[rel], writes=[relb])
            f.op(dve, lambda e: e.tensor_scalar(out=mge[:], in0=rel[:], scalar1=0.0, scalar2=K_SCALE, op0=ALU.is_ge, op1=ALU.mult), reads=[rel], writes=[mge])
            for h in range(4):
                f.op(act, lambda e, h=h: e.activation(out=ef[:], in_=relf[:], func=AF.Exp, scale=lg[:, h:h + 1]), reads=[relf, lg], writes=[ef])
                f.op(act, lambda e, h=h: e.activation(out=eb[:], in_=relb[:], func=AF.Exp, scale=lg[:, 4 + h:5 + h]), reads=[relb, lg], writes=[eb])
                f.op(dve, lambda e: e.tensor_tensor(out=ef[:], in0=ef[:], in1=eb[:], op=ALU.subtract), reads=[ef, eb], writes=[ef])
                f.op(dve, lambda e: e.tensor_tensor(out=ef[:], in0=ef[:], in1=mge[:], op=ALU.mult), reads=[ef, mge], writes=[ef])
                f.op(dve, lambda e, h=h: e.scalar_tensor_tensor(out=dmask[:, h, :], in0=eb[:], scalar=K_SCALE, in1=ef[:], op0=ALU.mult, op1=ALU.add), reads=[ef, eb], writes=[dmask])

        return qdec, dmask

    modv = f.sbuf("modv", [128, 48, 5], F32)
    with f.scope():
        ada_sb = f.sbuf("ada_sb", [128, 48, 1024], BF16)
        ada_g = [[Buf(ada_sb.t[:, g * 8:(g + 1) * 8, :], f"ada_g{g}_{k}") for k in range(3)] for g in range(6)]
        stg = [f.sbuf(f"stg{i}", [128, 8, 1024], F32) for i in range(2)]
        for g in range(6):
            st_ = stg[g % 2]
            f.dma(sp, st_[:], ada_d.ap()[g * 1024:(g + 1) * 1024, :].rearrange("(j p) c -> p j c", p=128), writes=[st_])
            for k_, (eng_, j0, j1) in enumerate(((act, 0, 3), (dve, 3, 6), (pool, 6, 8))):
                if eng_ is act:
                    f.op(eng_, lambda e, j0=j0, j1=j1, st_=st_, g=g, k_=k_: e.copy(out=ada_g[g][k_][:, j0:j1, :], in_=st_[:, j0:j1, :]), reads=[st_], writes=[ada_g[g][k_]])
                else:
                    f.op(eng_, lambda e, j0=j0, j1=j1, st_=st_, g=g, k_=k_: e.tensor_copy(out=ada_g[g][k_][:, j0:j1, :], in_=st_[:, j0:j1, :]), reads=[st_], writes=[ada_g[g][k_]])
        c_raw = f.sbuf("c_raw", [128, 40], F32)
        f.dma(sp, c_raw[:], cT_d.ap(), writes=[c_raw])
        c_bf = f.sbuf("c_bf", [128, 40], BF16)
        f.op(act, lambda e: e.activation(out=c_bf[:], in_=c_raw[:], func=AF.Silu), reads=[c_raw], writes=[c_bf])
        for oc in range(48):
            bank = PS[oc % 2]
            for kc in range(8):
                f.op(pe, lambda e, oc=oc, kc=kc, bank=bank: e.matmul(bank[:, 0:5], lhsT=ada_sb[:, oc, kc * 128:(kc + 1) * 128],
                                                                      rhs=c_bf[:, kc * 5:(kc + 1) * 5], start=(kc == 0), stop=(kc == 7)),
                     reads=ada_g[oc // 8] + [c_bf], writes=[bank])
            f.op(dve, lambda e, oc=oc, bank=bank: e.tensor_scalar(out=modv[:, oc, :], in0=bank[:, 0:5], scalar1=adab[:, oc:oc + 1], scalar2=None, op0=ALU.add),
                 reads=[adab], writes=[modv, bank])
    dbg("modv", modv, modv[:].rearrange("p a b -> p (a b)"), [128, 240])
    scl1 = f.sbuf("scl1", [128, 8, 5], F32); scl2 = f.sbuf("scl2", [128, 8, 5], F32)
    for (dst, gg, off) in ((scl1, gmix, 8), (scl2, gffn, 32)):
        f.op(dve, lambda e, dst=dst, off=off: e.tensor_scalar(out=dst[:], in0=modv[:, off:off + 8, :], scalar1=1.0, scalar2=None, op0=ALU.add), reads=[modv], writes=[dst])
        f.op(dve, lambda e, dst=dst, gg=gg: e.tensor_tensor(out=dst[:], in0=dst[:], in1=APX(gg[:], [[1, 8], [0, 5]]), op=ALU.mult), reads=[dst, gg], writes=[dst])

    NW = 4
    wbufs = [f.sbuf(f"wbuf{i}", [128, 1024], BF16) for i in range(NW)]
    wstate = {"i": 0, "n": NW}

    def load_w(scr, chunk, ncols=1024):
        b = wbufs[wstate["i"]]; wstate["i"] = (wstate["i"] + 1) % wstate["n"]
        _ap, _rd = scr.rows(chunk * 128, (chunk + 1) * 128)
        f.dma(sp, b[:, 0:ncols], _ap, reads=_rd, writes=[b])
        return b

    def rstd_from(ps_ap, out_ap, psbuf, outbuf):
        f.op(dve, lambda e: e.tensor_scalar(out=out_ap, in0=ps_ap, scalar1=1.0 / D, scalar2=EPS, op0=ALU.mult, op1=ALU.add), writes=[outbuf, psbuf])
        f.op(act, lambda e: e.activation(out=out_ap, in_=out_ap, func=AF.Sqrt), reads=[outbuf], writes=[outbuf])
        f.op(dve, lambda e: e.reciprocal(out=out_ap, in_=out_ap), reads=[outbuf], writes=[outbuf])

    for b in range(nb):
      with f.scope():
       U1 = f.sbuf("U1", [128, 16384], F32)
       U2 = f.sbuf("U2", [128, 16384], F32)
       u1b = U1.t[:, :].bitcast(BF16); u2b = U2.t[:, :].bitcast(BF16)
       hT = Buf(u1b[:, 0:16384].rearrange("p (k t) -> p k t", k=8), "hT")
       zT = Buf(u1b[:, 16384:32768].rearrange("p (k t) -> p k t", k=8), "zT")
       x1T = Buf(U1.t[:, :].rearrange("p (k t) -> p k t", k=8), "x1T")
       mergedT = Buf(u2b[:, 0:16384].rearrange("p (k t) -> p k t", k=8), "mergedT")
       mixedT = Buf(u2b[:, 16384:24576].rearrange("p (k t) -> p k t", k=4), "mixedT")
       sg = Buf(u2b[:, 24576:28672].rearrange("p (j t) -> p j t", j=2), "sg")
       r1 = [Buf(U2.t[:, 14336 + i * 512:14336 + (i + 1) * 512], f"r1_{i}") for i in range(2)]
       r2 = [Buf(U2.t[:, 15360 + i * 512:15360 + (i + 1) * 512], f"r2_{i}") for i in range(2)]
       WT = Buf(u2b.rearrange("p (t i) -> p t i", i=128), "WT")
       WTh = [Buf(u2b.rearrange("p (t i) -> p t i", i=128), "WTlo"), Buf(u2b.rearrange("p (t i) -> p t i", i=128), "WThi")]
       if True:
        _scS = f.scope(); _scS.__enter__()
        Sf0 = f.sbuf("Sf0", [128, 4, 256], F32); Sb0 = f.sbuf("Sb0", [128, 4, 256], F32)
        with f.scope():
            cx = f.sbuf("cx", [128, 8, LC], F32)
            f.dma(sp, cx[:], ctxT_d.ap()[b].rearrange("k p t -> p k t"), writes=[cx])
            csq = f.sbuf("csq", [128, 8, LC], F32)
            f.op(act, lambda e: e.activation(out=csq[:], in_=cx[:], func=AF.Square), reads=[cx], writes=[csq])
            for kc in range(8):
                f.op(pe, lambda e, kc=kc: e.matmul(PS[0][:, 0:LC], lhsT=ones_f[:], rhs=csq[:, kc, :], start=(kc == 0), stop=(kc == 7)), reads=[ones_f, csq], writes=[PS[0]])
            crstd = f.sbuf("crstd", [128, LC], F32)
            rstd_from(PS[0][:, 0:LC], crstd[:], PS[0], crstd)
            hcT = f.sbuf("hcT", [128, 8, LC], BF16)
            ctmp = f.sbuf("ctmp", [128, LC], F32)
            for kc in range(8):
                f.op(dve, lambda e, kc=kc: e.scalar_tensor_tensor(out=ctmp[:], in0=cx[:, kc, :], scalar=scl1[:, kc, 4:5], in1=crstd[:], op0=ALU.mult, op1=ALU.mult), reads=[cx, scl1, crstd], writes=[ctmp])
                f.op(dve, lambda e, kc=kc: e.tensor_scalar(out=hcT[:, kc, :], in0=ctmp[:], scalar1=modv[:, kc, 4:5], scalar2=None, op0=ALU.add), reads=[ctmp, modv], writes=[hcT])
            kcf = f.sbuf("kcf", [128, 2, 512], BF16); kcb = f.sbuf("kcb", [128, 2, 512], BF16); vc = f.sbuf("vc", [128, 2, 1024], BF16)
            for cc in range(12):
                wb = load_w(s_win, 8 + cc)
                for t in range(2):
                    if cc < 4:
                        bank = PS[1 + t]; col = cc * 128
                    else:
                        bank = PS[3 + 2 * t + (cc - 4) // 4]; col = ((cc - 4) % 4) * 128
                    for kc in range(8):
                        f.op(pe, lambda e, kc=kc, t=t, bank=bank, col=col, wb=wb: e.matmul(bank[:, col:col + 128], lhsT=hcT[:, kc, t * 128:(t + 1) * 128], rhs=wb[:, kc * 128:(kc + 1) * 128], start=(kc == 0), stop=(kc == 7)),
                             reads=[hcT, wb], writes=[bank])
            for t in range(2):
                for h in range(4):
                    f.op(act, lambda e, t=t, h=h: e.activation(out=kcf[:, t, h * 128:(h + 1) * 128], in_=PS[1 + t][:, h * 128:(h + 1) * 128], func=AF.Copy, scale=cdec[:, t * 4 + h:t * 4 + h + 1]), reads=[cdec], writes=[kcf, PS[1 + t]])
                    f.op(act, lambda e, t=t, h=h: e.activation(out=kcb[:, t, h * 128:(h + 1) * 128], in_=PS[1 + t][:, h * 128:(h + 1) * 128], func=AF.Copy, scale=cdec[:, 8 + t * 4 + h:8 + t * 4 + h + 1]), reads=[cdec], writes=[kcb, PS[1 + t]])
                for j in range(2):
                    f.op(dve, lambda e, t=t, j=j: e.tensor_copy(out=vc[:, t, j * 512:(j + 1) * 512], in_=PS[3 + 2 * t + j][:, :]), writes=[vc, PS[3 + 2 * t + j]])
            for h in range(4):
                for (kk, S0, bank) in ((kcf, Sf0, PS[0]), (kcb, Sb0, PS[7])):
                    for t in range(2):
                        f.op(pe, lambda e, kk=kk, t=t, h=h, bank=bank: e.matmul(bank[:, 0:256], lhsT=kk[:, t, h * 128:(h + 1) * 128], rhs=vc[:, t, h * 256:(h + 1) * 256], start=(t == 0), stop=(t == 1)), reads=[kk, vc], writes=[bank])
                    f.op(act, lambda e, S0=S0, h=h, bank=bank: e.copy(out=S0[:, h, :], in_=bank[:, 0:256]), writes=[S0, bank])
        if b == 0:
            dbg("Sf0", Sf0, Sf0[:].rearrange("p a b -> p (a b)"), [128, 1024])
            dbg("Sb0", Sb0, Sb0[:].rearrange("p a b -> p (a b)"), [128, 1024])

        with f.scope():
            with f.scope():
                rstd = f.sbuf("rstd", [128, L], F32)
                xc = [f.sbuf(f"xc{i}", [128, L], F32) for i in range(2)]
                xs = [f.sbuf(f"xs{i}", [128, L], F32) for i in range(2)]
                for kc in range(8):
                    xb_, sq_ = xc[kc % 2], xs[kc % 2]
                    f.dma(sp, xb_[:], xT_d.ap()[b, kc], writes=[xb_])
                    f.op(act, lambda e, xb_=xb_, sq_=sq_: e.activation(out=sq_[:], in_=xb_[:], func=AF.Square), reads=[xb_], writes=[sq_])
                    for tb in range(4):
                        f.op(pe, lambda e, tb=tb, sq_=sq_, kc=kc: e.matmul(PS[tb][:, :], lhsT=ones_f[:], rhs=sq_[:, tb * 512:(tb + 1) * 512], start=(kc == 0), stop=(kc == 7)), reads=[ones_f, sq_], writes=[PS[tb]])
                for tb in range(4):
                    rstd_from(PS[tb][:, :], rstd[:, tb * 512:(tb + 1) * 512], PS[tb], rstd)
                for kc in range(8):
                    xb_, sq_ = xc[kc % 2], xs[kc % 2]
                    f.dma(sp, xb_[:], xT_d.ap()[b, kc], writes=[xb_])
                    f.op(dve, lambda e, xb_=xb_, sq_=sq_, kc=kc: e.scalar_tensor_tensor(out=sq_[:], in0=xb_[:], scalar=scl1[:, kc, b:b + 1], in1=rstd[:], op0=ALU.mult, op1=ALU.mult), reads=[xb_, scl1, rstd], writes=[sq_])
                    f.op(act, lambda e, sq_=sq_, kc=kc: e.activation(out=hT[:, kc, :], in_=sq_[:], func=AF.Identity, bias=modv[:, kc, b:b + 1]), reads=[sq_, modv], writes=[hT])

            with f.scope():
                qdec, dmask = make_dec_tables()
                cst = [f.sbuf(f"cst{i}", [128, 512], F32) for i in range(1)]
                snt = [f.sbuf(f"snt{i}", [128, 512], F32) for i in range(1)]
                qT = f.sbuf("qT", [128, L], BF16); kT = f.sbuf("kT", [128, L], BF16)
                qfc = [f.sbuf(f"qfc{i}", [128, 128], BF16) for i in range(4)]
                qbc = [f.sbuf(f"qbc{i}", [128, 128], BF16) for i in range(4)]
                ktm = [f.sbuf(f"ktm{i}", [128, 128], BF16) for i in range(2)]
                V = f.sbuf("V", [128, 16, 256], BF16)
                Sf_bf = f.sbuf("Sf_bf", [128, 16, 256], BF16); Sb_bf = f.sbuf("Sb_bf", [128, 16, 256], BF16)
                Sf = f.sbuf("Sf", [128, 256], F32); Sb = f.sbuf("Sb", [128, 256], F32)
                sTm = [f.sbuf(f"sTm{i}", [128, 128], BF16) for i in range(4)]
                yh = [f.sbuf(f"yh{i}", [128, 256], BF16) for i in range(4)]
                st = [f.sbuf(f"st{i}", [128, 8], F32) for i in range(4)]
                junk = f.sbuf("junk", [128, 256], F32)
                for h in range(4):
                    for (which, dstT) in ((0, qT), (1, kT)):
                        w_n = load_w(s_win, 4 + which * 4 + h); w_s = load_w(s_win, 44 + which * 4 + h)
                        for tb in range(4):
                            bn, bs = PS[(tb % 2) * 2], PS[(tb % 2) * 2 + 1]
                            for (wt, bank) in ((w_n, bn), (w_s, bs)):
                                for kc in range(8):
                                    f.op(pe, lambda e, wt=wt, bank=bank, kc=kc, tb=tb: e.matmul(bank[:, :], lhsT=wt[:, kc * 128:(kc + 1) * 128], rhs=hT[:, kc, tb * 512:(tb + 1) * 512], start=(kc == 0), stop=(kc == 7)), reads=[wt, hT], writes=[bank])
                            a1, a2 = r1[tb % 2], r2[tb % 2]
                            sl = slice(tb * 512, (tb + 1) * 512)
                            cosT, sinT = cst[0], snt[0]
                            f.dma(sp, cosT[:], cos_d.ap()[:, sl], writes=[cosT]); f.dma(sp, sinT[:], sin_d.ap()[:, sl], writes=[sinT])
                            f.op(dve, lambda e, a1=a1, bn=bn, cosT=cosT: e.tensor_tensor(out=a1[:], in0=bn[:, :], in1=cosT[:], op=ALU.mult), reads=[cosT], writes=[a1, bn])
                            f.op(dve, lambda e, a2=a2, bs=bs, sinT=sinT: e.tensor_tensor(out=a2[:], in0=bs[:, :], in1=sinT[:], op=ALU.mult), reads=[sinT], writes=[a2, bs])
                            if which == 0:
                                f.op(pool, lambda e, a1=a1, a2=a2: e.tensor_tensor(out=a1[:], in0=a1[:], in1=a2[:], op=ALU.add), reads=[a1, a2], writes=[a1])
                                f.op(act, lambda e, a1=a1, sl=sl: e.copy(out=qT[:, sl], in_=a1[:]), reads=[a1], writes=[qT])
                            else:
                                f.op(pool, lambda e, a1=a1, a2=a2, sl=sl: e.tensor_tensor(out=kT[:, sl], in0=a1[:], in1=a2[:], op=ALU.add), reads=[a1, a2], writes=[kT])
                    cast_some(1)
                    wv0 = load_w(s_win, 12 + 2 * h); wv1 = load_w(s_win, 13 + 2 * h)
                    for c in range(16):
                        bank = PS[6 + (c % 2)]
                        for j, wv in enumerate((wv0, wv1)):
                            for kc in range(8):
                                f.op(pe, lambda e, c=c, bank=bank, j=j, wv=wv, kc=kc: e.matmul(bank[:, j * 128:(j + 1) * 128], lhsT=hT[:, kc, c * 128:(c + 1) * 128], rhs=wv[:, kc * 128:(kc + 1) * 128], start=(kc == 0), stop=(kc == 7)), reads=[hT, wv], writes=[bank])
                        f.op(act, lambda e, c=c, bank=bank: e.copy(out=V[:, c, :], in_=bank[:, 0:256]), writes=[V, bank])
                    cast_some(1)
                    for j in range(2):
                        wg = load_w(s_win, 20 + 2 * h + j)
                        for tb in range(4):
                            bank = PS[tb % 4]
                            for kc in range(8):
                                f.op(pe, lambda e, wg=wg, bank=bank, kc=kc, tb=tb: e.matmul(bank[:, :], lhsT=wg[:, kc * 128:(kc + 1) * 128], rhs=hT[:, kc, tb * 512:(tb + 1) * 512], start=(kc == 0), stop=(kc == 7)), reads=[wg, hT], writes=[bank])
                            f.op(act, lambda e, j=j, tb=tb, bank=bank: e.activation(out=sg[:, j, tb * 512:(tb + 1) * 512], in_=bank[:, :], func=AF.Silu), writes=[sg, bank])
                    cast_some(1)
                    for (dirn, S_, S0_, Sbf_, order, pb, kb, dcol) in (
                            (0, Sf, Sf0, Sf_bf, list(range(15)), 0, 4, h),
                            (1, Sb, Sb0, Sb_bf, list(range(15, 0, -1)), 2, 6, 4 + h)):
                        first = 0 if dirn == 0 else 15
                        f.op(dve, lambda e, S_=S_, S0_=S0_: e.tensor_copy(out=S_[:], in_=S0_[:, h, :]), reads=[S0_], writes=[S_])
                        f.op(act, lambda e, Sbf_=Sbf_, S0_=S0_, first=first: e.copy(out=Sbf_[:, first, :], in_=S0_[:, h, :]), reads=[S0_], writes=[Sbf_])

                        def tr(c, pb=pb, dcol=dcol):
                            kt_ = ktm[c % 2]
                            f.op(pe, lambda e: e.transpose(psbf(pb + c % 2, 128), kT[:, c * 128:(c + 1) * 128], ident_bf[:]), reads=[kT, ident_bf], writes=[PS[pb + c % 2]])
                            f.op(act, lambda e: e.activation(out=kt_[:], in_=psbf(pb + c % 2, 128), func=AF.Copy, scale=kdec[:, dcol:dcol + 1]), reads=[kdec], writes=[kt_, PS[pb + c % 2]])

                        tr(order[0])
                        for n_, c in enumerate(order):
                            bank = PS[kb + (c % 2)]
                            kt_ = ktm[c % 2]
                            f.op(pe, lambda e, c=c, bank=bank, kt_=kt_: e.matmul(bank[:, 0:256], lhsT=kt_[:], rhs=V[:, c, :], start=True, stop=True), reads=[kt_, V], writes=[bank])
                            if n_ + 1 < len(order):
                                tr(order[n_ + 1])
                            f.op(dve, lambda e, bank=bank, S_=S_, dcol=dcol: e.scalar_tensor_tensor(out=S_[:], in0=S_[:], scalar=blk[:, dcol:dcol + 1], in1=bank[:, 0:256], op0=ALU.mult, op1=ALU.add), reads=[S_, blk], writes=[S_, bank])
                            nxt_c = c + 1 if dirn == 0 else c - 1
                            f.op(act, lambda e, nxt_c=nxt_c, S_=S_, Sbf_=Sbf_: e.copy(out=Sbf_[:, nxt_c, :], in_=S_[:]), reads=[S_], writes=[Sbf_])
                    cast_some(1)
                    def chunk_gen(c, h=h):
                        i2 = c % 4
                        cs = slice(c * 128, (c + 1) * 128)
                        bank_ = PS[i2]
                        bs_ = bank_
                        ys_ = bank_[:, 128:384]
                        tr_ = bank_.t[:, 384:512].bitcast(BF16)
                        qf_, qb_ = qfc[i2], qbc[i2]
                        s_ = st[i2]
                        f.op(pe, lambda e: e.matmul(bs_[:, 0:128], lhsT=kT[:, cs], rhs=qT[:, cs], start=True, stop=True), reads=[kT, qT], writes=[bs_])
                        f.op(pool, lambda e: e.tensor_tensor(out=qf_[:], in0=qT[:, cs], in1=qdec[:, h, :], op=ALU.mult), reads=[qT, qdec], writes=[qf_])
                        f.op(pool, lambda e: e.tensor_tensor(out=qb_[:], in0=qT[:, cs], in1=qdec[:, 4 + h, :], op=ALU.mult), reads=[qT, qdec], writes=[qb_])
                        yield
                        f.op(dve, lambda e: e.tensor_tensor(out=sTm[i2][:], in0=bs_[:, 0:128], in1=dmask[:, h, :], op=ALU.mult), reads=[dmask], writes=[sTm[i2], bs_])
                        yield
                        f.op(pe, lambda e: e.matmul(ys_, lhsT=sTm[i2][:], rhs=V[:, c, :], start=True, stop=False), reads=[sTm[i2], V], writes=[bank_])
                        f.op(pe, lambda e: e.matmul(ys_, lhsT=qf_[:], rhs=Sf_bf[:, c, :], start=False, stop=False), reads=[qf_, Sf_bf], writes=[bank_])
                        f.op(pe, lambda e: e.matmul(ys_, lhsT=qb_[:], rhs=Sb_bf[:, c, :], start=False, stop=True), reads=[qb_, Sb_bf], writes=[bank_])
                        if debug and b == 0 and h == 0 and c == 3:
                            ydbg = f.sbuf("ydbg", [128, 256], F32)
                            f.op(act, lambda e: e.copy(out=ydbg[:], in_=ys_), writes=[ydbg, bank_])
                            dbg("y03", ydbg, ydbg[:], [128, 256])
                        yield
                        f.op(act, lambda e: e.activation(out=junk[:], in_=ys_, func=AF.Identity, accum_out=s_[:, 0:1]), writes=[junk, s_, bank_])
                        f.op(act, lambda e: e.activation(out=junk[:], in_=ys_, func=AF.Square, accum_out=s_[:, 1:2]), writes=[junk, s_, bank_])
                        yield
                        f.op(dve, lambda e: e.tensor_scalar(out=s_[:, 2:4], in0=s_[:, 0:2], scalar1=1.0 / 256, scalar2=None, op0=ALU.mult), reads=[s_], writes=[s_])
                        f.op(dve, lambda e: e.scalar_tensor_tensor(out=s_[:, 4:5], in0=s_[:, 2:3], scalar=s_[:, 2:3], in1=s_[:, 3:4], op0=ALU.mult, op1=ALU.subtract), reads=[s_], writes=[s_])
                        yield
                        f.op(act, lambda e: e.activation(out=s_[:, 7:8], in_=s_[:, 4:5], func=AF.Sqrt, scale=-1.0, bias=epsb[:, 0:1]), reads=[s_, epsb], writes=[s_])
                        yield
                        f.op(dve, lambda e: e.reciprocal(out=s_[:, 6:7], in_=s_[:, 7:8]), reads=[s_], writes=[s_])
                        f.op(dve, lambda e: e.tensor_scalar(out=yh[i2][:], in0=ys_, scalar1=s_[:, 2:3], scalar2=s_[:, 6:7], op0=ALU.subtract, op1=ALU.mult), reads=[s_], writes=[yh[i2], bank_])
                        yield
                        for j in range(2):
                            f.op(pe, lambda e, j=j: e.transpose(tr_[:, j * 128:(j + 1) * 128], yh[i2][:, j * 128:(j + 1) * 128], ident_bf[:]), reads=[yh[i2], ident_bf], writes=[bank_])
                        yield
                        for j in range(2):
                            f.op(dve, lambda e, j=j: e.scalar_tensor_tensor(out=zT[:, 2 * h + j, cs], in0=tr_[:, j * 128:(j + 1) * 128], scalar=rng[:, 2 * h + j:2 * h + j + 1], in1=sg[:, j, cs], op0=ALU.mult, op1=ALU.mult), reads=[rng, sg], writes=[zT, bank_])
                        yield

                    active = []
                    pending = list(range(16))
                    since = 99
                    while pending or active:
                        if pending and len(active) < 4 and since >= 2:
                            active.append(chunk_gen(pending.pop(0))); since = 0
                        for g_ in list(active):
                            try:
                                next(g_)
                            except StopIteration:
                                active.remove(g_)
                        since += 1
            if debug and b == 0:
                zdbg = f.sbuf("zdbg", [128, 512], F32)
                f.op(act, lambda e: e.copy(out=zdbg[:], in_=zT[:, 0, 0:512]), reads=[zT], writes=[zdbg])
                dbg("zT0", zdbg, zdbg[:], [128, 512])

            cast_some(99)
            with f.scope():
                PADW = L + 16
                pa = f.sbuf("pa", [128, PADW], F32); pb = f.sbuf("pb", [128, PADW], F32); pc = f.sbuf("pc", [128, PADW], F32)
                icnt = f.sbuf("icnt", [128, L], F32)
                dT = f.sbuf("dT", [128, L], BF16)
                for buf_ in (pa, pb, pc):
                    f.op(pool, lambda e, buf_=buf_: e.memset(buf_[:], 0.0), writes=[buf_])
                for gi in range(4):
                    wp = load_w(s_win, gi)
                    f.dma(sp, icnt[:], icnt_d.ap()[gi], writes=[icnt])
                    for tb in range(4):
                        bank = PS[tb]
                        for kc in range(8):
                            f.op(pe, lambda e, wp=wp, bank=bank, kc=kc, tb=tb: e.matmul(bank[:, :], lhsT=wp[:, kc * 128:(kc + 1) * 128], rhs=hT[:, kc, tb * 512:(tb + 1) * 512], start=(kc == 0), stop=(kc == 7)), reads=[wp, hT], writes=[bank])
                        f.op(act, lambda e, bank=bank, tb=tb: e.copy(out=pa[:, 8 + tb * 512:8 + (tb + 1) * 512], in_=bank[:, :]), writes=[pa, bank])
                    f.op(dve, lambda e: e.tensor_tensor(out=pb[:, 1:PADW], in0=pa[:, 0:PADW - 1], in1=pa[:, 1:PADW], op=ALU.add), reads=[pa], writes=[pb])
                    cur, oth = pb, pc
                    sh = 1
                    for lvl in range(gi):
                        f.op(dve, lambda e, cur=cur, oth=oth, sh=sh: e.tensor_tensor(out=oth[:, 8:8 + L + 1], in0=cur[:, 8 - sh:8 + L + 1 - sh], in1=cur[:, 8 + sh:8 + L + 1 + sh], op=ALU.add), reads=[cur], writes=[oth])
                        if lvl + 1 < gi:
                            f.op(dve, lambda e, cur=cur, oth=oth, sh=sh: e.tensor_tensor(out=oth[:, sh:8], in0=cur[:, 0:8 - sh], in1=cur[:, 2 * sh:8 + sh], op=ALU.add), reads=[cur], writes=[oth])
                            f.op(dve, lambda e, cur=cur, oth=oth, sh=sh: e.tensor_tensor(out=oth[:, 8 + L + 1:PADW - sh], in0=cur[:, 8 + L + 1 - sh:PADW - 2 * sh], in1=cur[:, 8 + L + 1 + sh:PADW], op=ALU.add), reads=[cur], writes=[oth])
                        cur, oth = oth, cur
                        sh *= 2
                    f.op(dve, lambda e, cur=cur, oth=oth: e.tensor_tensor(out=oth[:, 8:8 + L], in0=cur[:, 8:8 + L], in1=icnt[:], op=ALU.mult), reads=[cur, icnt], writes=[oth])
                    f.op(pool, lambda e, oth=oth: e.tensor_tensor(out=dT[:], in0=oth[:, 8:8 + L], in1=pa[:, 8:8 + L], op=ALU.subtract), reads=[oth, pa], writes=[dT])
                    f.op(pool, lambda e, oth=oth: e.memset(oth[:], 0.0), writes=[oth])
                    f.op(pool, lambda e, cur=cur: e.memset(cur[:], 0.0), writes=[cur])
                    for tb in range(4):
                        bank = PS[4 + (tb % 2)]
                        f.op(pe, lambda e, bank=bank, tb=tb, gi=gi: e.matmul(bank[:, :], lhsT=poolw[:, gi * 128:(gi + 1) * 128], rhs=dT[:, tb * 512:(tb + 1) * 512], start=True, stop=True), reads=[poolw, dT], writes=[bank])
                        f.op(act, lambda e, bank=bank, tb=tb, gi=gi: e.activation(out=mixedT[:, gi, tb * 512:(tb + 1) * 512], in_=bank[:, :], func=AF.Copy, scale=pscale[:, gi:gi + 1]), reads=[pscale], writes=[mixedT, bank])
            if debug and b == 0:
                mdbg = f.sbuf("mdbg", [128, 512], F32)
                f.op(act, lambda e: e.copy(out=mdbg[:], in_=mixedT[:, 2, 0:512]), reads=[mixedT], writes=[mdbg])
                dbg("mixedT2", mdbg, mdbg[:], [128, 512])

            with f.scope():
                s1 = [f.sbuf(f"s1_{i}", [128, 512], F32) for i in range(2)]
                s2 = [f.sbuf(f"s2_{i}", [128, 512], F32) for i in range(2)]
                for j in range(8):
                    wpo = load_w(s_poolout, j, 512); wro = load_w(s_retout, j); wgp = load_w(s_win, 28 + j); wgr = load_w(s_win, 36 + j)
                    for tb in range(4):
                        sl = slice(tb * 512, (tb + 1) * 512)
                        o = (tb % 2) * 4
                        bA, bB, bC, bD = PS[o], PS[o + 1], PS[o + 2], PS[o + 3]
                        for gi in range(4):
                            f.op(pe, lambda e, gi=gi, bA=bA, sl=sl, wpo=wpo: e.matmul(bA[:, :], lhsT=wpo[:, gi * 128:(gi + 1) * 128], rhs=mixedT[:, gi, sl], start=(gi == 0), stop=(gi == 3)), reads=[wpo, mixedT], writes=[bA])
                        for kc in range(8):
                            f.op(pe, lambda e, kc=kc, bB=bB, sl=sl, wro=wro: e.matmul(bB[:, :], lhsT=wro[:, kc * 128:(kc + 1) * 128], rhs=zT[:, kc, sl], start=(kc == 0), stop=(kc == 7)), reads=[wro, zT], writes=[bB])
                        for kc in range(8):
                            f.op(pe, lambda e, kc=kc, bC=bC, sl=sl, wgp=wgp: e.matmul(bC[:, :], lhsT=wgp[:, kc * 128:(kc + 1) * 128], rhs=hT[:, kc, sl], start=(kc == 0), stop=(kc == 7)), reads=[wgp, hT], writes=[bC])
                        for kc in range(8):
                            f.op(pe, lambda e, kc=kc, bD=bD, sl=sl, wgr=wgr: e.matmul(bD[:, :], lhsT=wgr[:, kc * 128:(kc + 1) * 128], rhs=hT[:, kc, sl], start=(kc == 0), stop=(kc == 7)), reads=[wgr, hT], writes=[bD])
                        a1, a2 = s1[tb % 2], s2[tb % 2]
                        f.op(act, lambda e, a1=a1, bC=bC: e.activation(out=a1[:], in_=bC[:, :], func=AF.Sigmoid), writes=[a1, bC])
                        f.op(act, lambda e, a2=a2, bD=bD: e.activation(out=a2[:], in_=bD[:, :], func=AF.Sigmoid), writes=[a2, bD])
                        f.op(dve, lambda e, a1=a1, bA=bA: e.tensor_tensor(out=a1[:], in0=bA[:, :], in1=a1[:], op=ALU.mult), reads=[a1], writes=[a1, bA])
                        f.op(dve, lambda e, a2=a2, bB=bB: e.tensor_tensor(out=a2[:], in0=bB[:, :], in1=a2[:], op=ALU.mult), reads=[a2], writes=[a2, bB])
                        f.op(pool, lambda e, a1=a1, a2=a2, j=j, sl=sl: e.tensor_tensor(out=mergedT[:, j, sl], in0=a1[:], in1=a2[:], op=ALU.add), reads=[a1, a2], writes=[mergedT])

        _scS.__exit__(None, None, None)
        with f.scope():
            with f.scope():
                xr = [f.sbuf(f"xr{i}", [128, L], F32) for i in range(2)]
                for j in range(8):
                    wo = load_w(s_wout, j)
                    xb_ = xr[j % 2]
                    f.dma(sp, xb_[:], xT_d.ap()[b, j], writes=[xb_])
                    for tb in range(4):
                        sl = slice(tb * 512, (tb + 1) * 512)
                        bank = PS[tb % 4]
                        for kc in range(8):
                            f.op(pe, lambda e, kc=kc, bank=bank, sl=sl, wo=wo: e.matmul(bank[:, :], lhsT=wo[:, kc * 128:(kc + 1) * 128], rhs=mergedT[:, kc, sl], start=(kc == 0), stop=(kc == 7)), reads=[wo, mergedT], writes=[bank])
                        f.op(dve, lambda e, bank=bank, sl=sl, j=j, xb_=xb_: e.scalar_tensor_tensor(out=x1T[:, j, sl], in0=bank[:, :], scalar=modv[:, 16 + j, b:b + 1], in1=xb_[:, sl], op0=ALU.mult, op1=ALU.add), reads=[modv, xb_], writes=[x1T, bank])
            if b == 0:
                dbg("x1T0", x1T, x1T[:, 0, :], [128, L])

            NT = TB // 128
            f.barrier()
            keysT = f.sbuf("keysT", [128, 2048], BF16)
            _ap, _rd = s_keys.rows(0, 128)
            f.dma(sp, keysT[:], _ap, reads=_rd, writes=[keysT])
            hfT = f.sbuf("hfT", [128, 8, TB], BF16)
            htmp = f.sbuf("htmp", [128, TB], F32)
            rstd2 = f.sbuf("rstd2", [128, TB], F32)
            qTc = [f.sbuf(f"qTc{i}", [128, TB], BF16) for i in range(2)]
            s_sb = [f.sbuf(f"s_sb{i}", [128, 128], F32) for i in range(2)]
            wk = [f.sbuf(f"wk{i}", [128, 256], F32) for i in range(2)]
            topvs = [f.sbuf(f"topv{i}", [128, 16, 16], F32) for i in range(NT)]
            topis = [f.sbuf(f"topi{i}", [128, 16, 16], U32) for i in range(NT)]
            topif = f.sbuf("topif", [128, 16, 16], F32)
            cand = f.sbuf("cand", [128, 8, 256], F32)
            best = f.sbuf("best", [128, 8, 16], F32); posu = f.sbuf("posu", [128, 8, 16], U32)
            bests = [f.sbuf(f"bests{i}", [128, 16], F32) for i in range(2)]
            posus = [f.sbuf(f"posus{i}", [128, 16], U32) for i in range(2)]
            xu = f.sbuf("xu", [128, 128], U32)
            xf = f.sbuf("xf", [128, 128], F32)
            IJG = [f.sbuf(f"IJG{i}", [128, 128], F32) for i in range(3)]
            IJGT = [f.sbuf(f"IJGT{i}", [128, 128], BF16) for i in range(3)]
            ebuf = xf; zs = f.sbuf("zs", [128, 16], F32)
            TG = 8
            Rg = [f.sbuf(f"Rg{i}", [128, TG, 64], BF16) for i in range(2)]
            Cg = [f.sbuf(f"Cg{i}", [128, TG, 128], BF16) for i in range(2)]
            iota_g = APX(iota_bf[:], [[0, TG], [1, 128]])
            gl = [f.sbuf(f"gl{i}", [128, TB], BF16) for i in range(3)]
            PT = [f.sbuf(f"PT{i}", [128, TB], BF16) for i in range(3)]
            GRP = 2
            ustr = [f.sbuf(f"ustr{i}", [128, GRP, 1024], BF16) for i in range(2)]
            vstr = [f.sbuf(f"vstr{i}", [128, GRP, 1024], BF16) for i in range(2)]
            iota16 = APX(iota_f[:, 0:16], [[0, 8], [0, 16], [1, 16]])
            ssv = [(s_sb[0][:], s_sb[0]), (s_sb[1][:], s_sb[1]),
                   (topif.t[:, 0:8, :].rearrange("p a b -> p (a b)"), topif), (topif.t[:, 8:16, :].rearrange("p a b -> p (a b)"), topif)]
            wkv = [Buf(wk[0].t[:, 0:128], "wkv0"), Buf(wk[0].t[:, 128:256], "wkv1"), Buf(wk[1].t[:, 0:128], "wkv2"), Buf(wk[1].t[:, 128:256], "wkv3")]

            eq = cand

            def v4(bf_):
                return bf_[:].rearrange("p h (x y) -> p h x y", x=16)

            hfT1 = f.sbuf("hfT1", [128, 8, TB], BF16)
            wstate["n"] = 2; wstate["i"] = 0

            def hfa(blk, kc):
                k3 = blk % 3
                if k3 == 0:
                    return hfT[:, kc, :]
                if k3 == 1:
                    return hfT1[:, kc, :]
                wb_ = wbufs[2 + kc // 4]
                return wb_[:, (kc % 4) * TB:(kc % 4 + 1) * TB]

            def hfb(blk):
                k3 = blk % 3
                return [hfT] if k3 == 0 else ([hfT1] if k3 == 1 else [wbufs[2], wbufs[3]])
            IJGTs = [[IJGT, [f.sbuf(f"IJGTb{i}", [128, 128], BF16) for i in range(3)]],
                     [[f.sbuf(f"IJGTc{i}", [128, 128], BF16) for i in range(3)], [f.sbuf(f"IJGTd{i}", [128, 128], BF16) for i in range(3)]]]
            NBLK = L // TB

            def topk_gen(blk_i):
                tsl = slice(blk_i * TB, (blk_i + 1) * TB)
                for kc in range(8):
                    f.op(act, lambda e, kc=kc: e.activation(out=htmp[:], in_=x1T[:, kc, tsl], func=AF.Square), reads=[x1T], writes=[htmp])
                    f.op(pe, lambda e, kc=kc: e.matmul(PS[6][:, 0:TB], lhsT=ones_f[:], rhs=htmp[:], start=(kc == 0), stop=(kc == 7)), reads=[ones_f, htmp], writes=[PS[6]])
                rstd_from(PS[6][:, 0:TB], rstd2[:], PS[6], rstd2)
                yield
                for kc in range(8):
                    f.op(dve, lambda e, kc=kc: e.scalar_tensor_tensor(out=htmp[:], in0=x1T[:, kc, tsl], scalar=scl2[:, kc, b:b + 1], in1=rstd2[:], op0=ALU.mult, op1=ALU.mult), reads=[x1T, scl2, rstd2], writes=[htmp])
                    f.op(act, lambda e, kc=kc: e.activation(out=hfa(blk_i, kc), in_=htmp[:], func=AF.Identity, bias=modv[:, 24 + kc, b:b + 1]), reads=[htmp, modv], writes=hfb(blk_i))
                    if kc % 2 == 1:
                        yield
                if debug and b == 0 and blk_i == 0:
                    hdbg = f.sbuf("hdbg", [128, TB], F32)
                    f.op(act, lambda e: e.copy(out=hdbg[:], in_=hfa(blk_i, 0)), reads=hfb(blk_i), writes=[hdbg])
                    dbg("hfT0", hdbg, hdbg[:], [128, TB])
                for g0 in range(0, 16, 2):
                    for g in (g0, g0 + 1):
                        wq_ = load_w(s_wq, g)
                        qb_ = qTc[g % 2]
                        for kc in range(8):
                            f.op(pe, lambda e, kc=kc, wq_=wq_: e.matmul(PS[6][:, 0:TB], lhsT=wq_[:, kc * 128:(kc + 1) * 128], rhs=hfa(blk_i, kc), start=(kc == 0), stop=(kc == 7)), reads=[wq_] + hfb(blk_i), writes=[PS[6]])
                        f.op(act, lambda e, qb_=qb_: e.copy(out=qb_[:], in_=PS[6][:, 0:TB]), writes=[qb_, PS[6]])
                    yield
                    chains = []
                    for g in (g0, g0 + 1):
                        qb_ = qTc[g % 2]
                        for tt in range(NT):
                            col = ((g % 2) * NT + tt) * 128
                            f.op(pe, lambda e, tt=tt, qb_=qb_, g=g, col=col: e.matmul(PS[7][:, col:col + 128], lhsT=qb_[:, tt * 128:(tt + 1) * 128], rhs=keysT[:, g * 128:(g + 1) * 128], start=True, stop=True), reads=[qb_, keysT], writes=[PS[7]])
                    for g in (g0, g0 + 1):
                        for tt in range(NT):
                            ci_ = (g % 2) * NT + tt
                            col = ci_ * 128
                            ss_ap, ss_b = ssv[ci_]
                            f.op(act, lambda e, ss_ap=ss_ap, col=col: e.copy(out=ss_ap, in_=PS[7][:, col:col + 128]), writes=[ss_b, PS[7]])
                            chains.append((g, tt, ss_ap, ss_b, wkv[ci_]))
                    for step in range(5):
                        for (g, tt, ss_ap, ss_b, w_) in chains:
                            TV, TI = topvs[tt], topis[tt]
                            if step == 0:
                                f.op(dve, lambda e, ss_ap=ss_ap, TV=TV, g=g: e.max(out=TV[:, g, 0:8], in_=ss_ap), reads=[ss_b], writes=[TV])
                            elif step == 1:
                                f.op(dve, lambda e, ss_ap=ss_ap, TV=TV, TI=TI, g=g: e.max_index(out=TI[:, g, 0:8], in_max=TV[:, g, 0:8], in_values=ss_ap), reads=[ss_b, TV], writes=[TI])
                            elif step == 2:
                                f.op(dve, lambda e, ss_ap=ss_ap, TV=TV, w_=w_, g=g: e.match_replace(out=w_[:], in_to_replace=TV[:, g, 0:8], in_values=ss_ap, imm_value=NEG), reads=[ss_b, TV], writes=[w_])
                            elif step == 3:
                                f.op(dve, lambda e, TV=TV, w_=w_, g=g: e.max(out=TV[:, g, 8:16], in_=w_[:]), reads=[w_], writes=[TV])
                            else:
                                f.op(dve, lambda e, TV=TV, TI=TI, w_=w_, g=g: e.max_index(out=TI[:, g, 8:16], in_max=TV[:, g, 8:16], in_values=w_[:]), reads=[w_, TV], writes=[TI])
                        if step in (1, 3):
                            yield
                    yield
                for tt in range(NT):
                    TV, TI = topvs[tt], topis[tt]
                    f.op(dve, lambda e, TI=TI: e.tensor_copy(out=topif[:], in_=TI[:]), reads=[TI], writes=[topif])
                    f.op(dve, lambda e, TV=TV: e.tensor_tensor(out=cand[:].rearrange("p h (x y) -> p h x y", x=16), in0=APX(TV[:], [[32, 8], [1, 16], [0, 16]]), in1=APX(TV[:, 1, :], [[32, 8], [0, 16], [1, 16]]), op=ALU.add), reads=[TV], writes=[cand])
                    yield
                    for hp in range(4):
                        for step in range(5):
                            for h in (2 * hp, 2 * hp + 1):
                                w_ = wk[h % 2]
                                bh = bests[h % 2]; ph = posus[h % 2]
                                if step == 0:
                                    f.op(dve, lambda e, h=h, bh=bh: e.max(out=bh[:, 0:8], in_=cand[:, h, :]), reads=[cand], writes=[bh])
                                elif step == 1:
                                    f.op(dve, lambda e, h=h, bh=bh, ph=ph: e.max_index(out=ph[:, 0:8], in_max=bh[:, 0:8], in_values=cand[:, h, :]), reads=[cand, bh], writes=[ph])
                                elif step == 2:
                                    f.op(dve, lambda e, h=h, w_=w_, bh=bh: e.match_replace(out=w_[:], in_to_replace=bh[:, 0:8], in_values=cand[:, h, :], imm_value=NEG), reads=[cand, bh], writes=[w_])
                                elif step == 3:
                                    f.op(dve, lambda e, h=h, w_=w_, bh=bh: e.max(out=bh[:, 8:16], in_=w_[:]), reads=[w_], writes=[bh])
                                else:
                                    f.op(dve, lambda e, h=h, w_=w_, bh=bh, ph=ph: e.max_index(out=ph[:, 8:16], in_max=bh[:, 8:16], in_values=w_[:]), reads=[w_, bh], writes=[ph])
                        for h in (2 * hp, 2 * hp + 1):
                            f.op(pool, lambda e, h=h: e.tensor_copy(out=best[:, h, :], in_=bests[h % 2][:]), reads=[bests[h % 2]], writes=[best])
                            f.op(pool, lambda e, h=h: e.tensor_copy(out=posu[:, h, :], in_=posus[h % 2][:]), reads=[posus[h % 2]], writes=[posu])
                        yield
                    pflat = posu[:].rearrange("p h k -> p (h k)")
                    for (src_, off, dst) in ((xf, 0, IJG[0]), (xf, 1, IJG[1])):
                        if off == 0:
                            f.op(dve, lambda e: e.tensor_scalar(out=xu[:], in0=pflat, scalar1=4, scalar2=None, op0=ALU.logical_shift_right), reads=[posu], writes=[xu])
                        else:
                            f.op(dve, lambda e: e.tensor_scalar(out=xu[:], in0=pflat, scalar1=15, scalar2=None, op0=ALU.bitwise_and), reads=[posu], writes=[xu])
                        f.op(dve, lambda e: e.tensor_copy(out=xf[:], in_=xu[:]), reads=[xu], writes=[xf])
                        f.op(dve, lambda e, src_=src_: e.tensor_tensor(out=v4(eq), in0=APX(src_[:], [[16, 8], [1, 16], [0, 16]]), in1=iota16, op=ALU.is_equal), reads=[src_, iota_f], writes=[eq])
                        f.op(dve, lambda e, off=off: e.tensor_tensor(out=v4(eq), in0=v4(eq), in1=APX(topif[:, off, :], [[32, 8], [0, 16], [1, 16]]), op=ALU.mult), reads=[eq, topif], writes=[eq])
                        f.op(dve, lambda e, dst=dst: e.tensor_reduce(out=dst[:].rearrange("p (h k) -> p h k", h=8), in_=v4(eq), axis=AX.X, op=ALU.add), reads=[eq], writes=[dst])
                        yield
                    e3 = ebuf[:].rearrange("p (h k) -> p h k", h=8)
                    f.op(dve, lambda e: e.tensor_tensor(out=e3, in0=best[:], in1=APX(best[:], [[16, 8], [0, 16]]), op=ALU.subtract), reads=[best], writes=[ebuf])
                    f.op(act, lambda e: e.activation(out=ebuf[:], in_=ebuf[:], func=AF.Exp), reads=[ebuf], writes=[ebuf])
                    f.op(dve, lambda e: e.tensor_reduce(out=zs[:, 0:8], in_=e3, axis=AX.X, op=ALU.add), reads=[ebuf], writes=[zs])
                    f.op(dve, lambda e: e.reciprocal(out=zs[:, 8:16], in_=zs[:, 0:8]), reads=[zs], writes=[zs])
                    f.op(dve, lambda e: e.tensor_tensor(out=IJG[2][:].rearrange("p (h k) -> p h k", h=8), in0=e3, in1=APX(zs[:, 8:16], [[1, 8], [0, 16]]), op=ALU.mult), reads=[ebuf, zs], writes=[IJG[2]])
                    yield
                    if debug and b == 0 and blk_i == 0 and tt == 0:
                        for i_, nm_ in enumerate(("I", "J", "G")):
                            dbg("peer" + nm_, IJG[i_], IJG[i_][:], [128, 128])
                    for i_ in range(3):
                        f.op(pe, lambda e, i_=i_: e.transpose(PS[6][:, 0:128], IJG[i_][:], ident_f[:]), reads=[IJG[i_], ident_f], writes=[PS[6]])
                        f.op(act, lambda e, i_=i_, tt=tt: e.copy(out=IJGTs[blk_i % 2][tt][i_][:], in_=PS[6][:, 0:128]), writes=[IJGTs[blk_i % 2][tt][i_], PS[6]])
                    yield

            def build_W(blk_i, half):
                i0 = half * 64
                wbuf_ = WTh[half]
                iota_h = APX(iota_bf[:, i0:i0 + 64], [[0, TG], [1, 64]])

                def pe_part(tt, tg, R_, C_):
                    bank = PS[6] if tg % 2 == 0 else PS[7]
                    for tl in range(TG):
                        f.op(pe, lambda e, bank=bank, tl=tl: e.matmul(bank[:, tl * 64:(tl + 1) * 64], lhsT=C_[:, tl, :], rhs=R_[:, tl, :], start=True, stop=True), reads=[R_, C_], writes=[bank])

                def evac_part(tt, tg, R_, C_):
                    bank = PS[6] if tg % 2 == 0 else PS[7]
                    tb0 = tt * 128 + tg * TG
                    f.op(act, lambda e, bank=bank, tb0=tb0: e.copy(out=WT[:, tb0:tb0 + TG, i0:i0 + 64], in_=bank[:, :].rearrange("p (t i) -> p t i", i=64)), writes=[wbuf_, bank])

                prev = None
                n_ = 0
                for tt in range(NT):
                    IJGT_ = IJGTs[blk_i % 2][tt]
                    for tg in range(128 // TG):
                        if prev is not None:
                            pe_part(*prev)
                        yield "pe"
                        if prev is not None:
                            evac_part(*prev)
                        R_, C_ = Rg[n_ % 2], Cg[n_ % 2]
                        n_ += 1
                        ts0 = tg * TG
                        f.op(dve, lambda e, ts0=ts0, R_=R_: e.tensor_tensor(out=R_[:], in0=iota_h, in1=APX(IJGT_[0][:, ts0:ts0 + TG], [[1, TG], [0, 64]]), op=ALU.is_equal), reads=[iota_bf, IJGT_[0]], writes=[R_])
                        f.op(pool, lambda e, ts0=ts0, R_=R_: e.tensor_tensor(out=R_[:], in0=R_[:], in1=APX(IJGT_[2][:, ts0:ts0 + TG], [[1, TG], [0, 64]]), op=ALU.mult), reads=[R_, IJGT_[2]], writes=[R_])
                        f.op(dve, lambda e, ts0=ts0, C_=C_: e.tensor_tensor(out=C_[:], in0=iota_g, in1=APX(IJGT_[1][:, ts0:ts0 + TG], [[1, TG], [0, 128]]), op=ALU.is_equal), reads=[iota_bf, IJGT_[1]], writes=[C_])
                        prev = (tt, tg, R_, C_)
                        yield "rest"
                pe_part(*prev)
                yield "pe"
                evac_part(*prev)
                yield "rest"

            bgs = {"gen": None, "blk": -1, "next": 0, "done": set()}

            def bg_step():
                if bgs["gen"] is None:
                    if bgs["next"] >= NBLK:
                        return False
                    bgs["blk"] = bgs["next"]; bgs["next"] += 1
                    bgs["gen"] = topk_gen(bgs["blk"])
                try:
                    next(bgs["gen"])
                except StopIteration:
                    bgs["gen"] = None; bgs["done"].add(bgs["blk"])
                return True

            def bg_finish(blk):
                while blk < NBLK and blk not in bgs["done"]:
                    bg_step()

            def experts(blk_i, genB, genA_fn):
                strm = {}

                def load_grp(grp):
                    ub, vb = ustr[grp % 2], vstr[grp % 2]
                    _ap, _rd = s_u.rows(grp * GRP * 128, (grp + 1) * GRP * 128)
                    f.dma(sp, ub[:], _ap.rearrange("(c p) x -> p c x", p=128), reads=_rd, writes=[ub])
                    _ap, _rd = s_v.rows(grp * GRP * 128, (grp + 1) * GRP * 128)
                    f.dma(sp, vb[:], _ap.rearrange("(c p) x -> p c x", p=128), reads=_rd, writes=[vb])
                    strm[grp] = (ub, vb)

                def emit_A(i):
                    grp, ci = divmod(i, GRP)
                    if grp not in strm:
                        load_grp(grp)
                    ub = strm[grp][0]
                    bankA = PS[4 + (i % 2)]
                    g_, p_ = gl[i % 3], PT[i % 3]
                    for kc in range(8):
                        f.op(pe, lambda e, kc=kc, ub=ub, ci=ci, bankA=bankA: e.matmul(bankA[:, 0:TB], lhsT=ub[:, ci, kc * 128:(kc + 1) * 128], rhs=hfa(blk_i, kc), start=(kc == 0), stop=(kc == 7)), reads=[ub] + hfb(blk_i), writes=[bankA])
                    f.op(act, lambda e, g_=g_, bankA=bankA: e.activation(out=g_[:], in_=bankA[:, 0:TB], func=AF.Gelu), writes=[g_, bankA])
                    f.op(pool, lambda e, g_=g_, p_=p_, i=i: e.tensor_tensor(out=p_[:], in0=g_[:], in1=WT[:, :, i], op=ALU.mult), reads=[g_, WTh[i // 64]], writes=[p_])

                def emit_out(i):
                    grp, ci = divmod(i, GRP)
                    vb = strm[grp][1]
                    p_ = PT[i % 3]
                    for dc in range(8):
                        bo = PS[dc // 2]
                        f.op(pe, lambda e, dc=dc, bo=bo, vb=vb, ci=ci, p_=p_, i=i: e.matmul(bo[:, (dc % 2) * TB:(dc % 2 + 1) * TB], lhsT=vb[:, ci, dc * 128:(dc + 1) * 128], rhs=p_[:], start=(i == 0), stop=(i == 127)), reads=[vb, p_], writes=[bo])

                emit_A(0)
                genA = None
                for i in range(128):
                    wgen = None
                    if i < 64:
                        if genB is not None and (i % 2 == 0 or i == 1):
                            wgen = genB
                    else:
                        if i == 64 and genA_fn is not None:
                            genA = genA_fn()
                        if genA is not None and (i % 2 == 0 or i == 65):
                            wgen = genA
                    if wgen is not None:
                        next(wgen, None)
                    if i + 1 < 128:
                        emit_A(i + 1)
                    emit_out(i)
                    if wgen is not None:
                        next(wgen, None)
                    if i < 64:
                        if genB is not None and i == 62:
                            for _ in genB:
                                pass
                        if i % 2 == 1 and (blk_i + 1) not in bgs["done"] and blk_i + 1 < NBLK:
                            bg_step()
                        if i == 61:
                            bg_finish(blk_i + 1)
                    else:
                        if i % 2 == 1:
                            cur = bgs["blk"] if bgs["gen"] is not None else bgs["next"]
                            if cur == blk_i + 2 and cur < NBLK:
                                bg_step()
                if genA is not None:
                    for _ in genA:
                        pass
                tsl = slice(blk_i * TB, (blk_i + 1) * TB)
                for dc in range(8):
                    bo = PS[dc // 2]
                    f.op(dve, lambda e, dc=dc, bo=bo: e.scalar_tensor_tensor(out=x1T[:, dc, tsl], in0=bo[:, (dc % 2) * TB:(dc % 2 + 1) * TB], scalar=modv[:, 40 + dc, b:b + 1], in1=x1T[:, dc, tsl], op0=ALU.mult, op1=ALU.add), reads=[modv, x1T], writes=[x1T, bo])

            bg_finish(0)
            for _ in build_W(0, 0):
                pass
            for blk_i in range(NBLK):
                nxt = blk_i + 1 < NBLK
                if os.environ.get("KDBG_SERIAL"):
                    if blk_i > 0:
                        bg_finish(blk_i)
                        for _ in build_W(blk_i, 0):
                            pass
                    for _ in build_W(blk_i, 1):
                        pass
                    experts(blk_i, None, None)
                    continue
                experts(blk_i, build_W(blk_i, 1), (lambda bi=blk_i + 1: build_W(bi, 0)) if nxt else None)
            wstate["n"] = NW
            if b == 0:
                dbg("x2T0", x1T, x1T[:, 0, :], [128, L])
            f.barrier()
            if True:
                xs = [Buf(U2.t[:, i * L:(i + 1) * L], f"xo{i}") for i in range(2)]
                rstd3 = Buf(U2.t[:, 2 * L:3 * L], "rstd3")
                for kc in range(8):
                    sq_ = xs[kc % 2]
                    f.op(act, lambda e, sq_=sq_, kc=kc: e.activation(out=sq_[:], in_=x1T[:, kc, :], func=AF.Square), reads=[x1T], writes=[sq_])
                    for tb in range(4):
                        f.op(pe, lambda e, tb=tb, sq_=sq_, kc=kc: e.matmul(PS[tb][:, :], lhsT=ones_f[:], rhs=sq_[:, tb * 512:(tb + 1) * 512], start=(kc == 0), stop=(kc == 7)), reads=[ones_f, sq_], writes=[PS[tb]])
                for tb in range(4):
                    rstd_from(PS[tb][:, :], rstd3[:, tb * 512:(tb + 1) * 512], PS[tb], rstd3)
                for kc in range(8):
                    o_ = xs[kc % 2]
                    f.op(dve, lambda e, o_=o_, kc=kc: e.scalar_tensor_tensor(out=o_[:], in0=x1T[:, kc, :], scalar=gfin[:, kc:kc + 1], in1=rstd3[:], op0=ALU.mult, op1=ALU.mult), reads=[x1T, gfin, rstd3], writes=[o_])
                    f.dma(sp, out_d.ap()[b, kc], o_[:], reads=[o_])
    f.barrier()
    stats = {e.name: (e.n_instr, e.n_wait) for e in f.engs}
    f.root.close()
    return nc, stats


def _chunked(W):
    K, N = W.shape
    return np.ascontiguousarray(W.reshape(K // 128, 128, N // 128, 128).transpose(2, 1, 0, 3)).reshape(N, K)


def _vec(v):
    n = v.size // 128
    return np.ascontiguousarray(v.reshape(n, 128).T)


def _consts():
    t = np.arange(L)
    row = (t // 64).astype(np.float32); col = (t % 64).astype(np.float32)
    inv = (np.float32(10000.0) ** (-np.arange(32, dtype=np.float32) / np.float32(32))).astype(np.float32)
    ang = np.concatenate([row[:, None] * inv, col[:, None] * inv], axis=-1).astype(np.float32)
    cos = np.cos(ang).astype(np.float32).T; sin = np.sin(ang).astype(np.float32).T
    ropecos = np.concatenate([cos, cos], 0); ropesin = np.concatenate([-sin, sin], 0)
    ic = np.zeros((4, 128, L), np.float32)
    for gi, w in enumerate((2, 4, 8, 16)):
        lo = np.clip(t - w // 2, 0, L); hi = np.clip(t + w - w // 2, 0, L)
        ic[gi] = (1.0 / (hi - lo).astype(np.float32))[None, :]
    return np.ascontiguousarray(ropecos), np.ascontiguousarray(ropesin), ic


def prepare_inputs(x, c, ctx, c_ctx, ada_w, ada_b, norm_mix_g, norm_ffn_g, w_in, pool_w, pool_scale,
                   pool_out, ret_decay, ret_norm_g, ret_out, w_out, peer_wq, peer_keys, peer_u, peer_v, final_g):
    f32 = lambda a: np.asarray(a, dtype=np.float32)
    x, c, ctx, c_ctx = f32(x), f32(c), f32(ctx), f32(c_ctx)
    w = f32(w_in)[0]
    qk = w[:, 512:1536].reshape(1024, 8, 2, 64)[:, :, ::-1, :].reshape(1024, 1024)
    w_all = np.concatenate([w, qk], axis=1)
    ropecos, ropesin, ic = _consts()
    u = f32(peer_u)[0]
    shared = {
        "ada_b": _vec(f32(ada_b)[0]), "g_mix": _vec(f32(norm_mix_g)[0]), "g_ffn": _vec(f32(norm_ffn_g)[0]),
        "g_fin": _vec(f32(final_g)), "pool_scale": _vec(f32(pool_scale)[0]), "ret_norm_g": _vec(f32(ret_norm_g)[0]),
        "ret_decay": np.ascontiguousarray(f32(ret_decay)[0].reshape(1, 8)),
        "ropecos": ropecos, "ropesin": ropesin, "invcnt": ic,
        "ada_w_r": _chunked(f32(ada_w)[0]), "w_in_r": _chunked(w_all),
        "pool_w": np.ascontiguousarray(f32(pool_w)[0].transpose(1, 0, 2)).reshape(128, 512),
        "pool_out_r": _chunked(f32(pool_out)[0]), "ret_out_r": _chunked(f32(ret_out)[0]),
        "w_out_r": _chunked(f32(w_out)[0]), "wq_r": _chunked(f32(peer_wq)[0]),
        "keysT": np.ascontiguousarray(f32(peer_keys)[0].reshape(16, 128, 128).transpose(2, 0, 1)).reshape(128, 2048),
        "uT_r": np.ascontiguousarray(u.reshape(128, 128, 8, 128).transpose(0, 3, 2, 1)).reshape(128 * 128, 1024),
        "v": np.ascontiguousarray(f32(peer_v)[0]),
    }
    in_maps = []
    for core in range(NCORES):
        bs = slice(core * BPC, (core + 1) * BPC)
        cc = np.concatenate([c[bs], c_ctx[None, :]], 0)
        m = dict(shared)
        m["xT"] = np.ascontiguousarray(x[bs].transpose(0, 2, 1)).reshape(BPC, 8, 128, L)
        m["ctxT"] = np.ascontiguousarray(ctx[bs].transpose(0, 2, 1)).reshape(BPC, 8, 128, LC)
        m["cT"] = np.ascontiguousarray(cc.reshape(5, 8, 128).transpose(2, 1, 0)).reshape(128, 40)
        in_maps.append(m)
    return in_maps


def kernel(**inputs):
    in_maps = prepare_inputs(**inputs)
    nc, _ = build_program()
    res = run_bass_kernel_spmd(nc, in_maps, core_ids=list(range(NCORES)))
    out = np.empty((NCORES * BPC, L, D), np.float32)
    for core in range(NCORES):
        o = np.asarray(res.results[core]["outT"]).reshape(BPC, D, L)
        out[core * BPC:(core + 1) * BPC] = o.transpose(0, 2, 1)
    return out
```

```python
import os, contextlib
import numpy as np
import concourse.bass as bass
import concourse.mybir as mybir
from concourse.bass_utils import run_bass_kernel_spmd

F32 = mybir.dt.float32; BF16 = mybir.dt.bfloat16; U32 = mybir.dt.uint32
ALU = mybir.AluOpType; AF = mybir.ActivationFunctionType; AX = mybir.AxisListType

NCORES = 8
D = 1024; L = 2048; BPC = 4; LC = 256
EPS = 1e-6
NEG = -1e30
TB = 256
K_SCALE = 128 ** -0.5


class Buf:
    __slots__ = ("t", "lw", "rd", "name")

    def __init__(self, t, name=""):
        self.t = t; self.lw = None; self.rd = {}; self.name = name

    def __getitem__(self, k):
        return self.t[k]


class EngW:
    SEM_LIMIT = 30000

    def __init__(self, fw, eng, name, is_pe=False):
        self.fw = fw; self.eng = eng; self.name = name; self.is_pe = is_pe
        self.sem = fw.new_sem(name + "_c0"); self.count = 0; self.nsem = 1
        self.waited = {}
        self.n_instr = 0; self.n_wait = 0

    def wait_tok(self, tok):
        sem, val = tok
        if self.waited.get(id(sem), 0) < val:
            self.eng.wait_ge(sem, val); self.waited[id(sem)] = val; self.n_wait += 1

    def bump(self, ins):
        if self.count >= self.SEM_LIMIT:
            self.sem = self.fw.new_sem(f"{self.name}_c{self.nsem}"); self.nsem += 1; self.count = 0
        self.count += 1; self.n_instr += 1
        ins.then_inc(self.sem, 1)
        return (self.sem, self.count)


class FW:
    def __init__(self, nc):
        self.nc = nc; self.root = contextlib.ExitStack(); self.es = self.root
        self.pe = EngW(self, nc.tensor, "pe", True)
        self.act = EngW(self, nc.scalar, "act")
        self.dve = EngW(self, nc.vector, "dve")
        self.pool = EngW(self, nc.gpsimd, "pool")
        self.sp = EngW(self, nc.sync, "sp")
        self.engs = [self.pe, self.act, self.dve, self.pool, self.sp]
        self.dma_pools = {"sp": [[self.new_sem(f"dmas{i}"), 0] for i in range(32)],
                          "pool": [[self.new_sem(f"dmap{i}"), 0] for i in range(16)]}
        self.dma_rr = {"sp": 0, "pool": 0}
        self.uid = 0

    def new_sem(self, name):
        return self.root.enter_context(self.nc.semaphore(name))

    def sbuf(self, name, shape, dt):
        self.uid += 1
        return Buf(self.es.enter_context(self.nc.sbuf_tensor(f"{name}_{self.uid}", list(shape), dt)), name)

    def psum(self, name, shape, dt=F32):
        return Buf(self.root.enter_context(self.nc.psum_tensor(name, list(shape), dt)), name)

    @contextlib.contextmanager
    def scope(self):
        old = self.es
        self.es = contextlib.ExitStack()
        try:
            yield
        finally:
            self.barrier()
            self.es.close()
            self.es = old

    def barrier(self):
        toks = [(e.sem, e.count) for e in self.engs if e.count > 0]
        toks += [(s, c) for s, c in self.dma_pools["sp"] if c > 0]
        for e in self.engs:
            for tok in toks:
                if tok[0] is e.sem:
                    continue
                e.wait_tok(tok)

    def _deps(self, ew, reads, writes):
        for b in reads:
            if b.lw is not None:
                self._w(ew, b.lw)
        for b in writes:
            if b.lw is not None:
                self._w(ew, b.lw)
            for tok in b.rd.values():
                self._w(ew, tok)

    def _w(self, ew, tok):
        if ew.is_pe and tok[0] is ew.sem:
            return
        ew.wait_tok(tok)

    def _upd(self, tok, reads, writes):
        k = id(tok[0])
        for b in reads:
            b.rd[k] = tok
        for b in writes:
            b.lw = tok; b.rd = {}

    def op(self, ew, fn, reads=(), writes=()):
        self._deps(ew, reads, writes)
        ins = fn(ew.eng)
        tok = ew.bump(ins)
        self._upd(tok, reads, writes)
        return tok

    def dma(self, ew, out, in_, reads=(), writes=()):
        pl = self.dma_pools[ew.name]
        ent = pl[self.dma_rr[ew.name]]; self.dma_rr[ew.name] = (self.dma_rr[ew.name] + 1) % len(pl)
        sem = ent[0]
        self._deps(ew, reads, writes)
        if ent[1] > 0:
            ew.wait_tok((sem, ent[1]))
        ew.eng.dma_start(out=out, in_=in_).then_inc(sem, 16)
        ent[1] += 16
        tok = (sem, ent[1])
        self._upd(tok, reads, writes)
        return tok


def APX(a, dims):
    return bass.AP(a.tensor, a.offset, [list(a.ap[0])] + [list(d) for d in dims])


def build_program(debug=False, nb=BPC):
    nc = bass.Bass("TRN2", target_bir_lowering=False)
    f = FW(nc)
    pe, act, dve, pool, sp = f.pe, f.act, f.dve, f.pool, f.sp

    def din(name, shape, dt=F32):
        return nc.dram_tensor(name, list(shape), dt, kind="ExternalInput")

    xT_d = din("xT", [BPC, 8, 128, L])
    ctxT_d = din("ctxT", [BPC, 8, 128, LC])
    cT_d = din("cT", [128, 8 * 5])
    adab_d = din("ada_b", [128, 48])
    gmix_d = din("g_mix", [128, 8]); gffn_d = din("g_ffn", [128, 8]); gfin_d = din("g_fin", [128, 8])
    pscale_d = din("pool_scale", [128, 4]); rng_d = din("ret_norm_g", [128, 8])
    decay_d = din("ret_decay", [1, 8])
    cos_d = din("ropecos", [128, L]); sin_d = din("ropesin", [128, L])
    icnt_d = din("invcnt", [4, 128, L])
    ada_d = din("ada_w_r", [48 * 128, 1024])
    win_d = din("w_in_r", [52 * 128, 1024])
    poolw_d = din("pool_w", [128, 512])
    poolout_d = din("pool_out_r", [8 * 128, 512])
    retout_d = din("ret_out_r", [8 * 128, 1024])
    wout_d = din("w_out_r", [8 * 128, 1024])
    wq_d = din("wq_r", [16 * 128, 1024])
    keys_d = din("keysT", [128, 2048])
    u_d = din("uT_r", [128 * 128, 1024])
    v_d = din("v", [128 * 128, 1024])
    out_d = nc.dram_tensor("outT", [BPC, 8, 128, L], F32, kind="ExternalOutput")

    dbg_outs = {}

    def dbg(name, buf, ap, shape):
        if not debug:
            return
        t = nc.dram_tensor("dbg_" + name, list(shape), F32, kind="ExternalOutput")
        dbg_outs[name] = f.dma(sp, t.ap(), ap, reads=[buf])

    class Scr:
        def __init__(self, name, src, rpp, order=None, defer=False):
            R, C = src.shape
            self.R = R; self.src = src
            self.t = nc.dram_tensor("scr_" + name, [R, C], BF16, kind="Internal")
            self.rpp = rpp
            n = (R + rpp - 1) // rpp
            self.pieces = [Buf(self.t, name) for _ in range(n)]
            self.order = list(order) if order is not None else list(range(n))
            if not defer:
                self.issue()

        def issue_piece(self, i):
            r0 = i * self.rpp
            r1 = min(self.R, r0 + self.rpp)
            f.dma(pool, self.t.ap()[r0:r1, :], self.src.ap()[r0:r1, :], writes=[self.pieces[i]])

        def issue(self):
            for i in self.order:
                r0 = i * self.rpp
                r1 = min(self.R, r0 + self.rpp)
                f.dma(pool, self.t.ap()[r0:r1, :], self.src.ap()[r0:r1, :], writes=[self.pieces[i]])

        def rows(self, r0, r1):
            return self.t.ap()[r0:r1, :], [self.pieces[i] for i in range(r0 // self.rpp, (r1 - 1) // self.rpp + 1)]

    s_poolw = Scr("poolw", poolw_d, 128)
    _ord = list(range(8, 20))
    for _h in range(4):
        _ord += [4 + _h, 44 + _h, 48 + _h, 20 + 2 * _h, 21 + 2 * _h]
    _ord += [0, 1, 2, 3] + list(range(28, 44))
    assert sorted(_ord) == list(range(52))
    s_win = Scr("win", win_d, 128, order=_ord)
    s_poolout = Scr("poolout", poolout_d, 1024)
    s_retout = Scr("retout", retout_d, 1024)
    s_wout = Scr("wout", wout_d, 1024)
    s_wq = Scr("wq", wq_d, 2048)
    s_keys = Scr("keys", keys_d, 128)
    s_u = Scr("u", u_d, 2048, defer=True)
    s_v = Scr("v", v_d, 2048, defer=True)
    pending_casts = []
    for _i in range(8):
        pending_casts += [(s_u, _i), (s_v, _i)]

    def cast_some(n=1):
        for _ in range(n):
            if pending_casts:
                scr_, i_ = pending_casts.pop(0)
                scr_.issue_piece(i_)

    PS = [f.psum(f"ps{i}", [128, 512], F32) for i in range(8)]

    def psbf(bank, ncols):
        return PS[bank].t[:, 0:ncols // 2].bitcast(BF16)

    iota_f = f.sbuf("iota_f", [128, 128], F32)
    f.op(pool, lambda e: e.iota(iota_f[:], pattern=[[1, 128]], base=0, channel_multiplier=0,
                                 allow_small_or_imprecise_dtypes=True), writes=[iota_f])
    pidx = f.sbuf("pidx", [128, 1], F32)
    f.op(pool, lambda e: e.iota(pidx[:], pattern=[[0, 1]], base=0, channel_multiplier=1,
                                 allow_small_or_imprecise_dtypes=True), writes=[pidx])
    rel = f.sbuf("rel", [128, 128], F32)
    f.op(pool, lambda e: e.iota(rel[:], pattern=[[1, 128]], base=0, channel_multiplier=-1,
                                 allow_small_or_imprecise_dtypes=True), writes=[rel])
    ident_bf = f.sbuf("ident_bf", [128, 128], BF16)
    ident_f = f.sbuf("ident_f", [128, 128], F32)
    ones_f = f.sbuf("ones_f", [128, 128], F32)
    f.op(dve, lambda e: e.tensor_scalar(out=ident_bf[:], in0=iota_f[:], scalar1=pidx[:, 0:1], scalar2=None,
                                        op0=ALU.is_equal), reads=[iota_f, pidx], writes=[ident_bf])
    f.op(dve, lambda e: e.tensor_scalar(out=ident_f[:], in0=iota_f[:], scalar1=pidx[:, 0:1], scalar2=None,
                                        op0=ALU.is_equal), reads=[iota_f, pidx], writes=[ident_f])
    f.op(dve, lambda e: e.memset(ones_f[:], 1.0), writes=[ones_f])
    epsb = f.sbuf("epsb", [128, 1], F32)
    f.op(dve, lambda e: e.memset(epsb[:], EPS), writes=[epsb])
    iota_bf = f.sbuf("iota_bf", [128, 128], BF16)
    f.op(dve, lambda e: e.tensor_copy(out=iota_bf[:], in_=iota_f[:]), reads=[iota_f], writes=[iota_bf])

    def load_const(name, src, shape, dt=F32):
        b = f.sbuf(name, shape, dt)
        f.dma(sp, b[:], src, writes=[b])
        return b

    adab = load_const("adab", adab_d.ap(), [128, 48])
    gmix = load_const("gmix", gmix_d.ap(), [128, 8])
    gffn = load_const("gffn", gffn_d.ap(), [128, 8])
    gfin = load_const("gfin", gfin_d.ap(), [128, 8])
    pscale = load_const("pscale", pscale_d.ap(), [128, 4])
    rng = load_const("rng", rng_d.ap(), [128, 8])
    dec_raw = load_const("dec_raw", bass.AP(decay_d, 0, [[0, 128], [1, 8]]), [128, 8])
    poolw = f.sbuf("poolw", [128, 512], BF16)
    _ap, _rd = s_poolw.rows(0, 128)
    f.dma(sp, poolw[:], _ap, reads=_rd, writes=[poolw])

    lg = f.sbuf("lg", [128, 8], F32)
    tmp8 = f.sbuf("tmp8", [128, 8], F32)
    f.op(act, lambda e: e.activation(out=tmp8[:], in_=dec_raw[:], func=AF.Exp, scale=-1.0), reads=[dec_raw], writes=[tmp8])
    f.op(act, lambda e: e.activation(out=lg[:], in_=tmp8[:], func=AF.Ln, bias=1.0), reads=[tmp8], writes=[lg])
    f.op(dve, lambda e: e.tensor_scalar(out=lg[:], in0=lg[:], scalar1=-1.0, scalar2=None, op0=ALU.mult), reads=[lg], writes=[lg])
    blk = f.sbuf("blk", [128, 8], F32)
    f.op(act, lambda e: e.activation(out=blk[:], in_=lg[:], func=AF.Exp, scale=128.0), reads=[lg], writes=[blk])
    pp = f.sbuf("pp", [128, 6], F32)
    f.op(dve, lambda e: e.tensor_scalar(out=pp[:, 0:1], in0=pidx[:], scalar1=-1.0, scalar2=127.0, op0=ALU.mult, op1=ALU.add), reads=[pidx], writes=[pp])
    f.op(dve, lambda e: e.tensor_copy(out=pp[:, 1:2], in_=pidx[:]), reads=[pidx], writes=[pp])
    f.op(dve, lambda e: e.tensor_scalar(out=pp[:, 2:3], in0=pidx[:], scalar1=-1.0, scalar2=255.0, op0=ALU.mult, op1=ALU.add), reads=[pidx], writes=[pp])
    f.op(dve, lambda e: e.tensor_scalar(out=pp[:, 3:4], in0=pidx[:], scalar1=-1.0, scalar2=127.0, op0=ALU.mult, op1=ALU.add), reads=[pidx], writes=[pp])
    f.op(dve, lambda e: e.tensor_copy(out=pp[:, 4:5], in_=pidx[:]), reads=[pidx], writes=[pp])
    f.op(dve, lambda e: e.tensor_scalar(out=pp[:, 5:6], in0=pidx[:], scalar1=1.0, scalar2=128.0, op0=ALU.mult, op1=ALU.add), reads=[pidx], writes=[pp])
    kdec = f.sbuf("kdec", [128, 8], F32)
    cdec = f.sbuf("cdec", [128, 16], F32)
    for h in range(4):
        f.op(act, lambda e, h=h: e.activation(out=kdec[:, h:h + 1], in_=pp[:, 0:1], func=AF.Exp, scale=lg[:, h:h + 1]), reads=[pp, lg], writes=[kdec])
        f.op(act, lambda e, h=h: e.activation(out=kdec[:, 4 + h:5 + h], in_=pp[:, 1:2], func=AF.Exp, scale=lg[:, 4 + h:5 + h]), reads=[pp, lg], writes=[kdec])
        for t in range(2):
            f.op(act, lambda e, h=h, t=t: e.activation(out=cdec[:, t * 4 + h:t * 4 + h + 1], in_=pp[:, 2 + t:3 + t], func=AF.Exp, scale=lg[:, h:h + 1]), reads=[pp, lg], writes=[cdec])
            f.op(act, lambda e, h=h, t=t: e.activation(out=cdec[:, 8 + t * 4 + h:8 + t * 4 + h + 1], in_=pp[:, 4 + t:5 + t], func=AF.Exp, scale=lg[:, 4 + h:5 + h]), reads=[pp, lg], writes=[cdec])
    f.op(dve, lambda e: e.tensor_scalar(out=kdec[:], in0=kdec[:], scalar1=K_SCALE, scalar2=None, op0=ALU.mult), reads=[kdec], writes=[kdec])
    f.op(dve, lambda e: e.tensor_scalar(out=cdec[:], in0=cdec[:], scalar1=K_SCALE, scalar2=None, op0=ALU.mult), reads=[cdec], writes=[cdec])
    def make_dec_tables():
        qdec = f.sbuf("qdec", [128, 8, 128], F32)
        dmask = f.sbuf("dmask", [128, 4, 128], F32)
        with f.scope():
            np1 = f.sbuf("np1", [128, 128], F32); nm = f.sbuf("nm", [128, 128], F32)
            f.op(dve, lambda e: e.tensor_scalar(out=np1[:], in0=iota_f[:], scalar1=1.0, scalar2=None, op0=ALU.add), reads=[iota_f], writes=[np1])
            f.op(dve, lambda e: e.tensor_scalar(out=nm[:], in0=iota_f[:], scalar1=-1.0, scalar2=128.0, op0=ALU.mult, op1=ALU.add), reads=[iota_f], writes=[nm])
            for h in range(4):
                f.op(act, lambda e, h=h: e.activation(out=qdec[:, h, :], in_=np1[:], func=AF.Exp, scale=lg[:, h:h + 1]), reads=[np1, lg], writes=[qdec])
                f.op(act, lambda e, h=h: e.activation(out=qdec[:, 4 + h, :], in_=nm[:], func=AF.Exp, scale=lg[:, 4 + h:5 + h]), reads=[nm, lg], writes=[qdec])
            relf = f.sbuf("relf", [128, 128], F32); relb = f.sbuf("relb", [128, 128], F32); mge = f.sbuf("mge", [128, 128], F32)
            ef = f.sbuf("ef", [128, 128], F32); eb = f.sbuf("eb", [128, 128], F32)
            f.op(dve, lambda e: e.tensor_scalar(out=relf[:], in0=rel[:], scalar1=0.0, scalar2=None, op0=ALU.max), reads=[rel], writes=[relf])
            f.op(dve, lambda e: e.tensor_scalar(out=relb[:], in0=rel[:], scalar1=-1.0, scalar2=0.0, op0=ALU.mult, op1=ALU.max), reads=[rel], writes=[relb])
            f.op(dve, lambda e: e.tensor_scalar(out=mge[:], in0=rel[:], scalar1=0.0, scalar2=K_SCALE, op0=ALU.is_ge, op1=ALU.mult), reads=[rel], writes=[mge])
            for h in range(4):
                f.op(act, lambda e, h=h: e.activation(out=ef[:], in_=relf[:], func=AF.Exp, scale=lg[:, h:h + 1]), reads=[relf, lg], writes=[ef])
                f.op(act, lambda e, h=h: e.activation(out=eb[:], in_=relb[:], func=AF.Exp, scale=lg[:, 4 + h:5 + h]), reads=[relb, lg], writes=[eb])
                f.op(dve, lambda e: e.tensor_tensor(out=ef[:], in0=ef[:], in1=eb[:], op=ALU.subtract), reads=[ef, eb], writes=[ef])
                f.op(dve, lambda e: e.tensor_tensor(out=ef[:], in0=ef[:], in1=mge[:], op=ALU.mult), reads=[ef, mge], writes=[ef])
                f.op(dve, lambda e, h=h: e.scalar_tensor_tensor(out=dmask[:, h, :], in0=eb[:], scalar=K_SCALE, in1=ef[:], op0=ALU.mult, op1=ALU.add), reads=[ef, eb], writes=[dmask])

        return qdec, dmask

    modv = f.sbuf("modv", [128, 48, 5], F32)
    with f.scope():
        ada_sb = f.sbuf("ada_sb", [128, 48, 1024], BF16)
        ada_g = [[Buf(ada_sb.t[:, g * 8:(g + 1) * 8, :], f"ada_g{g}_{k}") for k in range(3)] for g in range(6)]
        stg = [f.sbuf(f"stg{i}", [128, 8, 1024], F32) for i in range(2)]
        for g in range(6):
            st_ = stg[g % 2]
            f.dma(sp, st_[:], ada_d.ap()[g * 1024:(g + 1) * 1024, :].rearrange("(j p) c -> p j c", p=128), writes=[st_])
            for k_, (eng_, j0, j1) in enumerate(((act, 0, 3), (dve, 3, 6), (pool, 6, 8))):
                if eng_ is act:
                    f.op(eng_, lambda e, j0=j0, j1=j1, st_=st_, g=g, k_=k_: e.copy(out=ada_g[g][k_][:, j0:j1, :], in_=st_[:, j0:j1, :]), reads=[st_], writes=[ada_g[g][k_]])
                else:
                    f.op(eng_, lambda e, j0=j0, j1=j1, st_=st_, g=g, k_=k_: e.tensor_copy(out=ada_g[g][k_][:, j0:j1, :], in_=st_[:, j0:j1, :]), reads=[st_], writes=[ada_g[g][k_]])
        c_raw = f.sbuf("c_raw", [128, 40], F32)
        f.dma(sp, c_raw[:], cT_d.ap(), writes=[c_raw])
        c_bf = f.sbuf("c_bf", [128, 40], BF16)
        f.op(act, lambda e: e.activation(out=c_bf[:], in_=c_raw[:], func=AF.Silu), reads=[c_raw], writes=[c_bf])
        for oc in range(48):
            bank = PS[oc % 2]
            for kc in range(8):
                f.op(pe, lambda e, oc=oc, kc=kc, bank=bank: e.matmul(bank[:, 0:5], lhsT=ada_sb[:, oc, kc * 128:(kc + 1) * 128],
                                                                      rhs=c_bf[:, kc * 5:(kc + 1) * 5], start=(kc == 0), stop=(kc == 7)),
                     reads=ada_g[oc // 8] + [c_bf], writes=[bank])
            f.op(dve, lambda e, oc=oc, bank=bank: e.tensor_scalar(out=modv[:, oc, :], in0=bank[:, 0:5], scalar1=adab[:, oc:oc + 1], scalar2=None, op0=ALU.add),
                 reads=[adab], writes=[modv, bank])
    dbg("modv", modv, modv[:].rearrange("p a b -> p (a b)"), [128, 240])
    scl1 = f.sbuf("scl1", [128, 8, 5], F32); scl2 = f.sbuf("scl2", [128, 8, 5], F32)
    for (dst, gg, off) in ((scl1, gmix, 8), (scl2, gffn, 32)):
        f.op(dve, lambda e, dst=dst, off=off: e.tensor_scalar(out=dst[:], in0=modv[:, off:off + 8, :], scalar1=1.0, scalar2=None, op0=ALU.add), reads=[modv], writes=[dst])
        f.op(dve, lambda e, dst=dst, gg=gg: e.tensor_tensor(out=dst[:], in0=dst[:], in1=APX(gg[:], [[1, 8], [0, 5]]), op=ALU.mult), reads=[dst, gg], writes=[dst])

    NW = 4
    wbufs = [f.sbuf(f"wbuf{i}", [128, 1024], BF16) for i in range(NW)]
    wstate = {"i": 0, "n": NW}

    def load_w(scr, chunk, ncols=1024):
        b = wbufs[wstate["i"]]; wstate["i"] = (wstate["i"] + 1) % wstate["n"]
        _ap, _rd = scr.rows(chunk * 128, (chunk + 1) * 128)
        f.dma(sp, b[:, 0:ncols], _ap, reads=_rd, writes=[b])
        return b

    def rstd_from(ps_ap, out_ap, psbuf, outbuf):
        f.op(dve, lambda e: e.tensor_scalar(out=out_ap, in0=ps_ap, scalar1=1.0 / D, scalar2=EPS, op0=ALU.mult, op1=ALU.add), writes=[outbuf, psbuf])
        f.op(act, lambda e: e.activation(out=out_ap, in_=out_ap, func=AF.Sqrt), reads=[outbuf], writes=[outbuf])
        f.op(dve, lambda e: e.reciprocal(out=out_ap, in_=out_ap), reads=[outbuf], writes=[outbuf])

    for b in range(nb):
      with f.scope():
       U1 = f.sbuf("U1", [128, 16384], F32)
       U2 = f.sbuf("U2", [128, 16384], F32)
       u1b = U1.t[:, :].bitcast(BF16); u2b = U2.t[:, :].bitcast(BF16)
       hT = Buf(u1b[:, 0:16384].rearrange("p (k t) -> p k t", k=8), "hT")
       zT = Buf(u1b[:, 16384:32768].rearrange("p (k t) -> p k t", k=8), "zT")
       x1T = Buf(U1.t[:, :].rearrange("p (k t) -> p k t", k=8), "x1T")
       mergedT = Buf(u2b[:, 0:16384].rearrange("p (k t) -> p k t", k=8), "mergedT")
       mixedT = Buf(u2b[:, 16384:24576].rearrange("p (k t) -> p k t", k=4), "mixedT")
       sg = Buf(u2b[:, 24576:28672].rearrange("p (j t) -> p j t", j=2), "sg")
       r1 = [Buf(U2.t[:, 14336 + i * 512:14336 + (i + 1) * 512], f"r1_{i}") for i in range(2)]
       r2 = [Buf(U2.t[:, 15360 + i * 512:15360 + (i + 1) * 512], f"r2_{i}") for i in range(2)]
       WT = Buf(u2b.rearrange("p (t i) -> p t i", i=128), "WT")
       WTh = [Buf(u2b.rearrange("p (t i) -> p t i", i=128), "WTlo"), Buf(u2b.rearrange("p (t i) -> p t i", i=128), "WThi")]
       if True:
        _scS = f.scope(); _scS.__enter__()
        Sf0 = f.sbuf("Sf0", [128, 4, 256], F32); Sb0 = f.sbuf("Sb0", [128, 4, 256], F32)
        with f.scope():
            cx = f.sbuf("cx", [128, 8, LC], F32)
            f.dma(sp, cx[:], ctxT_d.ap()[b].rearrange("k p t -> p k t"), writes=[cx])
            csq = f.sbuf("csq", [128, 8, LC], F32)
            f.op(act, lambda e: e.activation(out=csq[:], in_=cx[:], func=AF.Square), reads=[cx], writes=[csq])
            for kc in range(8):
                f.op(pe, lambda e, kc=kc: e.matmul(PS[0][:, 0:LC], lhsT=ones_f[:], rhs=csq[:, kc, :], start=(kc == 0), stop=(kc == 7)), reads=[ones_f, csq], writes=[PS[0]])
            crstd = f.sbuf("crstd", [128, LC], F32)
            rstd_from(PS[0][:, 0:LC], crstd[:], PS[0], crstd)
            hcT = f.sbuf("hcT", [128, 8, LC], BF16)
            ctmp = f.sbuf("ctmp", [128, LC], F32)
            for kc in range(8):
                f.op(dve, lambda e, kc=kc: e.scalar_tensor_tensor(out=ctmp[:], in0=cx[:, kc, :], scalar=scl1[:, kc, 4:5], in1=crstd[:], op0=ALU.mult, op1=ALU.mult), reads=[cx, scl1, crstd], writes=[ctmp])
                f.op(dve, lambda e, kc=kc: e.tensor_scalar(out=hcT[:, kc, :], in0=ctmp[:], scalar1=modv[:, kc, 4:5], scalar2=None, op0=ALU.add), reads=[ctmp, modv], writes=[hcT])
            kcf = f.sbuf("kcf", [128, 2, 512], BF16); kcb = f.sbuf("kcb", [128, 2, 512], BF16); vc = f.sbuf("vc", [128, 2, 1024], BF16)
            for cc in range(12):
                wb = load_w(s_win, 8 + cc)
                for t in range(2):
                    if cc < 4:
                        bank = PS[1 + t]; col = cc * 128
                    else:
                        bank = PS[3 + 2 * t + (cc - 4) // 4]; col = ((cc - 4) % 4) * 128
                    for kc in range(8):
                        f.op(pe, lambda e, kc=kc, t=t, bank=bank, col=col, wb=wb: e.matmul(bank[:, col:col + 128], lhsT=hcT[:, kc, t * 128:(t + 1) * 128], rhs=wb[:, kc * 128:(kc + 1) * 128], start=(kc == 0), stop=(kc == 7)),
                             reads=[hcT, wb], writes=[bank])
            for t in range(2):
                for h in range(4):
                    f.op(act, lambda e, t=t, h=h: e.activation(out=kcf[:, t, h * 128:(h + 1) * 128], in_=PS[1 + t][:, h * 128:(h + 1) * 128], func=AF.Copy, scale=cdec[:, t * 4 + h:t * 4 + h + 1]), reads=[cdec], writes=[kcf, PS[1 + t]])
                    f.op(act, lambda e, t=t, h=h: e.activation(out=kcb[:, t, h * 128:(h + 1) * 128], in_=PS[1 + t][:, h * 128:(h + 1) * 128], func=AF.Copy, scale=cdec[:, 8 + t * 4 + h:8 + t * 4 + h + 1]), reads=[cdec], writes=[kcb, PS[1 + t]])
                for j in range(2):
                    f.op(dve, lambda e, t=t, j=j: e.tensor_copy(out=vc[:, t, j * 512:(j + 1) * 512], in_=PS[3 + 2 * t + j][:, :]), writes=[vc, PS[3 + 2 * t + j]])
            for h in range(4):
                for (kk, S0, bank) in ((kcf, Sf0, PS[0]), (kcb, Sb0, PS[7])):
                    for t in range(2):
                        f.op(pe, lambda e, kk=kk, t=t, h=h, bank=bank: e.matmul(bank[:, 0:256], lhsT=kk[:, t, h * 128:(h + 1) * 128], rhs=vc[:, t, h * 256:(h + 1) * 256], start=(t == 0), stop=(t == 1)), reads=[kk, vc], writes=[bank])
                    f.op(act, lambda e, S0=S0, h=h, bank=bank: e.copy(out=S0[:, h, :], in_=bank[:, 0:256]), writes=[S0, bank])
        if b == 0:
            dbg("Sf0", Sf0, Sf0[:].rearrange("p a b -> p (a b)"), [128, 1024])
            dbg("Sb0", Sb0, Sb0[:].rearrange("p a b -> p (a b)"), [128, 1024])

        with f.scope():
            with f.scope():
                rstd = f.sbuf("rstd", [128, L], F32)
                xc = [f.sbuf(f"xc{i}", [128, L], F32) for i in range(2)]
                xs = [f.sbuf(f"xs{i}", [128, L], F32) for i in range(2)]
                for kc in range(8):
                    xb_, sq_ = xc[kc % 2], xs[kc % 2]
                    f.dma(sp, xb_[:], xT_d.ap()[b, kc], writes=[xb_])
                    f.op(act, lambda e, xb_=xb_, sq_=sq_: e.activation(out=sq_[:], in_=xb_[:], func=AF.Square), reads=[xb_], writes=[sq_])
                    for tb in range(4):
                        f.op(pe, lambda e, tb=tb, sq_=sq_, kc=kc: e.matmul(PS[tb][:, :], lhsT=ones_f[:], rhs=sq_[:, tb * 512:(tb + 1) * 512], start=(kc == 0), stop=(kc == 7)), reads=[ones_f, sq_], writes=[PS[tb]])
                for tb in range(4):
                    rstd_from(PS[tb][:, :], rstd[:, tb * 512:(tb + 1) * 512], PS[tb], rstd)
                for kc in range(8):
                    xb_, sq_ = xc[kc % 2], xs[kc % 2]
                    f.dma(sp, xb_[:], xT_d.ap()[b, kc], writes=[xb_])
                    f.op(dve, lambda e, xb_=xb_, sq_=sq_, kc=kc: e.scalar_tensor_tensor(out=sq_[:], in0=xb_[:], scalar=scl1[:, kc, b:b + 1], in1=rstd[:], op0=ALU.mult, op1=ALU.mult), reads=[xb_, scl1, rstd], writes=[sq_])
                    f.op(act, lambda e, sq_=sq_, kc=kc: e.activation(out=hT[:, kc, :], in_=sq_[:], func=AF.Identity, bias=modv[:, kc, b:b + 1]), reads=[sq_, modv], writes=[hT])

            with f.scope():
                qdec, dmask = make_dec_tables()
                cst = [f.sbuf(f"cst{i}", [128, 512], F32) for i in range(1)]
                snt = [f.sbuf(f"snt{i}", [128, 512], F32) for i in range(1)]
                qT = f.sbuf("qT", [128, L], BF16); kT = f.sbuf("kT", [128, L], BF16)
                qfc = [f.sbuf(f"qfc{i}", [128, 128], BF16) for i in range(4)]
                qbc = [f.sbuf(f"qbc{i}", [128, 128], BF16) for i in range(4)]
                ktm = [f.sbuf(f"ktm{i}", [128, 128], BF16) for i in range(2)]
                V = f.sbuf("V", [128, 16, 256], BF16)
                Sf_bf = f.sbuf("Sf_bf", [128, 16, 256], BF16); Sb_bf = f.sbuf("Sb_bf", [128, 16, 256], BF16)
                Sf = f.sbuf("Sf", [128, 256], F32); Sb = f.sbuf("Sb", [128, 256], F32)
                sTm = [f.sbuf(f"sTm{i}", [128, 128], BF16) for i in range(4)]
                yh = [f.sbuf(f"yh{i}", [128, 256], BF16) for i in range(4)]
                st = [f.sbuf(f"st{i}", [128, 8], F32) for i in range(4)]
                junk = f.sbuf("junk", [128, 256], F32)
                for h in range(4):
                    for (which, dstT) in ((0, qT), (1, kT)):
                        w_n = load_w(s_win, 4 + which * 4 + h); w_s = load_w(s_win, 44 + which * 4 + h)
                        for tb in range(4):
                            bn, bs = PS[(tb % 2) * 2], PS[(tb % 2) * 2 + 1]
                            for (wt, bank) in ((w_n, bn), (w_s, bs)):
                                for kc in range(8):
                                    f.op(pe, lambda e, wt=wt, bank=bank, kc=kc, tb=tb: e.matmul(bank[:, :], lhsT=wt[:, kc * 128:(kc + 1) * 128], rhs=hT[:, kc, tb * 512:(tb + 1) * 512], start=(kc == 0), stop=(kc == 7)), reads=[wt, hT], writes=[bank])
                            a1, a2 = r1[tb % 2], r2[tb % 2]
                            sl = slice(tb * 512, (tb + 1) * 512)
                            cosT, sinT = cst[0], snt[0]
                            f.dma(sp, cosT[:], cos_d.ap()[:, sl], writes=[cosT]); f.dma(sp, sinT[:], sin_d.ap()[:, sl], writes=[sinT])
                            f.op(dve, lambda e, a1=a1, bn=bn, cosT=cosT: e.tensor_tensor(out=a1[:], in0=bn[:, :], in1=cosT[:], op=ALU.mult), reads=[cosT], writes=[a1, bn])
                            f.op(dve, lambda e, a2=a2, bs=bs, sinT=sinT: e.tensor_tensor(out=a2[:], in0=bs[:, :], in1=sinT[:], op=ALU.mult), reads=[sinT], writes=[a2, bs])
                            if which == 0:
                                f.op(pool, lambda e, a1=a1, a2=a2: e.tensor_tensor(out=a1[:], in0=a1[:], in1=a2[:], op=ALU.add), reads=[a1, a2], writes=[a1])
                                f.op(act, lambda e, a1=a1, sl=sl: e.copy(out=qT[:, sl], in_=a1[:]), reads=[a1], writes=[qT])
                            else:
                                f.op(pool, lambda e, a1=a1, a2=a2, sl=sl: e.tensor_tensor(out=kT[:, sl], in0=a1[:], in1=a2[:], op=ALU.add), reads=[a1, a2], writes=[kT])
                    cast_some(1)
                    wv0 = load_w(s_win, 12 + 2 * h); wv1 = load_w(s_win, 13 + 2 * h)
                    for c in range(16):
                        bank = PS[6 + (c % 2)]
                        for j, wv in enumerate((wv0, wv1)):
                            for kc in range(8):
                                f.op(pe, lambda e, c=c, bank=bank, j=j, wv=wv, kc=kc: e.matmul(bank[:, j * 128:(j + 1) * 128], lhsT=hT[:, kc, c * 128:(c + 1) * 128], rhs=wv[:, kc * 128:(kc + 1) * 128], start=(kc == 0), stop=(kc == 7)), reads=[hT, wv], writes=[bank])
                        f.op(act, lambda e, c=c, bank=bank: e.copy(out=V[:, c, :], in_=bank[:, 0:256]), writes=[V, bank])
                    cast_some(1)
                    for j in range(2):
                        wg = load_w(s_win, 20 + 2 * h + j)
                        for tb in range(4):
                            bank = PS[tb % 4]
                            for kc in range(8):
                                f.op(pe, lambda e, wg=wg, bank=bank, kc=kc, tb=tb: e.matmul(bank[:, :], lhsT=wg[:, kc * 128:(kc + 1) * 128], rhs=hT[:, kc, tb * 512:(tb + 1) * 512], start=(kc == 0), stop=(kc == 7)), reads=[wg, hT], writes=[bank])
                            f.op(act, lambda e, j=j, tb=tb, bank=bank: e.activation(out=sg[:, j, tb * 512:(tb + 1) * 512], in_=bank[:, :], func=AF.Silu), writes=[sg, bank])
                    cast_some(1)
                    for (dirn, S_, S0_, Sbf_, order, pb, kb, dcol) in (
                            (0, Sf, Sf0, Sf_bf, list(range(15)), 0, 4, h),
                            (1, Sb, Sb0, Sb_bf, list(range(15, 0, -1)), 2, 6, 4 + h)):
                        first = 0 if dirn == 0 else 15
                        f.op(dve, lambda e, S_=S_, S0_=S0_: e.tensor_copy(out=S_[:], in_=S0_[:, h, :]), reads=[S0_], writes=[S_])
                        f.op(act, lambda e, Sbf_=Sbf_, S0_=S0_, first=first: e.copy(out=Sbf_[:, first, :], in_=S0_[:, h, :]), reads=[S0_], writes=[Sbf_])

                        def tr(c, pb=pb, dcol=dcol):
                            kt_ = ktm[c % 2]
                            f.op(pe, lambda e: e.transpose(psbf(pb + c % 2, 128), kT[:, c * 128:(c + 1) * 128], ident_bf[:]), reads=[kT, ident_bf], writes=[PS[pb + c % 2]])
                            f.op(act, lambda e: e.activation(out=kt_[:], in_=psbf(pb + c % 2, 128), func=AF.Copy, scale=kdec[:, dcol:dcol + 1]), reads=[kdec], writes=[kt_, PS[pb + c % 2]])

                        tr(order[0])
                        for n_, c in enumerate(order):
                            bank = PS[kb + (c % 2)]
                            kt_ = ktm[c % 2]
                            f.op(pe, lambda e, c=c, bank=bank, kt_=kt_: e.matmul(bank[:, 0:256], lhsT=kt_[:], rhs=V[:, c, :], start=True, stop=True), reads=[kt_, V], writes=[bank])
                            if n_ + 1 < len(order):
                                tr(order[n_ + 1])
                            f.op(dve, lambda e, bank=bank, S_=S_, dcol=dcol: e.scalar_tensor_tensor(out=S_[:], in0=S_[:], scalar=blk[:, dcol:dcol + 1], in1=bank[:, 0:256], op0=ALU.mult, op1=ALU.add), reads=[S_, blk], writes=[S_, bank])
                            nxt_c = c + 1 if dirn == 0 else c - 1
                            f.op(act, lambda e, nxt_c=nxt_c, S_=S_, Sbf_=Sbf_: e.copy(out=Sbf_[:, nxt_c, :], in_=S_[:]), reads=[S_], writes=[Sbf_])
                    cast_some(1)
                    def chunk_gen(c, h=h):
                        i2 = c % 4
                        cs = slice(c * 128, (c + 1) * 128)
                        bank_ = PS[i2]
                        bs_ = bank_
                        ys_ = bank_[:, 128:384]
                        tr_ = bank_.t[:, 384:512].bitcast(BF16)
                        qf_, qb_ = qfc[i2], qbc[i2]
                        s_ = st[i2]
                        f.op(pe, lambda e: e.matmul(bs_[:, 0:128], lhsT=kT[:, cs], rhs=qT[:, cs], start=True, stop=True), reads=[kT, qT], writes=[bs_])
                        f.op(pool, lambda e: e.tensor_tensor(out=qf_[:], in0=qT[:, cs], in1=qdec[:, h, :], op=ALU.mult), reads=[qT, qdec], writes=[qf_])
                        f.op(pool, lambda e: e.tensor_tensor(out=qb_[:], in0=qT[:, cs], in1=qdec[:, 4 + h, :], op=ALU.mult), reads=[qT, qdec], writes=[qb_])
                        yield
                        f.op(dve, lambda e: e.tensor_tensor(out=sTm[i2][:], in0=bs_[:, 0:128], in1=dmask[:, h, :], op=ALU.mult), reads=[dmask], writes=[sTm[i2], bs_])
                        yield
                        f.op(pe, lambda e: e.matmul(ys_, lhsT=sTm[i2][:], rhs=V[:, c, :], start=True, stop=False), reads=[sTm[i2], V], writes=[bank_])
                        f.op(pe, lambda e: e.matmul(ys_, lhsT=qf_[:], rhs=Sf_bf[:, c, :], start=False, stop=False), reads=[qf_, Sf_bf], writes=[bank_])
                        f.op(pe, lambda e: e.matmul(ys_, lhsT=qb_[:], rhs=Sb_bf[:, c, :], start=False, stop=True), reads=[qb_, Sb_bf], writes=[bank_])
                        if debug and b == 0 and h == 0 and c == 3:
                            ydbg = f.sbuf("ydbg", [128, 256], F32)
                            f.op(act, lambda e: e.copy(out=ydbg[:], in_=ys_), writes=[ydbg, bank_])
                            dbg("y03", ydbg, ydbg[:], [128, 256])
                        yield
                        f.op(act, lambda e: e.activation(out=junk[:], in_=ys_, func=AF.Identity, accum_out=s_[:, 0:1]), writes=[junk, s_, bank_])
                        f.op(act, lambda e: e.activation(out=junk[:], in_=ys_, func=AF.Square, accum_out=s_[:, 1:2]), writes=[junk, s_, bank_])
                        yield
                        f.op(dve, lambda e: e.tensor_scalar(out=s_[:, 2:4], in0=s_[:, 0:2], scalar1=1.0 / 256, scalar2=None, op0=ALU.mult), reads=[s_], writes=[s_])
                        f.op(dve, lambda e: e.scalar_tensor_tensor(out=s_[:, 4:5], in0=s_[:, 2:3], scalar=s_[:, 2:3], in1=s_[:, 3:4], op0=ALU.mult, op1=ALU.subtract), reads=[s_], writes=[s_])
                        yield
                        f.op(act, lambda e: e.activation(out=s_[:, 7:8], in_=s_[:, 4:5], func=AF.Sqrt, scale=-1.0, bias=epsb[:, 0:1]), reads=[s_, epsb], writes=[s_])
                        yield
                        f.op(dve, lambda e: e.reciprocal(out=s_[:, 6:7], in_=s_[:, 7:8]), reads=[s_], writes=[s_])
                        f.op(dve, lambda e: e.tensor_scalar(out=yh[i2][:], in0=ys_, scalar1=s_[:, 2:3], scalar2=s_[:, 6:7], op0=ALU.subtract, op1=ALU.mult), reads=[s_], writes=[yh[i2], bank_])
                        yield
                        for j in range(2):
                            f.op(pe, lambda e, j=j: e.transpose(tr_[:, j * 128:(j + 1) * 128], yh[i2][:, j * 128:(j + 1) * 128], ident_bf[:]), reads=[yh[i2], ident_bf], writes=[bank_])
                        yield
                        for j in range(2):
                            f.op(dve, lambda e, j=j: e.scalar_tensor_tensor(out=zT[:, 2 * h + j, cs], in0=tr_[:, j * 128:(j + 1) * 128], scalar=rng[:, 2 * h + j:2 * h + j + 1], in1=sg[:, j, cs], op0=ALU.mult, op1=ALU.mult), reads=[rng, sg], writes=[zT, bank_])
                        yield

                    active = []
                    pending = list(range(16))
                    since = 99
                    while pending or active:
                        if pending and len(active) < 4 and since >= 2:
                            active.append(chunk_gen(pending.pop(0))); since = 0
                        for g_ in list(active):
                            try:
                                next(g_)
                            except StopIteration:
                                active.remove(g_)
                        since += 1
            if debug and b == 0:
                zdbg = f.sbuf("zdbg", [128, 512], F32)
                f.op(act, lambda e: e.copy(out=zdbg[:], in_=zT[:, 0, 0:512]), reads=[zT], writes=[zdbg])
                dbg("zT0", zdbg, zdbg[:], [128, 512])

            cast_some(99)
            with f.scope():
                PADW = L + 16
                pa = f.sbuf("pa", [128, PADW], F32); pb = f.sbuf("pb", [128, PADW], F32); pc = f.sbuf("pc", [128, PADW], F32)
                icnt = f.sbuf("icnt", [128, L], F32)
                dT = f.sbuf("dT", [128, L], BF16)
                for buf_ in (pa, pb, pc):
                    f.op(pool, lambda e, buf_=buf_: e.memset(buf_[:], 0.0), writes=[buf_])
                for gi in range(4):
                    wp = load_w(s_win, gi)
                    f.dma(sp, icnt[:], icnt_d.ap()[gi], writes=[icnt])
                    for tb in range(4):
                        bank = PS[tb]
                        for kc in range(8):
                            f.op(pe, lambda e, wp=wp, bank=bank, kc=kc, tb=tb: e.matmul(bank[:, :], lhsT=wp[:, kc * 128:(kc + 1) * 128], rhs=hT[:, kc, tb * 512:(tb + 1) * 512], start=(kc == 0), stop=(kc == 7)), reads=[wp, hT], writes=[bank])
                        f.op(act, lambda e, bank=bank, tb=tb: e.copy(out=pa[:, 8 + tb * 512:8 + (tb + 1) * 512], in_=bank[:, :]), writes=[pa, bank])
                    f.op(dve, lambda e: e.tensor_tensor(out=pb[:, 1:PADW], in0=pa[:, 0:PADW - 1], in1=pa[:, 1:PADW], op=ALU.add), reads=[pa], writes=[pb])
                    cur, oth = pb, pc
                    sh = 1
                    for lvl in range(gi):
                        f.op(dve, lambda e, cur=cur, oth=oth, sh=sh: e.tensor_tensor(out=oth[:, 8:8 + L + 1], in0=cur[:, 8 - sh:8 + L + 1 - sh], in1=cur[:, 8 + sh:8 + L + 1 + sh], op=ALU.add), reads=[cur], writes=[oth])
                        if lvl + 1 < gi:
                            f.op(dve, lambda e, cur=cur, oth=oth, sh=sh: e.tensor_tensor(out=oth[:, sh:8], in0=cur[:, 0:8 - sh], in1=cur[:, 2 * sh:8 + sh], op=ALU.add), reads=[cur], writes=[oth])
                            f.op(dve, lambda e, cur=cur, oth=oth, sh=sh: e.tensor_tensor(out=oth[:, 8 + L + 1:PADW - sh], in0=cur[:, 8 + L + 1 - sh:PADW - 2 * sh], in1=cur[:, 8 + L + 1 + sh:PADW], op=ALU.add), reads=[cur], writes=[oth])
                        cur, oth = oth, cur
                        sh *= 2
                    f.op(dve, lambda e, cur=cur, oth=oth: e.tensor_tensor(out=oth[:, 8:8 + L], in0=cur[:, 8:8 + L], in1=icnt[:], op=ALU.mult), reads=[cur, icnt], writes=[oth])
                    f.op(pool, lambda e, oth=oth: e.tensor_tensor(out=dT[:], in0=oth[:, 8:8 + L], in1=pa[:, 8:8 + L], op=ALU.subtract), reads=[oth, pa], writes=[dT])
                    f.op(pool, lambda e, oth=oth: e.memset(oth[:], 0.0), writes=[oth])
                    f.op(pool, lambda e, cur=cur: e.memset(cur[:], 0.0), writes=[cur])
                    for tb in range(4):
                        bank = PS[4 + (tb % 2)]
                        f.op(pe, lambda e, bank=bank, tb=tb, gi=gi: e.matmul(bank[:, :], lhsT=poolw[:, gi * 128:(gi + 1) * 128], rhs=dT[:, tb * 512:(tb + 1) * 512], start=True, stop=True), reads=[poolw, dT], writes=[bank])
                        f.op(act, lambda e, bank=bank, tb=tb, gi=gi: e.activation(out=mixedT[:, gi, tb * 512:(tb + 1) * 512], in_=bank[:, :], func=AF.Copy, scale=pscale[:, gi:gi + 1]), reads=[pscale], writes=[mixedT, bank])
            if debug and b == 0:
                mdbg = f.sbuf("mdbg", [128, 512], F32)
                f.op(act, lambda e: e.copy(out=mdbg[:], in_=mixedT[:, 2, 0:512]), reads=[mixedT], writes=[mdbg])
                dbg("mixedT2", mdbg, mdbg[:], [128, 512])

            with f.scope():
                s1 = [f.sbuf(f"s1_{i}", [128, 512], F32) for i in range(2)]
                s2 = [f.sbuf(f"s2_{i}", [128, 512], F32) for i in range(2)]
                for j in range(8):
                    wpo = load_w(s_poolout, j, 512); wro = load_w(s_retout, j); wgp = load_w(s_win, 28 + j); wgr = load_w(s_win, 36 + j)
                    for tb in range(4):
                        sl = slice(tb * 512, (tb + 1) * 512)
                        o = (tb % 2) * 4
                        bA, bB, bC, bD = PS[o], PS[o + 1], PS[o + 2], PS[o + 3]
                        for gi in range(4):
                            f.op(pe, lambda e, gi=gi, bA=bA, sl=sl, wpo=wpo: e.matmul(bA[:, :], lhsT=wpo[:, gi * 128:(gi + 1) * 128], rhs=mixedT[:, gi, sl], start=(gi == 0), stop=(gi == 3)), reads=[wpo, mixedT], writes=[bA])
                        for kc in range(8):
                            f.op(pe, lambda e, kc=kc, bB=bB, sl=sl, wro=wro: e.matmul(bB[:, :], lhsT=wro[:, kc * 128:(kc + 1) * 128], rhs=zT[:, kc, sl], start=(kc == 0), stop=(kc == 7)), reads=[wro, zT], writes=[bB])
                        for kc in range(8):
                            f.op(pe, lambda e, kc=kc, bC=bC, sl=sl, wgp=wgp: e.matmul(bC[:, :], lhsT=wgp[:, kc * 128:(kc + 1) * 128], rhs=hT[:, kc, sl], start=(kc == 0), stop=(kc == 7)), reads=[wgp, hT], writes=[bC])
                        for kc in range(8):
                            f.op(pe, lambda e, kc=kc, bD=bD, sl=sl, wgr=wgr: e.matmul(bD[:, :], lhsT=wgr[:, kc * 128:(kc + 1) * 128], rhs=hT[:, kc, sl], start=(kc == 0), stop=(kc == 7)), reads=[wgr, hT], writes=[bD])
                        a1, a2 = s1[tb % 2], s2[tb % 2]
                        f.op(act, lambda e, a1=a1, bC=bC: e.activation(out=a1[:], in_=bC[:, :], func=AF.Sigmoid), writes=[a1, bC])
                        f.op(act, lambda e, a2=a2, bD=bD: e.activation(out=a2[:], in_=bD[:, :], func=AF.Sigmoid), writes=[a2, bD])
                        f.op(dve, lambda e, a1=a1, bA=bA: e.tensor_tensor(out=a1[:], in0=bA[:, :], in1=a1[:], op=ALU.mult), reads=[a1], writes=[a1, bA])
                        f.op(dve, lambda e, a2=a2, bB=bB: e.tensor_tensor(out=a2[:], in0=bB[:, :], in1=a2[:], op=ALU.mult), reads=[a2], writes=[a2, bB])
                        f.op(pool, lambda e, a1=a1, a2=a2, j=j, sl=sl: e.tensor_tensor(out=mergedT[:, j, sl], in0=a1[:], in1=a2[:], op=ALU.add), reads=[a1, a2], writes=[mergedT])

        _scS.__exit__(None, None, None)
        with f.scope():
            with f.scope():
                xr = [f.sbuf(f"xr{i}", [128, L], F32) for i in range(2)]
                for j in range(8):
                    wo = load_w(s_wout, j)
                    xb_ = xr[j % 2]
                    f.dma(sp, xb_[:], xT_d.ap()[b, j], writes=[xb_])
                    for tb in range(4):
                        sl = slice(tb * 512, (tb + 1) * 512)
                        bank = PS[tb % 4]
                        for kc in range(8):
                            f.op(pe, lambda e, kc=kc, bank=bank, sl=sl, wo=wo: e.matmul(bank[:, :], lhsT=wo[:, kc * 128:(kc + 1) * 128], rhs=mergedT[:, kc, sl], start=(kc == 0), stop=(kc == 7)), reads=[wo, mergedT], writes=[bank])
                        f.op(dve, lambda e, bank=bank, sl=sl, j=j, xb_=xb_: e.scalar_tensor_tensor(out=x1T[:, j, sl], in0=bank[:, :], scalar=modv[:, 16 + j, b:b + 1], in1=xb_[:, sl], op0=ALU.mult, op1=ALU.add), reads=[modv, xb_], writes=[x1T, bank])
            if b == 0:
                dbg("x1T0", x1T, x1T[:, 0, :], [128, L])

            NT = TB // 128
            f.barrier()
            keysT = f.sbuf("keysT", [128, 2048], BF16)
            _ap, _rd = s_keys.rows(0, 128)
            f.dma(sp, keysT[:], _ap, reads=_rd, writes=[keysT])
            hfT = f.sbuf("hfT", [128, 8, TB], BF16)
            htmp = f.sbuf("htmp", [128, TB], F32)
            rstd2 = f.sbuf("rstd2", [128, TB], F32)
            qTc = [f.sbuf(f"qTc{i}", [128, TB], BF16) for i in range(2)]
            s_sb = [f.sbuf(f"s_sb{i}", [128, 128], F32) for i in range(2)]
            wk = [f.sbuf(f"wk{i}", [128, 256], F32) for i in range(2)]
            topvs = [f.sbuf(f"topv{i}", [128, 16, 16], F32) for i in range(NT)]
            topis = [f.sbuf(f"topi{i}", [128, 16, 16], U32) for i in range(NT)]
            topif = f.sbuf("topif", [128, 16, 16], F32)
            cand = f.sbuf("cand", [128, 8, 256], F32)
            best = f.sbuf("best", [128, 8, 16], F32); posu = f.sbuf("posu", [128, 8, 16], U32)
            bests = [f.sbuf(f"bests{i}", [128, 16], F32) for i in range(2)]
            posus = [f.sbuf(f"posus{i}", [128, 16], U32) for i in range(2)]
            xu = f.sbuf("xu", [128, 128], U32)
            xf = f.sbuf("xf", [128, 128], F32)
            IJG = [f.sbuf(f"IJG{i}", [128, 128], F32) for i in range(3)]
            IJGT = [f.sbuf(f"IJGT{i}", [128, 128], BF16) for i in range(3)]
            ebuf = xf; zs = f.sbuf("zs", [128, 16], F32)
            TG = 8
            Rg = [f.sbuf(f"Rg{i}", [128, TG, 64], BF16) for i in range(2)]
            Cg = [f.sbuf(f"Cg{i}", [128, TG, 128], BF16) for i in range(2)]
            iota_g = APX(iota_bf[:], [[0, TG], [1, 128]])
            gl = [f.sbuf(f"gl{i}", [128, TB], BF16) for i in range(3)]
            PT = [f.sbuf(f"PT{i}", [128, TB], BF16) for i in range(3)]
            GRP = 2
            ustr = [f.sbuf(f"ustr{i}", [128, GRP, 1024], BF16) for i in range(2)]
            vstr = [f.sbuf(f"vstr{i}", [128, GRP, 1024], BF16) for i in range(2)]
            iota16 = APX(iota_f[:, 0:16], [[0, 8], [0, 16], [1, 16]])
            ssv = [(s_sb[0][:], s_sb[0]), (s_sb[1][:], s_sb[1]),
                   (topif.t[:, 0:8, :].rearrange("p a b -> p (a b)"), topif), (topif.t[:, 8:16, :].rearrange("p a b -> p (a b)"), topif)]
            wkv = [Buf(wk[0].t[:, 0:128], "wkv0"), Buf(wk[0].t[:, 128:256], "wkv1"), Buf(wk[1].t[:, 0:128], "wkv2"), Buf(wk[1].t[:, 128:256], "wkv3")]
            topv_g = [[Buf(topvs[tt].t[:, g, :], f"tv{tt}_{g}") for g in range(16)] for tt in range(NT)]
            topi_g = [[Buf(topis[tt].t[:, g, :], f"ti{tt}_{g}") for g in range(16)] for tt in range(NT)]
            besth = [Buf(best.t[:, h, :], f"best{h}") for h in range(8)]
            posuh = [Buf(posu.t[:, h, :], f"posu{h}") for h in range(8)]

            eq = cand

            def v4(bf_):
                return bf_[:].rearrange("p h (x y) -> p h x y", x=16)

            hfT1 = f.sbuf("hfT1", [128, 8, TB], BF16)
            wstate["n"] = 2; wstate["i"] = 0

            def hfa(blk, kc):
                k3 = blk % 3
                if k3 == 0:
                    return hfT[:, kc, :]
                if k3 == 1:
                    return hfT1[:, kc, :]
                wb_ = wbufs[2 + kc // 4]
                return wb_[:, (kc % 4) * TB:(kc % 4 + 1) * TB]

            def hfb(blk):
                k3 = blk % 3
                return [hfT] if k3 == 0 else ([hfT1] if k3 == 1 else [wbufs[2], wbufs[3]])
            IJGTs = [[IJGT, [f.sbuf(f"IJGTb{i}", [128, 128], BF16) for i in range(3)]],
                     [[f.sbuf(f"IJGTc{i}", [128, 128], BF16) for i in range(3)], [f.sbuf(f"IJGTd{i}", [128, 128], BF16) for i in range(3)]]]
            NBLK = L // TB

            def topk_gen(blk_i):
                tsl = slice(blk_i * TB, (blk_i + 1) * TB)
                for kc in range(8):
                    f.op(act, lambda e, kc=kc: e.activation(out=htmp[:], in_=x1T[:, kc, tsl], func=AF.Square), reads=[x1T], writes=[htmp])
                    f.op(pe, lambda e, kc=kc: e.matmul(PS[6][:, 0:TB], lhsT=ones_f[:], rhs=htmp[:], start=(kc == 0), stop=(kc == 7)), reads=[ones_f, htmp], writes=[PS[6]])
                rstd_from(PS[6][:, 0:TB], rstd2[:], PS[6], rstd2)
                yield
                for kc in range(8):
                    f.op(dve, lambda e, kc=kc: e.scalar_tensor_tensor(out=htmp[:], in0=x1T[:, kc, tsl], scalar=scl2[:, kc, b:b + 1], in1=rstd2[:], op0=ALU.mult, op1=ALU.mult), reads=[x1T, scl2, rstd2], writes=[htmp])
                    f.op(act, lambda e, kc=kc: e.activation(out=hfa(blk_i, kc), in_=htmp[:], func=AF.Identity, bias=modv[:, 24 + kc, b:b + 1]), reads=[htmp, modv], writes=hfb(blk_i))
                    if kc % 2 == 1:
                        yield
                if debug and b == 0 and blk_i == 0:
                    hdbg = f.sbuf("hdbg", [128, TB], F32)
                    f.op(act, lambda e: e.copy(out=hdbg[:], in_=hfa(blk_i, 0)), reads=hfb(blk_i), writes=[hdbg])
                    dbg("hfT0", hdbg, hdbg[:], [128, TB])
                def q_part(g0):
                    for g in (g0, g0 + 1):
                        wq_ = load_w(s_wq, g)
                        qb_ = qTc[g % 2]
                        for kc in range(8):
                            f.op(pe, lambda e, kc=kc, wq_=wq_: e.matmul(PS[6][:, 0:TB], lhsT=wq_[:, kc * 128:(kc + 1) * 128], rhs=hfa(blk_i, kc), start=(kc == 0), stop=(kc == 7)), reads=[wq_] + hfb(blk_i), writes=[PS[6]])
                        f.op(act, lambda e, qb_=qb_: e.copy(out=qb_[:], in_=PS[6][:, 0:TB]), writes=[qb_, PS[6]])

                q_part(0)
                yield
                for g0 in range(0, 16, 2):
                    chains = []
                    for g in (g0, g0 + 1):
                        qb_ = qTc[g % 2]
                        for tt in range(NT):
                            col = ((g % 2) * NT + tt) * 128
                            f.op(pe, lambda e, tt=tt, qb_=qb_, g=g, col=col: e.matmul(PS[7][:, col:col + 128], lhsT=qb_[:, tt * 128:(tt + 1) * 128], rhs=keysT[:, g * 128:(g + 1) * 128], start=True, stop=True), reads=[qb_, keysT], writes=[PS[7]])
                    for g in (g0, g0 + 1):
                        for tt in range(NT):
                            ci_ = (g % 2) * NT + tt
                            col = ci_ * 128
                            ss_ap, ss_b = ssv[ci_]
                            f.op(act, lambda e, ss_ap=ss_ap, col=col: e.copy(out=ss_ap, in_=PS[7][:, col:col + 128]), writes=[ss_b, PS[7]])
                            chains.append((g, tt, ss_ap, ss_b, wkv[ci_]))
                    yield
                    for step in range(5):
                        for (g, tt, ss_ap, ss_b, w_) in chains:
                            TV, TI = topv_g[tt][g], topi_g[tt][g]
                            if step == 0:
                                f.op(dve, lambda e, ss_ap=ss_ap, TV=TV: e.max(out=TV[:, 0:8], in_=ss_ap), reads=[ss_b], writes=[TV])
                            elif step == 1:
                                f.op(dve, lambda e, ss_ap=ss_ap, TV=TV, TI=TI: e.max_index(out=TI[:, 0:8], in_max=TV[:, 0:8], in_values=ss_ap), reads=[ss_b, TV], writes=[TI])
                            elif step == 2:
                                f.op(dve, lambda e, ss_ap=ss_ap, TV=TV, w_=w_: e.match_replace(out=w_[:], in_to_replace=TV[:, 0:8], in_values=ss_ap, imm_value=NEG), reads=[ss_b, TV], writes=[w_])
                            elif step == 3:
                                f.op(dve, lambda e, TV=TV, w_=w_: e.max(out=TV[:, 8:16], in_=w_[:]), reads=[w_], writes=[TV])
                            else:
                                f.op(dve, lambda e, TV=TV, TI=TI, w_=w_: e.max_index(out=TI[:, 8:16], in_max=TV[:, 8:16], in_values=w_[:]), reads=[w_, TV], writes=[TI])
                        if step in (1, 3):
                            yield
                    if g0 + 2 < 16:
                        q_part(g0 + 2)
                    yield
                for tt in range(NT):
                    TV, TI = topvs[tt], topis[tt]
                    f.op(dve, lambda e, TI=TI: e.tensor_copy(out=topif[:], in_=TI[:]), reads=topi_g[tt], writes=[topif])
                    f.op(dve, lambda e, TV=TV: e.tensor_tensor(out=cand[:].rearrange("p h (x y) -> p h x y", x=16), in0=APX(TV[:], [[32, 8], [1, 16], [0, 16]]), in1=APX(TV[:, 1, :], [[32, 8], [0, 16], [1, 16]]), op=ALU.add), reads=topv_g[tt], writes=[cand])
                    yield
                    for hp in range(4):
                        for step in range(5):
                            for h in (2 * hp, 2 * hp + 1):
                                w_ = wk[h % 2]
                                bh = besth[h]; ph = posuh[h]
                                if step == 0:
                                    f.op(dve, lambda e, h=h, bh=bh: e.max(out=bh[:, 0:8], in_=cand[:, h, :]), reads=[cand], writes=[bh])
                                elif step == 1:
                                    f.op(dve, lambda e, h=h, bh=bh, ph=ph: e.max_index(out=ph[:, 0:8], in_max=bh[:, 0:8], in_values=cand[:, h, :]), reads=[cand, bh], writes=[ph])
                                elif step == 2:
                                    f.op(dve, lambda e, h=h, w_=w_, bh=bh: e.match_replace(out=w_[:], in_to_replace=bh[:, 0:8], in_values=cand[:, h, :], imm_value=NEG), reads=[cand, bh], writes=[w_])
                                elif step == 3:
                                    f.op(dve, lambda e, h=h, w_=w_, bh=bh: e.max(out=bh[:, 8:16], in_=w_[:]), reads=[w_], writes=[bh])
                                else:
                                    f.op(dve, lambda e, h=h, w_=w_, bh=bh, ph=ph: e.max_index(out=ph[:, 8:16], in_max=bh[:, 8:16], in_values=w_[:]), reads=[w_, bh], writes=[ph])
                        yield
                    pflat = posu[:].rearrange("p h k -> p (h k)")
                    for (src_, off, dst) in ((xf, 0, IJG[0]), (xf, 1, IJG[1])):
                        if off == 0:
                            f.op(dve, lambda e: e.tensor_scalar(out=xu[:], in0=pflat, scalar1=4, scalar2=None, op0=ALU.logical_shift_right), reads=posuh, writes=[xu])
                        else:
                            f.op(dve, lambda e: e.tensor_scalar(out=xu[:], in0=pflat, scalar1=15, scalar2=None, op0=ALU.bitwise_and), reads=posuh, writes=[xu])
                        f.op(dve, lambda e: e.tensor_copy(out=xf[:], in_=xu[:]), reads=[xu], writes=[xf])
                        f.op(dve, lambda e, src_=src_: e.tensor_tensor(out=v4(eq), in0=APX(src_[:], [[16, 8], [1, 16], [0, 16]]), in1=iota16, op=ALU.is_equal), reads=[src_, iota_f], writes=[eq])
                        f.op(dve, lambda e, off=off: e.tensor_tensor(out=v4(eq), in0=v4(eq), in1=APX(topif[:, off, :], [[32, 8], [0, 16], [1, 16]]), op=ALU.mult), reads=[eq, topif], writes=[eq])
                        f.op(dve, lambda e, dst=dst: e.tensor_reduce(out=dst[:].rearrange("p (h k) -> p h k", h=8), in_=v4(eq), axis=AX.X, op=ALU.add), reads=[eq], writes=[dst])
                        yield
                    e3 = ebuf[:].rearrange("p (h k) -> p h k", h=8)
                    f.op(dve, lambda e: e.tensor_tensor(out=e3, in0=best[:], in1=APX(best[:], [[16, 8], [0, 16]]), op=ALU.subtract), reads=besth, writes=[ebuf])
                    f.op(act, lambda e: e.activation(out=ebuf[:], in_=ebuf[:], func=AF.Exp), reads=[ebuf], writes=[ebuf])
                    f.op(dve, lambda e: e.tensor_reduce(out=zs[:, 0:8], in_=e3, axis=AX.X, op=ALU.add), reads=[ebuf], writes=[zs])
                    f.op(dve, lambda e: e.reciprocal(out=zs[:, 8:16], in_=zs[:, 0:8]), reads=[zs], writes=[zs])
                    f.op(dve, lambda e: e.tensor_tensor(out=IJG[2][:].rearrange("p (h k) -> p h k", h=8), in0=e3, in1=APX(zs[:, 8:16], [[1, 8], [0, 16]]), op=ALU.mult), reads=[ebuf, zs], writes=[IJG[2]])
                    yield
                    if debug and b == 0 and blk_i == 0 and tt == 0:
                        for i_, nm_ in enumerate(("I", "J", "G")):
                            dbg("peer" + nm_, IJG[i_], IJG[i_][:], [128, 128])
                    for i_ in range(3):
                        f.op(pe, lambda e, i_=i_: e.transpose(PS[6][:, 0:128], IJG[i_][:], ident_f[:]), reads=[IJG[i_], ident_f], writes=[PS[6]])
                        f.op(act, lambda e, i_=i_, tt=tt: e.copy(out=IJGTs[blk_i % 2][tt][i_][:], in_=PS[6][:, 0:128]), writes=[IJGTs[blk_i % 2][tt][i_], PS[6]])
                    yield

            def build_W(blk_i, half):
                i0 = half * 64
                wbuf_ = WTh[half]
                iota_h = APX(iota_bf[:, i0:i0 + 64], [[0, TG], [1, 64]])

                def pe_part(tt, tg, R_, C_):
                    bank = PS[6] if tg % 2 == 0 else PS[7]
                    for tl in range(TG):
                        f.op(pe, lambda e, bank=bank, tl=tl: e.matmul(bank[:, tl * 64:(tl + 1) * 64], lhsT=C_[:, tl, :], rhs=R_[:, tl, :], start=True, stop=True), reads=[R_, C_], writes=[bank])
                    tb0 = tt * 128 + tg * TG
                    f.op(act, lambda e, bank=bank, tb0=tb0: e.copy(out=WT[:, tb0:tb0 + TG, i0:i0 + 64], in_=bank[:, :].rearrange("p (t i) -> p t i", i=64)), writes=[wbuf_, bank])

                prev = None
                n_ = 0
                for tt in range(NT):
                    IJGT_ = IJGTs[blk_i % 2][tt]
                    for tg in range(128 // TG):
                        if prev is not None:
                            pe_part(*prev)
                        R_, C_ = Rg[n_ % 2], Cg[n_ % 2]
                        n_ += 1
                        ts0 = tg * TG
                        f.op(dve, lambda e, ts0=ts0, R_=R_: e.tensor_tensor(out=R_[:], in0=iota_h, in1=APX(IJGT_[0][:, ts0:ts0 + TG], [[1, TG], [0, 64]]), op=ALU.is_equal), reads=[iota_bf, IJGT_[0]], writes=[R_])
                        f.op(pool, lambda e, ts0=ts0, R_=R_: e.tensor_tensor(out=R_[:], in0=R_[:], in1=APX(IJGT_[2][:, ts0:ts0 + TG], [[1, TG], [0, 64]]), op=ALU.mult), reads=[R_, IJGT_[2]], writes=[R_])
                        f.op(dve, lambda e, ts0=ts0, C_=C_: e.tensor_tensor(out=C_[:], in0=iota_g, in1=APX(IJGT_[1][:, ts0:ts0 + TG], [[1, TG], [0, 128]]), op=ALU.is_equal), reads=[iota_bf, IJGT_[1]], writes=[C_])
                        prev = (tt, tg, R_, C_)
                        yield
                pe_part(*prev)
                yield

            bgs = {"gen": None, "blk": -1, "next": 0, "done": set()}

            def bg_step():
                if bgs["gen"] is None:
                    if bgs["next"] >= NBLK:
                        return False
                    bgs["blk"] = bgs["next"]; bgs["next"] += 1
                    bgs["gen"] = topk_gen(bgs["blk"])
                try:
                    next(bgs["gen"])
                except StopIteration:
                    bgs["gen"] = None; bgs["done"].add(bgs["blk"])
                return True

            def bg_finish(blk):
                while blk < NBLK and blk not in bgs["done"]:
                    bg_step()

            def experts(blk_i, genB, genA_fn):
                strm = {}

                def load_grp(grp):
                    ub, vb = ustr[grp % 2], vstr[grp % 2]
                    _ap, _rd = s_u.rows(grp * GRP * 128, (grp + 1) * GRP * 128)
                    f.dma(sp, ub[:], _ap.rearrange("(c p) x -> p c x", p=128), reads=_rd, writes=[ub])
                    _ap, _rd = s_v.rows(grp * GRP * 128, (grp + 1) * GRP * 128)
                    f.dma(sp, vb[:], _ap.rearrange("(c p) x -> p c x", p=128), reads=_rd, writes=[vb])
                    strm[grp] = (ub, vb)

                def emit_A(i):
                    grp, ci = divmod(i, GRP)
                    if grp not in strm:
                        load_grp(grp)
                    ub = strm[grp][0]
                    bankA = PS[4 + (i % 2)]
                    g_, p_ = gl[i % 3], PT[i % 3]
                    for kc in range(8):
                        f.op(pe, lambda e, kc=kc, ub=ub, ci=ci, bankA=bankA: e.matmul(bankA[:, 0:TB], lhsT=ub[:, ci, kc * 128:(kc + 1) * 128], rhs=hfa(blk_i, kc), start=(kc == 0), stop=(kc == 7)), reads=[ub] + hfb(blk_i), writes=[bankA])
                    f.op(act, lambda e, g_=g_, bankA=bankA: e.activation(out=g_[:], in_=bankA[:, 0:TB], func=AF.Gelu), writes=[g_, bankA])
                    f.op(pool, lambda e, g_=g_, p_=p_, i=i: e.tensor_tensor(out=p_[:], in0=g_[:], in1=WT[:, :, i], op=ALU.mult), reads=[g_, WTh[i // 64]], writes=[p_])

                def emit_out(i):
                    grp, ci = divmod(i, GRP)
                    vb = strm[grp][1]
                    p_ = PT[i % 3]
                    for dc in range(8):
                        bo = PS[dc // 2]
                        f.op(pe, lambda e, dc=dc, bo=bo, vb=vb, ci=ci, p_=p_, i=i: e.matmul(bo[:, (dc % 2) * TB:(dc % 2 + 1) * TB], lhsT=vb[:, ci, dc * 128:(dc + 1) * 128], rhs=p_[:], start=(i == 0), stop=(i == 127)), reads=[vb, p_], writes=[bo])

                emit_A(0)
                genA = None
                for i in range(128):
                    if i + 1 < 128:
                        emit_A(i + 1)
                    emit_out(i)
                    if i < 64:
                        if genB is not None:
                            if i % 2 == 0 or i == 1:
                                next(genB, None)
                            if i == 62:
                                for _ in genB:
                                    pass
                        if i % 2 == 1 and (blk_i + 1) not in bgs["done"] and blk_i + 1 < NBLK:
                            bg_step()
                        if i == 61:
                            bg_finish(blk_i + 1)
                    else:
                        if i == 64 and genA_fn is not None:
                            genA = genA_fn()
                        if genA is not None and (i % 2 == 0 or i == 65):
                            next(genA, None)
                        if i % 2 == 1:
                            cur = bgs["blk"] if bgs["gen"] is not None else bgs["next"]
                            if cur == blk_i + 2 and cur < NBLK:
                                bg_step()
                if genA is not None:
                    for _ in genA:
                        pass
                tsl = slice(blk_i * TB, (blk_i + 1) * TB)
                for dc in range(8):
                    bo = PS[dc // 2]
                    f.op(dve, lambda e, dc=dc, bo=bo: e.scalar_tensor_tensor(out=x1T[:, dc, tsl], in0=bo[:, (dc % 2) * TB:(dc % 2 + 1) * TB], scalar=modv[:, 40 + dc, b:b + 1], in1=x1T[:, dc, tsl], op0=ALU.mult, op1=ALU.add), reads=[modv, x1T], writes=[x1T, bo])

            bg_finish(0)
            for _ in build_W(0, 0):
                pass
            for blk_i in range(NBLK):
                nxt = blk_i + 1 < NBLK
                if os.environ.get("KDBG_SERIAL"):
                    if blk_i > 0:
                        bg_finish(blk_i)
                        for _ in build_W(blk_i, 0):
                            pass
                    for _ in build_W(blk_i, 1):
                        pass
                    experts(blk_i, None, None)
                    continue
                experts(blk_i, build_W(blk_i, 1), (lambda bi=blk_i + 1: build_W(bi, 0)) if nxt else None)
            wstate["n"] = NW
            if b == 0:
                dbg("x2T0", x1T, x1T[:, 0, :], [128, L])
            f.barrier()
            if True:
                xs = [Buf(U2.t[:, i * L:(i + 1) * L], f"xo{i}") for i in range(2)]
                rstd3 = Buf(U2.t[:, 2 * L:3 * L], "rstd3")
                for kc in range(8):
                    sq_ = xs[kc % 2]
                    f.op(act, lambda e, sq_=sq_, kc=kc: e.activation(out=sq_[:], in_=x1T[:, kc, :], func=AF.Square), reads=[x1T], writes=[sq_])
                    for tb in range(4):
                        f.op(pe, lambda e, tb=tb, sq_=sq_, kc=kc: e.matmul(PS[tb][:, :], lhsT=ones_f[:], rhs=sq_[:, tb * 512:(tb + 1) * 512], start=(kc == 0), stop=(kc == 7)), reads=[ones_f, sq_], writes=[PS[tb]])
                for tb in range(4):
                    rstd_from(PS[tb][:, :], rstd3[:, tb * 512:(tb + 1) * 512], PS[tb], rstd3)
                for kc in range(8):
                    o_ = xs[kc % 2]
                    f.op(dve, lambda e, o_=o_, kc=kc: e.scalar_tensor_tensor(out=o_[:], in0=x1T[:, kc, :], scalar=gfin[:, kc:kc + 1], in1=rstd3[:], op0=ALU.mult, op1=ALU.mult), reads=[x1T, gfin, rstd3], writes=[o_])
                    f.dma(sp, out_d.ap()[b, kc], o_[:], reads=[o_])
    f.barrier()
    stats = {e.name: (e.n_instr, e.n_wait) for e in f.engs}
    f.root.close()
    return nc, stats


def _chunked(W):
    K, N = W.shape
    return np.ascontiguousarray(W.reshape(K // 128, 128, N // 128, 128).transpose(2, 1, 0, 3)).reshape(N, K)


def _vec(v):
    n = v.size // 128
    return np.ascontiguousarray(v.reshape(n, 128).T)


def _consts():
    t = np.arange(L)
    row = (t // 64).astype(np.float32); col = (t % 64).astype(np.float32)
    inv = (np.float32(10000.0) ** (-np.arange(32, dtype=np.float32) / np.float32(32))).astype(np.float32)
    ang = np.concatenate([row[:, None] * inv, col[:, None] * inv], axis=-1).astype(np.float32)
    cos = np.cos(ang).astype(np.float32).T; sin = np.sin(ang).astype(np.float32).T
    ropecos = np.concatenate([cos, cos], 0); ropesin = np.concatenate([-sin, sin], 0)
    ic = np.zeros((4, 128, L), np.float32)
    for gi, w in enumerate((2, 4, 8, 16)):
        lo = np.clip(t - w // 2, 0, L); hi = np.clip(t + w - w // 2, 0, L)
        ic[gi] = (1.0 / (hi - lo).astype(np.float32))[None, :]
    return np.ascontiguousarray(ropecos), np.ascontiguousarray(ropesin), ic


def prepare_inputs(x, c, ctx, c_ctx, ada_w, ada_b, norm_mix_g, norm_ffn_g, w_in, pool_w, pool_scale,
                   pool_out, ret_decay, ret_norm_g, ret_out, w_out, peer_wq, peer_keys, peer_u, peer_v, final_g):
    f32 = lambda a: np.asarray(a, dtype=np.float32)
    x, c, ctx, c_ctx = f32(x), f32(c), f32(ctx), f32(c_ctx)
    w = f32(w_in)[0]
    qk = w[:, 512:1536].reshape(1024, 8, 2, 64)[:, :, ::-1, :].reshape(1024, 1024)
    w_all = np.concatenate([w, qk], axis=1)
    ropecos, ropesin, ic = _consts()
    u = f32(peer_u)[0]
    shared = {
        "ada_b": _vec(f32(ada_b)[0]), "g_mix": _vec(f32(norm_mix_g)[0]), "g_ffn": _vec(f32(norm_ffn_g)[0]),
        "g_fin": _vec(f32(final_g)), "pool_scale": _vec(f32(pool_scale)[0]), "ret_norm_g": _vec(f32(ret_norm_g)[0]),
        "ret_decay": np.ascontiguousarray(f32(ret_decay)[0].reshape(1, 8)),
        "ropecos": ropecos, "ropesin": ropesin, "invcnt": ic,
        "ada_w_r": _chunked(f32(ada_w)[0]), "w_in_r": _chunked(w_all),
        "pool_w": np.ascontiguousarray(f32(pool_w)[0].transpose(1, 0, 2)).reshape(128, 512),
        "pool_out_r": _chunked(f32(pool_out)[0]), "ret_out_r": _chunked(f32(ret_out)[0]),
        "w_out_r": _chunked(f32(w_out)[0]), "wq_r": _chunked(f32(peer_wq)[0]),
        "keysT": np.ascontiguousarray(f32(peer_keys)[0].reshape(16, 128, 128).transpose(2, 0, 1)).reshape(128, 2048),
        "uT_r": np.ascontiguousarray(u.reshape(128, 128, 8, 128).transpose(0, 3, 2, 1)).reshape(128 * 128, 1024),
        "v": np.ascontiguousarray(f32(peer_v)[0]),
    }
    in_maps = []
    for core in range(NCORES):
        bs = slice(core * BPC, (core + 1) * BPC)
        cc = np.concatenate([c[bs], c_ctx[None, :]], 0)
        m = dict(shared)
        m["xT"] = np.ascontiguousarray(x[bs].transpose(0, 2, 1)).reshape(BPC, 8, 128, L)
        m["ctxT"] = np.ascontiguousarray(ctx[bs].transpose(0, 2, 1)).reshape(BPC, 8, 128, LC)
        m["cT"] = np.ascontiguousarray(cc.reshape(5, 8, 128).transpose(2, 1, 0)).reshape(128, 40)
        in_maps.append(m)
    return in_maps


def kernel(**inputs):
    in_maps = prepare_inputs(**inputs)
    nc, _ = build_program()
    res = run_bass_kernel_spmd(nc, in_maps, core_ids=list(range(NCORES)))
    out = np.empty((NCORES * BPC, L, D), np.float32)
    for core in range(NCORES):
        o = np.asarray(res.results[core]["outT"]).reshape(BPC, D, L)
        out[core * BPC:(core + 1) * BPC] = o.transpose(0, 2, 1)
    return out
```

```python
import os, contextlib
import numpy as np
import concourse.bass as bass
import concourse.mybir as mybir
from concourse.bass_utils import run_bass_kernel_spmd

F32 = mybir.dt.float32; BF16 = mybir.dt.bfloat16; U32 = mybir.dt.uint32
ALU = mybir.AluOpType; AF = mybir.ActivationFunctionType; AX = mybir.AxisListType

NCORES = 8
D = 1024; L = 2048; BPC = 4; LC = 256
EPS = 1e-6
NEG = -1e30
TB = 256
K_SCALE = 128 ** -0.5


class Buf:
    __slots__ = ("t", "lw", "rd", "name")

    def __init__(self, t, name=""):
        self.t = t; self.lw = None; self.rd = {}; self.name = name

    def __getitem__(self, k):
        return self.t[k]


class EngW:
    SEM_LIMIT = 30000

    def __init__(self, fw, eng, name, is_pe=False):
        self.fw = fw; self.eng = eng; self.name = name; self.is_pe = is_pe
        self.sem = fw.new_sem(name + "_c0"); self.count = 0; self.nsem = 1
        self.waited = {}
        self.n_instr = 0; self.n_wait = 0

    def wait_tok(self, tok):
        sem, val = tok
        if self.waited.get(id(sem), 0) < val:
            self.eng.wait_ge(sem, val); self.waited[id(sem)] = val; self.n_wait += 1

    def bump(self, ins):
        if self.count >= self.SEM_LIMIT:
            self.sem = self.fw.new_sem(f"{self.name}_c{self.nsem}"); self.nsem += 1; self.count = 0
        self.count += 1; self.n_instr += 1
        ins.then_inc(self.sem, 1)
        return (self.sem, self.count)


class FW:
    def __init__(self, nc):
        self.nc = nc; self.root = contextlib.ExitStack(); self.es = self.root
        self.pe = EngW(self, nc.tensor, "pe", True)
        self.act = EngW(self, nc.scalar, "act")
        self.dve = EngW(self, nc.vector, "dve")
        self.pool = EngW(self, nc.gpsimd, "pool")
        self.sp = EngW(self, nc.sync, "sp")
        self.engs = [self.pe, self.act, self.dve, self.pool, self.sp]
        self.dma_pools = {"sp": [[self.new_sem(f"dmas{i}"), 0] for i in range(32)],
                          "pool": [[self.new_sem(f"dmap{i}"), 0] for i in range(16)]}
        self.dma_rr = {"sp": 0, "pool": 0}
        self.uid = 0

    def new_sem(self, name):
        return self.root.enter_context(self.nc.semaphore(name))

    def sbuf(self, name, shape, dt):
        self.uid += 1
        return Buf(self.es.enter_context(self.nc.sbuf_tensor(f"{name}_{self.uid}", list(shape), dt)), name)

    def psum(self, name, shape, dt=F32):
        return Buf(self.root.enter_context(self.nc.psum_tensor(name, list(shape), dt)), name)

    @contextlib.contextmanager
    def scope(self):
        old = self.es
        self.es = contextlib.ExitStack()
        try:
            yield
        finally:
            self.barrier()
            self.es.close()
            self.es = old

    def barrier(self):
        toks = [(e.sem, e.count) for e in self.engs if e.count > 0]
        toks += [(s, c) for s, c in self.dma_pools["sp"] if c > 0]
        for e in self.engs:
            for tok in toks:
                if tok[0] is e.sem:
                    continue
                e.wait_tok(tok)

    def _deps(self, ew, reads, writes):
        for b in reads:
            if b.lw is not None:
                self._w(ew, b.lw)
        for b in writes:
            if b.lw is not None:
                self._w(ew, b.lw)
            for tok in b.rd.values():
                self._w(ew, tok)

    def _w(self, ew, tok):
        if ew.is_pe and tok[0] is ew.sem:
            return
        ew.wait_tok(tok)

    def _upd(self, tok, reads, writes):
        k = id(tok[0])
        for b in reads:
            b.rd[k] = tok
        for b in writes:
            b.lw = tok; b.rd = {}

    def op(self, ew, fn, reads=(), writes=()):
        self._deps(ew, reads, writes)
        ins = fn(ew.eng)
        tok = ew.bump(ins)
        self._upd(tok, reads, writes)
        return tok

    def dma(self, ew, out, in_, reads=(), writes=()):
        pl = self.dma_pools[ew.name]
        ent = pl[self.dma_rr[ew.name]]; self.dma_rr[ew.name] = (self.dma_rr[ew.name] + 1) % len(pl)
        sem = ent[0]
        self._deps(ew, reads, writes)
        if ent[1] > 0:
            ew.wait_tok((sem, ent[1]))
        ew.eng.dma_start(out=out, in_=in_).then_inc(sem, 16)
        ent[1] += 16
        tok = (sem, ent[1])
        self._upd(tok, reads, writes)
        return tok


def APX(a, dims):
    return bass.AP(a.tensor, a.offset, [list(a.ap[0])] + [list(d) for d in dims])


def build_program(debug=False, nb=BPC):
    nc = bass.Bass("TRN2", target_bir_lowering=False)
    f = FW(nc)
    pe, act, dve, pool, sp = f.pe, f.act, f.dve, f.pool, f.sp

    def din(name, shape, dt=F32):
        return nc.dram_tensor(name, list(shape), dt, kind="ExternalInput")

    xT_d = din("xT", [BPC, 8, 128, L])
    ctxT_d = din("ctxT", [BPC, 8, 128, LC])
    cT_d = din("cT", [128, 8 * 5])
    adab_d = din("ada_b", [128, 48])
    gmix_d = din("g_mix", [128, 8]); gffn_d = din("g_ffn", [128, 8]); gfin_d = din("g_fin", [128, 8])
    pscale_d = din("pool_scale", [128, 4]); rng_d = din("ret_norm_g", [128, 8])
    decay_d = din("ret_decay", [1, 8])
    cos_d = din("ropecos", [128, L]); sin_d = din("ropesin", [128, L])
    icnt_d = din("invcnt", [4, 128, L])
    ada_d = din("ada_w_r", [48 * 128, 1024])
    win_d = din("w_in_r", [52 * 128, 1024])
    poolw_d = din("pool_w", [128, 512])
    poolout_d = din("pool_out_r", [8 * 128, 512])
    retout_d = din("ret_out_r", [8 * 128, 1024])
    wout_d = din("w_out_r", [8 * 128, 1024])
    wq_d = din("wq_r", [16 * 128, 1024])
    keys_d = din("keysT", [128, 2048])
    u_d = din("uT_r", [128 * 128, 1024])
    v_d = din("v", [128 * 128, 1024])
    out_d = nc.dram_tensor("outT", [BPC, 8, 128, L], F32, kind="ExternalOutput")

    dbg_outs = {}

    def dbg(name, buf, ap, shape):
        if not debug:
            return
        t = nc.dram_tensor("dbg_" + name, list(shape), F32, kind="ExternalOutput")
        dbg_outs[name] = f.dma(sp, t.ap(), ap, reads=[buf])

    class Scr:
        def __init__(self, name, src, rpp, order=None, defer=False):
            R, C = src.shape
            self.R = R; self.src = src
            self.t = nc.dram_tensor("scr_" + name, [R, C], BF16, kind="Internal")
            self.rpp = rpp
            n = (R + rpp - 1) // rpp
            self.pieces = [Buf(self.t, name) for _ in range(n)]
            self.order = list(order) if order is not None else list(range(n))
            if not defer:
                self.issue()

        def issue_piece(self, i):
            r0 = i * self.rpp
            r1 = min(self.R, r0 + self.rpp)
            f.dma(pool, self.t.ap()[r0:r1, :], self.src.ap()[r0:r1, :], writes=[self.pieces[i]])

        def issue(self):
            for i in self.order:
                r0 = i * self.rpp
                r1 = min(self.R, r0 + self.rpp)
                f.dma(pool, self.t.ap()[r0:r1, :], self.src.ap()[r0:r1, :], writes=[self.pieces[i]])

        def rows(self, r0, r1):
            return self.t.ap()[r0:r1, :], [self.pieces[i] for i in range(r0 // self.rpp, (r1 - 1) // self.rpp + 1)]

    s_poolw = Scr("poolw", poolw_d, 128)
    _ord = list(range(8, 20))
    for _h in range(4):
        _ord += [4 + _h, 44 + _h, 48 + _h, 20 + 2 * _h, 21 + 2 * _h]
    _ord += [0, 1, 2, 3] + list(range(28, 44))
    assert sorted(_ord) == list(range(52))
    s_win = Scr("win", win_d, 128, order=_ord)
    s_poolout = Scr("poolout", poolout_d, 1024)
    s_retout = Scr("retout", retout_d, 1024)
    s_wout = Scr("wout", wout_d, 1024)
    s_wq = Scr("wq", wq_d, 2048)
    s_keys = Scr("keys", keys_d, 128)
    s_u = Scr("u", u_d, 2048, defer=True)
    s_v = Scr("v", v_d, 2048, defer=True)
    pending_casts = []
    for _i in range(8):
        pending_casts += [(s_u, _i), (s_v, _i)]

    def cast_some(n=1):
        for _ in range(n):
            if pending_casts:
                scr_, i_ = pending_casts.pop(0)
                scr_.issue_piece(i_)

    PS = [f.psum(f"ps{i}", [128, 512], F32) for i in range(8)]

    def psbf(bank, ncols):
        return PS[bank].t[:, 0:ncols // 2].bitcast(BF16)

    iota_f = f.sbuf("iota_f", [128, 128], F32)
    f.op(pool, lambda e: e.iota(iota_f[:], pattern=[[1, 128]], base=0, channel_multiplier=0,
                                 allow_small_or_imprecise_dtypes=True), writes=[iota_f])
    pidx = f.sbuf("pidx", [128, 1], F32)
    f.op(pool, lambda e: e.iota(pidx[:], pattern=[[0, 1]], base=0, channel_multiplier=1,
                                 allow_small_or_imprecise_dtypes=True), writes=[pidx])
    rel = f.sbuf("rel", [128, 128], F32)
    f.op(pool, lambda e: e.iota(rel[:], pattern=[[1, 128]], base=0, channel_multiplier=-1,
                                 allow_small_or_imprecise_dtypes=True), writes=[rel])
    ident_bf = f.sbuf("ident_bf", [128, 128], BF16)
    ident_f = f.sbuf("ident_f", [128, 128], F32)
    ones_f = f.sbuf("ones_f", [128, 128], F32)
    f.op(dve, lambda e: e.tensor_scalar(out=ident_bf[:], in0=iota_f[:], scalar1=pidx[:, 0:1], scalar2=None,
                                        op0=ALU.is_equal), reads=[iota_f, pidx], writes=[ident_bf])
    f.op(dve, lambda e: e.tensor_scalar(out=ident_f[:], in0=iota_f[:], scalar1=pidx[:, 0:1], scalar2=None,
                                        op0=ALU.is_equal), reads=[iota_f, pidx], writes=[ident_f])
    f.op(dve, lambda e: e.memset(ones_f[:], 1.0), writes=[ones_f])
    epsb = f.sbuf("epsb", [128, 1], F32)
    f.op(dve, lambda e: e.memset(epsb[:], EPS), writes=[epsb])
    iota_bf = f.sbuf("iota_bf", [128, 128], BF16)
    f.op(dve, lambda e: e.tensor_copy(out=iota_bf[:], in_=iota_f[:]), reads=[iota_f], writes=[iota_bf])

    def load_const(name, src, shape, dt=F32):
        b = f.sbuf(name, shape, dt)
        f.dma(sp, b[:], src, writes=[b])
        return b

    adab = load_const("adab", adab_d.ap(), [128, 48])
    gmix = load_const("gmix", gmix_d.ap(), [128, 8])
    gffn = load_const("gffn", gffn_d.ap(), [128, 8])
    gfin = load_const("gfin", gfin_d.ap(), [128, 8])
    pscale = load_const("pscale", pscale_d.ap(), [128, 4])
    rng = load_const("rng", rng_d.ap(), [128, 8])
    dec_raw = load_const("dec_raw", bass.AP(decay_d, 0, [[0, 128], [1, 8]]), [128, 8])
    poolw = f.sbuf("poolw", [128, 512], BF16)
    _ap, _rd = s_poolw.rows(0, 128)
    f.dma(sp, poolw[:], _ap, reads=_rd, writes=[poolw])

    lg = f.sbuf("lg", [128, 8], F32)
    tmp8 = f.sbuf("tmp8", [128, 8], F32)
    f.op(act, lambda e: e.activation(out=tmp8[:], in_=dec_raw[:], func=AF.Exp, scale=-1.0), reads=[dec_raw], writes=[tmp8])
    f.op(act, lambda e: e.activation(out=lg[:], in_=tmp8[:], func=AF.Ln, bias=1.0), reads=[tmp8], writes=[lg])
    f.op(dve, lambda e: e.tensor_scalar(out=lg[:], in0=lg[:], scalar1=-1.0, scalar2=None, op0=ALU.mult), reads=[lg], writes=[lg])
    blk = f.sbuf("blk", [128, 8], F32)
    f.op(act, lambda e: e.activation(out=blk[:], in_=lg[:], func=AF.Exp, scale=128.0), reads=[lg], writes=[blk])
    pp = f.sbuf("pp", [128, 6], F32)
    f.op(dve, lambda e: e.tensor_scalar(out=pp[:, 0:1], in0=pidx[:], scalar1=-1.0, scalar2=127.0, op0=ALU.mult, op1=ALU.add), reads=[pidx], writes=[pp])
    f.op(dve, lambda e: e.tensor_copy(out=pp[:, 1:2], in_=pidx[:]), reads=[pidx], writes=[pp])
    f.op(dve, lambda e: e.tensor_scalar(out=pp[:, 2:3], in0=pidx[:], scalar1=-1.0, scalar2=255.0, op0=ALU.mult, op1=ALU.add), reads=[pidx], writes=[pp])
    f.op(dve, lambda e: e.tensor_scalar(out=pp[:, 3:4], in0=pidx[:], scalar1=-1.0, scalar2=127.0, op0=ALU.mult, op1=ALU.add), reads=[pidx], writes=[pp])
    f.op(dve, lambda e: e.tensor_copy(out=pp[:, 4:5], in_=pidx[:]), reads=[pidx], writes=[pp])
    f.op(dve, lambda e: e.tensor_scalar(out=pp[:, 5:6], in0=pidx[:], scalar1=1.0, scalar2=128.0, op0=ALU.mult, op1=ALU.add), reads=[pidx], writes=[pp])
    kdec = f.sbuf("kdec", [128, 8], F32)
    cdec = f.sbuf("cdec", [128, 16], F32)
    for h in range(4):
        f.op(act, lambda e, h=h: e.activation(out=kdec[:, h:h + 1], in_=pp[:, 0:1], func=AF.Exp, scale=lg[:, h:h + 1]), reads=[pp, lg], writes=[kdec])
        f.op(act, lambda e, h=h: e.activation(out=kdec[:, 4 + h:5 + h], in_=pp[:, 1:2], func=AF.Exp, scale=lg[:, 4 + h:5 + h]), reads=[pp, lg], writes=[kdec])
        for t in range(2):
            f.op(act, lambda e, h=h, t=t: e.activation(out=cdec[:, t * 4 + h:t * 4 + h + 1], in_=pp[:, 2 + t:3 + t], func=AF.Exp, scale=lg[:, h:h + 1]), reads=[pp, lg], writes=[cdec])
            f.op(act, lambda e, h=h, t=t: e.activation(out=cdec[:, 8 + t * 4 + h:8 + t * 4 + h + 1], in_=pp[:, 4 + t:5 + t], func=AF.Exp, scale=lg[:, 4 + h:5 + h]), reads=[pp, lg], writes=[cdec])
    f.op(dve, lambda e: e.tensor_scalar(out=kdec[:], in0=kdec[:], scalar1=K_SCALE, scalar2=None, op0=ALU.mult), reads=[kdec], writes=[kdec])
    f.op(dve, lambda e: e.tensor_scalar(out=cdec[:], in0=cdec[:], scalar1=K_SCALE, scalar2=None, op0=ALU.mult), reads=[cdec], writes=[cdec])
    def make_dec_tables():
        qdec = f.sbuf("qdec", [128, 8, 128], F32)
        dmask = f.sbuf("dmask", [128, 4, 128], F32)
        with f.scope():
            np1 = f.sbuf("np1", [128, 128], F32); nm = f.sbuf("nm", [128, 128], F32)
            f.op(dve, lambda e: e.tensor_scalar(out=np1[:], in0=iota_f[:], scalar1=1.0, scalar2=None, op0=ALU.add), reads=[iota_f], writes=[np1])
            f.op(dve, lambda e: e.tensor_scalar(out=nm[:], in0=iota_f[:], scalar1=-1.0, scalar2=128.0, op0=ALU.mult, op1=ALU.add), reads=[iota_f], writes=[nm])
            for h in range(4):
                f.op(act, lambda e, h=h: e.activation(out=qdec[:, h, :], in_=np1[:], func=AF.Exp, scale=lg[:, h:h + 1]), reads=[np1, lg], writes=[qdec])
                f.op(act, lambda e, h=h: e.activation(out=qdec[:, 4 + h, :], in_=nm[:], func=AF.Exp, scale=lg[:, 4 + h:5 + h]), reads=[nm, lg], writes=[qdec])
            relf = f.sbuf("relf", [128, 128], F32); relb = f.sbuf("relb", [128, 128], F32); mge = f.sbuf("mge", [128, 128], F32)
            ef = f.sbuf("ef", [128, 128], F32); eb = f.sbuf("eb", [128, 128], F32)
            f.op(dve, lambda e: e.tensor_scalar(out=relf[:], in0=rel[:], scalar1=0.0, scalar2=None, op0=ALU.max), reads=[rel], writes=[relf])
            f.op(dve, lambda e: e.tensor_scalar(out=relb[:], in0=rel[:], scalar1=-1.0, scalar2=0.0, op0=ALU.mult, op1=ALU.max), reads=[rel], writes=[relb])
            f.op(dve, lambda e: e.tensor_scalar(out=mge[:], in0=rel[:], scalar1=0.0, scalar2=K_SCALE, op0=ALU.is_ge, op1=ALU.mult), reads=[rel], writes=[mge])
            for h in range(4):
                f.op(act, lambda e, h=h: e.activation(out=ef[:], in_=relf[:], func=AF.Exp, scale=lg[:, h:h + 1]), reads=[relf, lg], writes=[ef])
                f.op(act, lambda e, h=h: e.activation(out=eb[:], in_=relb[:], func=AF.Exp, scale=lg[:, 4 + h:5 + h]), reads=[relb, lg], writes=[eb])
                f.op(dve, lambda e: e.tensor_tensor(out=ef[:], in0=ef[:], in1=eb[:], op=ALU.subtract), reads=[ef, eb], writes=[ef])
                f.op(dve, lambda e: e.tensor_tensor(out=ef[:], in0=ef[:], in1=mge[:], op=ALU.mult), reads=[ef, mge], writes=[ef])
                f.op(dve, lambda e, h=h: e.scalar_tensor_tensor(out=dmask[:, h, :], in0=eb[:], scalar=K_SCALE, in1=ef[:], op0=ALU.mult, op1=ALU.add), reads=[ef, eb], writes=[dmask])

        return qdec, dmask

    modv = f.sbuf("modv", [128, 48, 5], F32)
    with f.scope():
        ada_sb = f.sbuf("ada_sb", [128, 48, 1024], BF16)
        ada_g = [[Buf(ada_sb.t[:, g * 8:(g + 1) * 8, :], f"ada_g{g}_{k}") for k in range(3)] for g in range(6)]
        stg = [f.sbuf(f"stg{i}", [128, 8, 1024], F32) for i in range(2)]
        for g in range(6):
            st_ = stg[g % 2]
            f.dma(sp, st_[:], ada_d.ap()[g * 1024:(g + 1) * 1024, :].rearrange("(j p) c -> p j c", p=128), writes=[st_])
            for k_, (eng_, j0, j1) in enumerate(((act, 0, 3), (dve, 3, 6), (pool, 6, 8))):
                if eng_ is act:
                    f.op(eng_, lambda e, j0=j0, j1=j1, st_=st_, g=g, k_=k_: e.copy(out=ada_g[g][k_][:, j0:j1, :], in_=st_[:, j0:j1, :]), reads=[st_], writes=[ada_g[g][k_]])
                else:
                    f.op(eng_, lambda e, j0=j0, j1=j1, st_=st_, g=g, k_=k_: e.tensor_copy(out=ada_g[g][k_][:, j0:j1, :], in_=st_[:, j0:j1, :]), reads=[st_], writes=[ada_g[g][k_]])
        c_raw = f.sbuf("c_raw", [128, 40], F32)
        f.dma(sp, c_raw[:], cT_d.ap(), writes=[c_raw])
        c_bf = f.sbuf("c_bf", [128, 40], BF16)
        f.op(act, lambda e: e.activation(out=c_bf[:], in_=c_raw[:], func=AF.Silu), reads=[c_raw], writes=[c_bf])
        for oc in range(48):
            bank = PS[oc % 2]
            for kc in range(8):
                f.op(pe, lambda e, oc=oc, kc=kc, bank=bank: e.matmul(bank[:, 0:5], lhsT=ada_sb[:, oc, kc * 128:(kc + 1) * 128],
                                                                      rhs=c_bf[:, kc * 5:(kc + 1) * 5], start=(kc == 0), stop=(kc == 7)),
                     reads=ada_g[oc // 8] + [c_bf], writes=[bank])
            f.op(dve, lambda e, oc=oc, bank=bank: e.tensor_scalar(out=modv[:, oc, :], in0=bank[:, 0:5], scalar1=adab[:, oc:oc + 1], scalar2=None, op0=ALU.add),
                 reads=[adab], writes=[modv, bank])
    dbg("modv", modv, modv[:].rearrange("p a b -> p (a b)"), [128, 240])
    scl1 = f.sbuf("scl1", [128, 8, 5], F32); scl2 = f.sbuf("scl2", [128, 8, 5], F32)
    for (dst, gg, off) in ((scl1, gmix, 8), (scl2, gffn, 32)):
        f.op(dve, lambda e, dst=dst, off=off: e.tensor_scalar(out=dst[:], in0=modv[:, off:off + 8, :], scalar1=1.0, scalar2=None, op0=ALU.add), reads=[modv], writes=[dst])
        f.op(dve, lambda e, dst=dst, gg=gg: e.tensor_tensor(out=dst[:], in0=dst[:], in1=APX(gg[:], [[1, 8], [0, 5]]), op=ALU.mult), reads=[dst, gg], writes=[dst])

    NW = 4
    wbufs = [f.sbuf(f"wbuf{i}", [128, 1024], BF16) for i in range(NW)]
    wstate = {"i": 0, "n": NW}

    def load_w(scr, chunk, ncols=1024):
        b = wbufs[wstate["i"]]; wstate["i"] = (wstate["i"] + 1) % wstate["n"]
        _ap, _rd = scr.rows(chunk * 128, (chunk + 1) * 128)
        f.dma(sp, b[:, 0:ncols], _ap, reads=_rd, writes=[b])
        return b

    def rstd_from(ps_ap, out_ap, psbuf, outbuf):
        f.op(dve, lambda e: e.tensor_scalar(out=out_ap, in0=ps_ap, scalar1=1.0 / D, scalar2=EPS, op0=ALU.mult, op1=ALU.add), writes=[outbuf, psbuf])
        f.op(act, lambda e: e.activation(out=out_ap, in_=out_ap, func=AF.Sqrt), reads=[outbuf], writes=[outbuf])
        f.op(dve, lambda e: e.reciprocal(out=out_ap, in_=out_ap), reads=[outbuf], writes=[outbuf])

    for b in range(nb):
      with f.scope():
       U1 = f.sbuf("U1", [128, 16384], F32)
       U2 = f.sbuf("U2", [128, 16384], F32)
       u1b = U1.t[:, :].bitcast(BF16); u2b = U2.t[:, :].bitcast(BF16)
       hT = Buf(u1b[:, 0:16384].rearrange("p (k t) -> p k t", k=8), "hT")
       zT = Buf(u1b[:, 16384:32768].rearrange("p (k t) -> p k t", k=8), "zT")
       x1T = Buf(U1.t[:, :].rearrange("p (k t) -> p k t", k=8), "x1T")
       mergedT = Buf(u2b[:, 0:16384].rearrange("p (k t) -> p k t", k=8), "mergedT")
       mixedT = Buf(u2b[:, 16384:24576].rearrange("p (k t) -> p k t", k=4), "mixedT")
       sg = Buf(u2b[:, 24576:28672].rearrange("p (j t) -> p j t", j=2), "sg")
       r1 = [Buf(U2.t[:, 14336 + i * 512:14336 + (i + 1) * 512], f"r1_{i}") for i in range(2)]
       r2 = [Buf(U2.t[:, 15360 + i * 512:15360 + (i + 1) * 512], f"r2_{i}") for i in range(2)]
       WT = Buf(u2b.rearrange("p (t i) -> p t i", i=128), "WT")
       WTh = [Buf(u2b.rearrange("p (t i) -> p t i", i=128), "WTlo"), Buf(u2b.rearrange("p (t i) -> p t i", i=128), "WThi")]
       if True:
        _scS = f.scope(); _scS.__enter__()
        Sf0 = f.sbuf("Sf0", [128, 4, 256], F32); Sb0 = f.sbuf("Sb0", [128, 4, 256], F32)
        with f.scope():
            cx = f.sbuf("cx", [128, 8, LC], F32)
            f.dma(sp, cx[:], ctxT_d.ap()[b].rearrange("k p t -> p k t"), writes=[cx])
            csq = f.sbuf("csq", [128, 8, LC], F32)
            f.op(act, lambda e: e.activation(out=csq[:], in_=cx[:], func=AF.Square), reads=[cx], writes=[csq])
            for kc in range(8):
                f.op(pe, lambda e, kc=kc: e.matmul(PS[0][:, 0:LC], lhsT=ones_f[:], rhs=csq[:, kc, :], start=(kc == 0), stop=(kc == 7)), reads=[ones_f, csq], writes=[PS[0]])
            crstd = f.sbuf("crstd", [128, LC], F32)
            rstd_from(PS[0][:, 0:LC], crstd[:], PS[0], crstd)
            hcT = f.sbuf("hcT", [128, 8, LC], BF16)
            ctmp = f.sbuf("ctmp", [128, LC], F32)
            for kc in range(8):
                f.op(dve, lambda e, kc=kc: e.scalar_tensor_tensor(out=ctmp[:], in0=cx[:, kc, :], scalar=scl1[:, kc, 4:5], in1=crstd[:], op0=ALU.mult, op1=ALU.mult), reads=[cx, scl1, crstd], writes=[ctmp])
                f.op(dve, lambda e, kc=kc: e.tensor_scalar(out=hcT[:, kc, :], in0=ctmp[:], scalar1=modv[:, kc, 4:5], scalar2=None, op0=ALU.add), reads=[ctmp, modv], writes=[hcT])
            kcf = f.sbuf("kcf", [128, 2, 512], BF16); kcb = f.sbuf("kcb", [128, 2, 512], BF16); vc = f.sbuf("vc", [128, 2, 1024], BF16)
            for cc in range(12):
                wb = load_w(s_win, 8 + cc)
                for t in range(2):
                    if cc < 4:
                        bank = PS[1 + t]; col = cc * 128
                    else:
                        bank = PS[3 + 2 * t + (cc - 4) // 4]; col = ((cc - 4) % 4) * 128
                    for kc in range(8):
                        f.op(pe, lambda e, kc=kc, t=t, bank=bank, col=col, wb=wb: e.matmul(bank[:, col:col + 128], lhsT=hcT[:, kc, t * 128:(t + 1) * 128], rhs=wb[:, kc * 128:(kc + 1) * 128], start=(kc == 0), stop=(kc == 7)),
                             reads=[hcT, wb], writes=[bank])
            for t in range(2):
                for h in range(4):
                    f.op(act, lambda e, t=t, h=h: e.activation(out=kcf[:, t, h * 128:(h + 1) * 128], in_=PS[1 + t][:, h * 128:(h + 1) * 128], func=AF.Copy, scale=cdec[:, t * 4 + h:t * 4 + h + 1]), reads=[cdec], writes=[kcf, PS[1 + t]])
                    f.op(act, lambda e, t=t, h=h: e.activation(out=kcb[:, t, h * 128:(h + 1) * 128], in_=PS[1 + t][:, h * 128:(h + 1) * 128], func=AF.Copy, scale=cdec[:, 8 + t * 4 + h:8 + t * 4 + h + 1]), reads=[cdec], writes=[kcb, PS[1 + t]])
                for j in range(2):
                    f.op(dve, lambda e, t=t, j=j: e.tensor_copy(out=vc[:, t, j * 512:(j + 1) * 512], in_=PS[3 + 2 * t + j][:, :]), writes=[vc, PS[3 + 2 * t + j]])
            for h in range(4):
                for (kk, S0, bank) in ((kcf, Sf0, PS[0]), (kcb, Sb0, PS[7])):
                    for t in range(2):
                        f.op(pe, lambda e, kk=kk, t=t, h=h, bank=bank: e.matmul(bank[:, 0:256], lhsT=kk[:, t, h * 128:(h + 1) * 128], rhs=vc[:, t, h * 256:(h + 1) * 256], start=(t == 0), stop=(t == 1)), reads=[kk, vc], writes=[bank])
                    f.op(act, lambda e, S0=S0, h=h, bank=bank: e.copy(out=S0[:, h, :], in_=bank[:, 0:256]), writes=[S0, bank])
        if b == 0:
            dbg("Sf0", Sf0, Sf0[:].rearrange("p a b -> p (a b)"), [128, 1024])
            dbg("Sb0", Sb0, Sb0[:].rearrange("p a b -> p (a b)"), [128, 1024])

        with f.scope():
            with f.scope():
                rstd = f.sbuf("rstd", [128, L], F32)
                xc = [f.sbuf(f"xc{i}", [128, L], F32) for i in range(2)]
                xs = [f.sbuf(f"xs{i}", [128, L], F32) for i in range(2)]
                for kc in range(8):
                    xb_, sq_ = xc[kc % 2], xs[kc % 2]
                    f.dma(sp, xb_[:], xT_d.ap()[b, kc], writes=[xb_])
                    f.op(act, lambda e, xb_=xb_, sq_=sq_: e.activation(out=sq_[:], in_=xb_[:], func=AF.Square), reads=[xb_], writes=[sq_])
                    for tb in range(4):
                        f.op(pe, lambda e, tb=tb, sq_=sq_, kc=kc: e.matmul(PS[tb][:, :], lhsT=ones_f[:], rhs=sq_[:, tb * 512:(tb + 1) * 512], start=(kc == 0), stop=(kc == 7)), reads=[ones_f, sq_], writes=[PS[tb]])
                for tb in range(4):
                    rstd_from(PS[tb][:, :], rstd[:, tb * 512:(tb + 1) * 512], PS[tb], rstd)
                for kc in range(8):
                    xb_, sq_ = xc[kc % 2], xs[kc % 2]
                    f.dma(sp, xb_[:], xT_d.ap()[b, kc], writes=[xb_])
                    f.op(dve, lambda e, xb_=xb_, sq_=sq_, kc=kc: e.scalar_tensor_tensor(out=sq_[:], in0=xb_[:], scalar=scl1[:, kc, b:b + 1], in1=rstd[:], op0=ALU.mult, op1=ALU.mult), reads=[xb_, scl1, rstd], writes=[sq_])
                    f.op(act, lambda e, sq_=sq_, kc=kc: e.activation(out=hT[:, kc, :], in_=sq_[:], func=AF.Identity, bias=modv[:, kc, b:b + 1]), reads=[sq_, modv], writes=[hT])

            with f.scope():
                qdec, dmask = make_dec_tables()
                cst = [f.sbuf(f"cst{i}", [128, 512], F32) for i in range(1)]
                snt = [f.sbuf(f"snt{i}", [128, 512], F32) for i in range(1)]
                qT = f.sbuf("qT", [128, L], BF16); kT = f.sbuf("kT", [128, L], BF16)
                qfc = [f.sbuf(f"qfc{i}", [128, 128], BF16) for i in range(4)]
                qbc = [f.sbuf(f"qbc{i}", [128, 128], BF16) for i in range(4)]
                ktm = [f.sbuf(f"ktm{i}", [128, 128], BF16) for i in range(2)]
                V = f.sbuf("V", [128, 16, 256], BF16)
                Sf_bf = f.sbuf("Sf_bf", [128, 16, 256], BF16); Sb_bf = f.sbuf("Sb_bf", [128, 16, 256], BF16)
                Sf = f.sbuf("Sf", [128, 256], F32); Sb = f.sbuf("Sb", [128, 256], F32)
                sTm = [f.sbuf(f"sTm{i}", [128, 128], BF16) for i in range(4)]
                yh = [f.sbuf(f"yh{i}", [128, 256], BF16) for i in range(4)]
                st = [f.sbuf(f"st{i}", [128, 8], F32) for i in range(4)]
                junk = f.sbuf("junk", [128, 256], F32)
                for h in range(4):
                    for (which, dstT) in ((0, qT), (1, kT)):
                        w_n = load_w(s_win, 4 + which * 4 + h); w_s = load_w(s_win, 44 + which * 4 + h)
                        for tb in range(4):
                            bn, bs = PS[(tb % 2) * 2], PS[(tb % 2) * 2 + 1]
                            for (wt, bank) in ((w_n, bn), (w_s, bs)):
                                for kc in range(8):
                                    f.op(pe, lambda e, wt=wt, bank=bank, kc=kc, tb=tb: e.matmul(bank[:, :], lhsT=wt[:, kc * 128:(kc + 1) * 128], rhs=hT[:, kc, tb * 512:(tb + 1) * 512], start=(kc == 0), stop=(kc == 7)), reads=[wt, hT], writes=[bank])
                            a1, a2 = r1[tb % 2], r2[tb % 2]
                            sl = slice(tb * 512, (tb + 1) * 512)
                            cosT, sinT = cst[0], snt[0]
                            f.dma(sp, cosT[:], cos_d.ap()[:, sl], writes=[cosT]); f.dma(sp, sinT[:], sin_d.ap()[:, sl], writes=[sinT])
                            f.op(dve, lambda e, a1=a1, bn=bn, cosT=cosT: e.tensor_tensor(out=a1[:], in0=bn[:, :], in1=cosT[:], op=ALU.mult), reads=[cosT], writes=[a1, bn])
                            f.op(dve, lambda e, a2=a2, bs=bs, sinT=sinT: e.tensor_tensor(out=a2[:], in0=bs[:, :], in1=sinT[:], op=ALU.mult), reads=[sinT], writes=[a2, bs])
                            if which == 0:
                                f.op(pool, lambda e, a1=a1, a2=a2: e.tensor_tensor(out=a1[:], in0=a1[:], in1=a2[:], op=ALU.add), reads=[a1, a2], writes=[a1])
                                f.op(act, lambda e, a1=a1, sl=sl: e.copy(out=qT[:, sl], in_=a1[:]), reads=[a1], writes=[qT])
                            else:
                                f.op(pool, lambda e, a1=a1, a2=a2, sl=sl: e.tensor_tensor(out=kT[:, sl], in0=a1[:], in1=a2[:], op=ALU.add), reads=[a1, a2], writes=[kT])
                    cast_some(1)
                    wv0 = load_w(s_win, 12 + 2 * h); wv1 = load_w(s_win, 13 + 2 * h)
                    for c in range(16):
                        bank = PS[6 + (c % 2)]
                        for j, wv in enumerate((wv0, wv1)):
                            for kc in range(8):
                                f.op(pe, lambda e, c=c, bank=bank, j=j, wv=wv, kc=kc: e.matmul(bank[:, j * 128:(j + 1) * 128], lhsT=hT[:, kc, c * 128:(c + 1) * 128], rhs=wv[:, kc * 128:(kc + 1) * 128], start=(kc == 0), stop=(kc == 7)), reads=[hT, wv], writes=[bank])
                        f.op(act, lambda e, c=c, bank=bank: e.copy(out=V[:, c, :], in_=bank[:, 0:256]), writes=[V, bank])
                    cast_some(1)
                    for j in range(2):
                        wg = load_w(s_win, 20 + 2 * h + j)
                        for tb in range(4):
                            bank = PS[tb % 4]
                            for kc in range(8):
                                f.op(pe, lambda e, wg=wg, bank=bank, kc=kc, tb=tb: e.matmul(bank[:, :], lhsT=wg[:, kc * 128:(kc + 1) * 128], rhs=hT[:, kc, tb * 512:(tb + 1) * 512], start=(kc == 0), stop=(kc == 7)), reads=[wg, hT], writes=[bank])
                            f.op(act, lambda e, j=j, tb=tb, bank=bank: e.activation(out=sg[:, j, tb * 512:(tb + 1) * 512], in_=bank[:, :], func=AF.Silu), writes=[sg, bank])
                    cast_some(1)
                    for (dirn, S_, S0_, Sbf_, order, pb, kb, dcol) in (
                            (0, Sf, Sf0, Sf_bf, list(range(15)), 0, 4, h),
                            (1, Sb, Sb0, Sb_bf, list(range(15, 0, -1)), 2, 6, 4 + h)):
                        first = 0 if dirn == 0 else 15
                        f.op(dve, lambda e, S_=S_, S0_=S0_: e.tensor_copy(out=S_[:], in_=S0_[:, h, :]), reads=[S0_], writes=[S_])
                        f.op(act, lambda e, Sbf_=Sbf_, S0_=S0_, first=first: e.copy(out=Sbf_[:, first, :], in_=S0_[:, h, :]), reads=[S0_], writes=[Sbf_])

                        def tr(c, pb=pb, dcol=dcol):
                            kt_ = ktm[c % 2]
                            f.op(pe, lambda e: e.transpose(psbf(pb + c % 2, 128), kT[:, c * 128:(c + 1) * 128], ident_bf[:]), reads=[kT, ident_bf], writes=[PS[pb + c % 2]])
                            f.op(act, lambda e: e.activation(out=kt_[:], in_=psbf(pb + c % 2, 128), func=AF.Copy, scale=kdec[:, dcol:dcol + 1]), reads=[kdec], writes=[kt_, PS[pb + c % 2]])

                        tr(order[0])
                        for n_, c in enumerate(order):
                            bank = PS[kb + (c % 2)]
                            kt_ = ktm[c % 2]
                            f.op(pe, lambda e, c=c, bank=bank, kt_=kt_: e.matmul(bank[:, 0:256], lhsT=kt_[:], rhs=V[:, c, :], start=True, stop=True), reads=[kt_, V], writes=[bank])
                            if n_ + 1 < len(order):
                                tr(order[n_ + 1])
                            f.op(dve, lambda e, bank=bank, S_=S_, dcol=dcol: e.scalar_tensor_tensor(out=S_[:], in0=S_[:], scalar=blk[:, dcol:dcol + 1], in1=bank[:, 0:256], op0=ALU.mult, op1=ALU.add), reads=[S_, blk], writes=[S_, bank])
                            nxt_c = c + 1 if dirn == 0 else c - 1
                            f.op(act, lambda e, nxt_c=nxt_c, S_=S_, Sbf_=Sbf_: e.copy(out=Sbf_[:, nxt_c, :], in_=S_[:]), reads=[S_], writes=[Sbf_])
                    cast_some(1)
                    def chunk_gen(c, h=h):
                        i2 = c % 4
                        cs = slice(c * 128, (c + 1) * 128)
                        bank_ = PS[i2]
                        bs_ = bank_
                        ys_ = bank_[:, 128:384]
                        tr_ = bank_.t[:, 384:512].bitcast(BF16)
                        qf_, qb_ = qfc[i2], qbc[i2]
                        s_ = st[i2]
                        f.op(pe, lambda e: e.matmul(bs_[:, 0:128], lhsT=kT[:, cs], rhs=qT[:, cs], start=True, stop=True), reads=[kT, qT], writes=[bs_])
                        f.op(pool, lambda e: e.tensor_tensor(out=qf_[:], in0=qT[:, cs], in1=qdec[:, h, :], op=ALU.mult), reads=[qT, qdec], writes=[qf_])
                        f.op(pool, lambda e: e.tensor_tensor(out=qb_[:], in0=qT[:, cs], in1=qdec[:, 4 + h, :], op=ALU.mult), reads=[qT, qdec], writes=[qb_])
                        yield
                        f.op(dve, lambda e: e.tensor_tensor(out=sTm[i2][:], in0=bs_[:, 0:128], in1=dmask[:, h, :], op=ALU.mult), reads=[dmask], writes=[sTm[i2], bs_])
                        yield
                        f.op(pe, lambda e: e.matmul(ys_, lhsT=sTm[i2][:], rhs=V[:, c, :], start=True, stop=False), reads=[sTm[i2], V], writes=[bank_])
                        f.op(pe, lambda e: e.matmul(ys_, lhsT=qf_[:], rhs=Sf_bf[:, c, :], start=False, stop=False), reads=[qf_, Sf_bf], writes=[bank_])
                        f.op(pe, lambda e: e.matmul(ys_, lhsT=qb_[:], rhs=Sb_bf[:, c, :], start=False, stop=True), reads=[qb_, Sb_bf], writes=[bank_])
                        if debug and b == 0 and h == 0 and c == 3:
                            ydbg = f.sbuf("ydbg", [128, 256], F32)
                            f.op(act, lambda e: e.copy(out=ydbg[:], in_=ys_), writes=[ydbg, bank_])
                            dbg("y03", ydbg, ydbg[:], [128, 256])
                        yield
                        f.op(act, lambda e: e.activation(out=junk[:], in_=ys_, func=AF.Identity, accum_out=s_[:, 0:1]), writes=[junk, s_, bank_])
                        f.op(act, lambda e: e.activation(out=junk[:], in_=ys_, func=AF.Square, accum_out=s_[:, 1:2]), writes=[junk, s_, bank_])
                        yield
                        f.op(dve, lambda e: e.tensor_scalar(out=s_[:, 2:4], in0=s_[:, 0:2], scalar1=1.0 / 256, scalar2=None, op0=ALU.mult), reads=[s_], writes=[s_])
                        f.op(dve, lambda e: e.scalar_tensor_tensor(out=s_[:, 4:5], in0=s_[:, 2:3], scalar=s_[:, 2:3], in1=s_[:, 3:4], op0=ALU.mult, op1=ALU.subtract), reads=[s_], writes=[s_])
                        yield
                        f.op(act, lambda e: e.activation(out=s_[:, 7:8], in_=s_[:, 4:5], func=AF.Sqrt, scale=-1.0, bias=epsb[:, 0:1]), reads=[s_, epsb], writes=[s_])
                        yield
                        f.op(dve, lambda e: e.reciprocal(out=s_[:, 6:7], in_=s_[:, 7:8]), reads=[s_], writes=[s_])
                        f.op(dve, lambda e: e.tensor_scalar(out=yh[i2][:], in0=ys_, scalar1=s_[:, 2:3], scalar2=s_[:, 6:7], op0=ALU.subtract, op1=ALU.mult), reads=[s_], writes=[yh[i2], bank_])
                        yield
                        for j in range(2):
                            f.op(pe, lambda e, j=j: e.transpose(tr_[:, j * 128:(j + 1) * 128], yh[i2][:, j * 128:(j + 1) * 128], ident_bf[:]), reads=[yh[i2], ident_bf], writes=[bank_])
                        yield
                        for j in range(2):
                            f.op(dve, lambda e, j=j: e.scalar_tensor_tensor(out=zT[:, 2 * h + j, cs], in0=tr_[:, j * 128:(j + 1) * 128], scalar=rng[:, 2 * h + j:2 * h + j + 1], in1=sg[:, j, cs], op0=ALU.mult, op1=ALU.mult), reads=[rng, sg], writes=[zT, bank_])
                        yield

                    active = []
                    pending = list(range(16))
                    since = 99
                    while pending or active:
                        if pending and len(active) < 4 and since >= 2:
                            active.append(chunk_gen(pending.pop(0))); since = 0
                        for g_ in list(active):
                            try:
                                next(g_)
                            except StopIteration:
                                active.remove(g_)
                        since += 1
            if debug and b == 0:
                zdbg = f.sbuf("zdbg", [128, 512], F32)
                f.op(act, lambda e: e.copy(out=zdbg[:], in_=zT[:, 0, 0:512]), reads=[zT], writes=[zdbg])
                dbg("zT0", zdbg, zdbg[:], [128, 512])

            cast_some(99)
            with f.scope():
                PADW = L + 16
                pa = f.sbuf("pa", [128, PADW], F32); pb = f.sbuf("pb", [128, PADW], F32); pc = f.sbuf("pc", [128, PADW], F32)
                icnt = f.sbuf("icnt", [128, L], F32)
                dT = f.sbuf("dT", [128, L], BF16)
                for buf_ in (pa, pb, pc):
                    f.op(pool, lambda e, buf_=buf_: e.memset(buf_[:], 0.0), writes=[buf_])
                for gi in range(4):
                    wp = load_w(s_win, gi)
                    f.dma(sp, icnt[:], icnt_d.ap()[gi], writes=[icnt])
                    for tb in range(4):
                        bank = PS[tb]
                        for kc in range(8):
                            f.op(pe, lambda e, wp=wp, bank=bank, kc=kc, tb=tb: e.matmul(bank[:, :], lhsT=wp[:, kc * 128:(kc + 1) * 128], rhs=hT[:, kc, tb * 512:(tb + 1) * 512], start=(kc == 0), stop=(kc == 7)), reads=[wp, hT], writes=[bank])
                        f.op(act, lambda e, bank=bank, tb=tb: e.copy(out=pa[:, 8 + tb * 512:8 + (tb + 1) * 512], in_=bank[:, :]), writes=[pa, bank])
                    f.op(dve, lambda e: e.tensor_tensor(out=pb[:, 1:PADW], in0=pa[:, 0:PADW - 1], in1=pa[:, 1:PADW], op=ALU.add), reads=[pa], writes=[pb])
                    cur, oth = pb, pc
                    sh = 1
                    for lvl in range(gi):
                        f.op(dve, lambda e, cur=cur, oth=oth, sh=sh: e.tensor_tensor(out=oth[:, 8:8 + L + 1], in0=cur[:, 8 - sh:8 + L + 1 - sh], in1=cur[:, 8 + sh:8 + L + 1 + sh], op=ALU.add), reads=[cur], writes=[oth])
                        if lvl + 1 < gi:
                            f.op(dve, lambda e, cur=cur, oth=oth, sh=sh: e.tensor_tensor(out=oth[:, sh:8], in0=cur[:, 0:8 - sh], in1=cur[:, 2 * sh:8 + sh], op=ALU.add), reads=[cur], writes=[oth])
                            f.op(dve, lambda e, cur=cur, oth=oth, sh=sh: e.tensor_tensor(out=oth[:, 8 + L + 1:PADW - sh], in0=cur[:, 8 + L + 1 - sh:PADW - 2 * sh], in1=cur[:, 8 + L + 1 + sh:PADW], op=ALU.add), reads=[cur], writes=[oth])
                        cur, oth = oth, cur
                        sh *= 2
                    f.op(dve, lambda e, cur=cur, oth=oth: e.tensor_tensor(out=oth[:, 8:8 + L], in0=cur[:, 8:8 + L], in1=icnt[:], op=ALU.mult), reads=[cur, icnt], writes=[oth])
                    f.op(pool, lambda e, oth=oth: e.tensor_tensor(out=dT[:], in0=oth[:, 8:8 + L], in1=pa[:, 8:8 + L], op=ALU.subtract), reads=[oth, pa], writes=[dT])
                    f.op(pool, lambda e, oth=oth: e.memset(oth[:], 0.0), writes=[oth])
                    f.op(pool, lambda e, cur=cur: e.memset(cur[:], 0.0), writes=[cur])
                    for tb in range(4):
                        bank = PS[4 + (tb % 2)]
                        f.op(pe, lambda e, bank=bank, tb=tb, gi=gi: e.matmul(bank[:, :], lhsT=poolw[:, gi * 128:(gi + 1) * 128], rhs=dT[:, tb * 512:(tb + 1) * 512], start=True, stop=True), reads=[poolw, dT], writes=[bank])
                        f.op(act, lambda e, bank=bank, tb=tb, gi=gi: e.activation(out=mixedT[:, gi, tb * 512:(tb + 1) * 512], in_=bank[:, :], func=AF.Copy, scale=pscale[:, gi:gi + 1]), reads=[pscale], writes=[mixedT, bank])
            if debug and b == 0:
                mdbg = f.sbuf("mdbg", [128, 512], F32)
                f.op(act, lambda e: e.copy(out=mdbg[:], in_=mixedT[:, 2, 0:512]), reads=[mixedT], writes=[mdbg])
                dbg("mixedT2", mdbg, mdbg[:], [128, 512])

            with f.scope():
                s1 = [f.sbuf(f"s1_{i}", [128, 512], F32) for i in range(2)]
                s2 = [f.sbuf(f"s2_{i}", [128, 512], F32) for i in range(2)]
                for j in range(8):
                    wpo = load_w(s_poolout, j, 512); wro = load_w(s_retout, j); wgp = load_w(s_win, 28 + j); wgr = load_w(s_win, 36 + j)
                    for tb in range(4):
                        sl = slice(tb * 512, (tb + 1) * 512)
                        o = (tb % 2) * 4
                        bA, bB, bC, bD = PS[o], PS[o + 1], PS[o + 2], PS[o + 3]
                        for gi in range(4):
                            f.op(pe, lambda e, gi=gi, bA=bA, sl=sl, wpo=wpo: e.matmul(bA[:, :], lhsT=wpo[:, gi * 128:(gi + 1) * 128], rhs=mixedT[:, gi, sl], start=(gi == 0), stop=(gi == 3)), reads=[wpo, mixedT], writes=[bA])
                        for kc in range(8):
                            f.op(pe, lambda e, kc=kc, bB=bB, sl=sl, wro=wro: e.matmul(bB[:, :], lhsT=wro[:, kc * 128:(kc + 1) * 128], rhs=zT[:, kc, sl], start=(kc == 0), stop=(kc == 7)), reads=[wro, zT], writes=[bB])
                        for kc in range(8):
                            f.op(pe, lambda e, kc=kc, bC=bC, sl=sl, wgp=wgp: e.matmul(bC[:, :], lhsT=wgp[:, kc * 128:(kc + 1) * 128], rhs=hT[:, kc, sl], start=(kc == 0), stop=(kc == 7)), reads=[wgp, hT], writes=[bC])
                        for kc in range(8):
                            f.op(pe, lambda e, kc=kc, bD=bD, sl=sl, wgr=wgr: e.matmul(bD[:, :], lhsT=wgr[:, kc * 128:(kc + 1) * 128], rhs=hT[:, kc, sl], start=(kc == 0), stop=(kc == 7)), reads=[wgr, hT], writes=[bD])
                        a1, a2 = s1[tb % 2], s2[tb % 2]
                        f.op(act, lambda e, a1=a1, bC=bC: e.activation(out=a1[:], in_=bC[:, :], func=AF.Sigmoid), writes=[a1, bC])
                        f.op(act, lambda e, a2=a2, bD=bD: e.activation(out=a2[:], in_=bD[:, :], func=AF.Sigmoid), writes=[a2, bD])
                        f.op(dve, lambda e, a1=a1, bA=bA: e.tensor_tensor(out=a1[:], in0=bA[:, :], in1=a1[:], op=ALU.mult), reads=[a1], writes=[a1, bA])
                        f.op(dve, lambda e, a2=a2, bB=bB: e.tensor_tensor(out=a2[:], in0=bB[:, :], in1=a2[:], op=ALU.mult), reads=[a2], writes=[a2, bB])
                        f.op(pool, lambda e, a1=a1, a2=a2, j=j, sl=sl: e.tensor_tensor(out=mergedT[:, j, sl], in0=a1[:], in1=a2[:], op=ALU.add), reads=[a1, a2], writes=[mergedT])

        _scS.__exit__(None, None, None)
        with f.scope():
            with f.scope():
                xr = [f.sbuf(f"xr{i}", [128, L], F32) for i in range(2)]
                for j in range(8):
                    wo = load_w(s_wout, j)
                    xb_ = xr[j % 2]
                    f.dma(sp, xb_[:], xT_d.ap()[b, j], writes=[xb_])
                    for tb in range(4):
                        sl = slice(tb * 512, (tb + 1) * 512)
                        bank = PS[tb % 4]
                        for kc in range(8):
                            f.op(pe, lambda e, kc=kc, bank=bank, sl=sl, wo=wo: e.matmul(bank[:, :], lhsT=wo[:, kc * 128:(kc + 1) * 128], rhs=mergedT[:, kc, sl], start=(kc == 0), stop=(kc == 7)), reads=[wo, mergedT], writes=[bank])
                        f.op(dve, lambda e, bank=bank, sl=sl, j=j, xb_=xb_: e.scalar_tensor_tensor(out=x1T[:, j, sl], in0=bank[:, :], scalar=modv[:, 16 + j, b:b + 1], in1=xb_[:, sl], op0=ALU.mult, op1=ALU.add), reads=[modv, xb_], writes=[x1T, bank])
            if b == 0:
                dbg("x1T0", x1T, x1T[:, 0, :], [128, L])

            NT = TB // 128
            f.barrier()
            keysT = f.sbuf("keysT", [128, 2048], BF16)
            _ap, _rd = s_keys.rows(0, 128)
            f.dma(sp, keysT[:], _ap, reads=_rd, writes=[keysT])
            hfT = f.sbuf("hfT", [128, 8, TB], BF16)
            htmp = f.sbuf("htmp", [128, TB], F32)
            rstd2 = f.sbuf("rstd2", [128, TB], F32)
            qTc = [f.sbuf(f"qTc{i}", [128, TB], BF16) for i in range(2)]
            s_sb = [f.sbuf(f"s_sb{i}", [128, 128], F32) for i in range(2)]
            wk = [f.sbuf(f"wk{i}", [128, 256], F32) for i in range(2)]
            topvs = [f.sbuf(f"topv{i}", [128, 16, 16], F32) for i in range(NT)]
            topis = [f.sbuf(f"topi{i}", [128, 16, 16], U32) for i in range(NT)]
            topif = f.sbuf("topif", [128, 16, 16], F32)
            cand = f.sbuf("cand", [128, 8, 256], F32)
            best = f.sbuf("best", [128, 8, 16], F32); posu = f.sbuf("posu", [128, 8, 16], U32)
            bests = [f.sbuf(f"bests{i}", [128, 16], F32) for i in range(2)]
            posus = [f.sbuf(f"posus{i}", [128, 16], U32) for i in range(2)]
            xu = f.sbuf("xu", [128, 128], U32)
            xf = f.sbuf("xf", [128, 128], F32)
            IJG = [f.sbuf(f"IJG{i}", [128, 128], F32) for i in range(3)]
            IJGT = [f.sbuf(f"IJGT{i}", [128, 128], BF16) for i in range(3)]
            ebuf = xf; zs = f.sbuf("zs", [128, 16], F32)
            TG = 8
            Rg = [f.sbuf(f"Rg{i}", [128, TG, 64], BF16) for i in range(2)]
            Cg = [f.sbuf(f"Cg{i}", [128, TG, 128], BF16) for i in range(2)]
            iota_g = APX(iota_bf[:], [[0, TG], [1, 128]])
            gl = [f.sbuf(f"gl{i}", [128, TB], BF16) for i in range(3)]
            PT = [f.sbuf(f"PT{i}", [128, TB], BF16) for i in range(3)]
            GRP = 2
            ustr = [f.sbuf(f"ustr{i}", [128, GRP, 1024], BF16) for i in range(2)]
            vstr = [f.sbuf(f"vstr{i}", [128, GRP, 1024], BF16) for i in range(2)]
            iota16 = APX(iota_f[:, 0:16], [[0, 8], [0, 16], [1, 16]])
            ssv = [(s_sb[0][:], s_sb[0]), (s_sb[1][:], s_sb[1]),
                   (topif.t[:, 0:8, :].rearrange("p a b -> p (a b)"), topif), (topif.t[:, 8:16, :].rearrange("p a b -> p (a b)"), topif)]
            wkv = [Buf(wk[0].t[:, 0:128], "wkv0"), Buf(wk[0].t[:, 128:256], "wkv1"), Buf(wk[1].t[:, 0:128], "wkv2"), Buf(wk[1].t[:, 128:256], "wkv3")]
            wk4 = [wk[0], wk[1], f.sbuf("wk2", [128, 256], F32), f.sbuf("wk3", [128, 256], F32)]
            topv_g = [[Buf(topvs[tt].t[:, g, :], f"tv{tt}_{g}") for g in range(16)] for tt in range(NT)]
            topi_g = [[Buf(topis[tt].t[:, g, :], f"ti{tt}_{g}") for g in range(16)] for tt in range(NT)]
            besth = [Buf(best.t[:, h, :], f"best{h}") for h in range(8)]
            posuh = [Buf(posu.t[:, h, :], f"posu{h}") for h in range(8)]

            eq = cand

            def v4(bf_):
                return bf_[:].rearrange("p h (x y) -> p h x y", x=16)

            hfT1 = f.sbuf("hfT1", [128, 8, TB], BF16)
            wstate["n"] = 2; wstate["i"] = 0

            def hfa(blk, kc):
                k3 = blk % 3
                if k3 == 0:
                    return hfT[:, kc, :]
                if k3 == 1:
                    return hfT1[:, kc, :]
                wb_ = wbufs[2 + kc // 4]
                return wb_[:, (kc % 4) * TB:(kc % 4 + 1) * TB]

            def hfb(blk):
                k3 = blk % 3
                return [hfT] if k3 == 0 else ([hfT1] if k3 == 1 else [wbufs[2], wbufs[3]])
            IJGTs = [[IJGT, [f.sbuf(f"IJGTb{i}", [128, 128], BF16) for i in range(3)]],
                     [[f.sbuf(f"IJGTc{i}", [128, 128], BF16) for i in range(3)], [f.sbuf(f"IJGTd{i}", [128, 128], BF16) for i in range(3)]]]
            NBLK = L // TB

            def topk_gen(blk_i):
                tsl = slice(blk_i * TB, (blk_i + 1) * TB)
                for kc in range(8):
                    f.op(act, lambda e, kc=kc: e.activation(out=htmp[:], in_=x1T[:, kc, tsl], func=AF.Square), reads=[x1T], writes=[htmp])
                    f.op(pe, lambda e, kc=kc: e.matmul(PS[6][:, 0:TB], lhsT=ones_f[:], rhs=htmp[:], start=(kc == 0), stop=(kc == 7)), reads=[ones_f, htmp], writes=[PS[6]])
                rstd_from(PS[6][:, 0:TB], rstd2[:], PS[6], rstd2)
                yield
                for kc in range(8):
                    f.op(dve, lambda e, kc=kc: e.scalar_tensor_tensor(out=htmp[:], in0=x1T[:, kc, tsl], scalar=scl2[:, kc, b:b + 1], in1=rstd2[:], op0=ALU.mult, op1=ALU.mult), reads=[x1T, scl2, rstd2], writes=[htmp])
                    f.op(act, lambda e, kc=kc: e.activation(out=hfa(blk_i, kc), in_=htmp[:], func=AF.Identity, bias=modv[:, 24 + kc, b:b + 1]), reads=[htmp, modv], writes=hfb(blk_i))
                    if kc % 2 == 1:
                        yield
                if debug and b == 0 and blk_i == 0:
                    hdbg = f.sbuf("hdbg", [128, TB], F32)
                    f.op(act, lambda e: e.copy(out=hdbg[:], in_=hfa(blk_i, 0)), reads=hfb(blk_i), writes=[hdbg])
                    dbg("hfT0", hdbg, hdbg[:], [128, TB])
                def q_part(g0):
                    for g in (g0, g0 + 1):
                        wq_ = load_w(s_wq, g)
                        qb_ = qTc[g % 2]
                        for kc in range(8):
                            f.op(pe, lambda e, kc=kc, wq_=wq_: e.matmul(PS[6][:, 0:TB], lhsT=wq_[:, kc * 128:(kc + 1) * 128], rhs=hfa(blk_i, kc), start=(kc == 0), stop=(kc == 7)), reads=[wq_] + hfb(blk_i), writes=[PS[6]])
                        f.op(act, lambda e, qb_=qb_: e.copy(out=qb_[:], in_=PS[6][:, 0:TB]), writes=[qb_, PS[6]])

                q_part(0)
                yield
                for g0 in range(0, 16, 2):
                    chains = []
                    for g in (g0, g0 + 1):
                        qb_ = qTc[g % 2]
                        for tt in range(NT):
                            col = ((g % 2) * NT + tt) * 128
                            f.op(pe, lambda e, tt=tt, qb_=qb_, g=g, col=col: e.matmul(PS[7][:, col:col + 128], lhsT=qb_[:, tt * 128:(tt + 1) * 128], rhs=keysT[:, g * 128:(g + 1) * 128], start=True, stop=True), reads=[qb_, keysT], writes=[PS[7]])
                    for g in (g0, g0 + 1):
                        for tt in range(NT):
                            ci_ = (g % 2) * NT + tt
                            col = ci_ * 128
                            ss_ap, ss_b = ssv[ci_]
                            f.op(act, lambda e, ss_ap=ss_ap, col=col: e.copy(out=ss_ap, in_=PS[7][:, col:col + 128]), writes=[ss_b, PS[7]])
                            chains.append((g, tt, ss_ap, ss_b, wkv[ci_]))
                    yield
                    for step in range(5):
                        for (g, tt, ss_ap, ss_b, w_) in chains:
                            TV, TI = topv_g[tt][g], topi_g[tt][g]
                            if step == 0:
                                f.op(dve, lambda e, ss_ap=ss_ap, TV=TV: e.max(out=TV[:, 0:8], in_=ss_ap), reads=[ss_b], writes=[TV])
                            elif step == 1:
                                f.op(dve, lambda e, ss_ap=ss_ap, TV=TV, TI=TI: e.max_index(out=TI[:, 0:8], in_max=TV[:, 0:8], in_values=ss_ap), reads=[ss_b, TV], writes=[TI])
                            elif step == 2:
                                f.op(dve, lambda e, ss_ap=ss_ap, TV=TV, w_=w_: e.match_replace(out=w_[:], in_to_replace=TV[:, 0:8], in_values=ss_ap, imm_value=NEG), reads=[ss_b, TV], writes=[w_])
                            elif step == 3:
                                f.op(dve, lambda e, TV=TV, w_=w_: e.max(out=TV[:, 8:16], in_=w_[:]), reads=[w_], writes=[TV])
                            else:
                                f.op(dve, lambda e, TV=TV, TI=TI, w_=w_: e.max_index(out=TI[:, 8:16], in_max=TV[:, 8:16], in_values=w_[:]), reads=[w_, TV], writes=[TI])
                        if step in (1, 3):
                            yield
                    if g0 + 2 < 16:
                        q_part(g0 + 2)
                    yield
                for tt in range(NT):
                    TV, TI = topvs[tt], topis[tt]
                    f.op(dve, lambda e, TI=TI: e.tensor_copy(out=topif[:], in_=TI[:]), reads=topi_g[tt], writes=[topif])
                    f.op(dve, lambda e, TV=TV: e.tensor_tensor(out=cand[:].rearrange("p h (x y) -> p h x y", x=16), in0=APX(TV[:], [[32, 8], [1, 16], [0, 16]]), in1=APX(TV[:, 1, :], [[32, 8], [0, 16], [1, 16]]), op=ALU.add), reads=topv_g[tt], writes=[cand])
                    yield
                    for hq in range(2):
                        for step in range(5):
                            for h in range(4 * hq, 4 * hq + 4):
                                w_ = wk4[h % 4]
                                bh = besth[h]; ph = posuh[h]
                                if step == 0:
                                    f.op(dve, lambda e, h=h, bh=bh: e.max(out=bh[:, 0:8], in_=cand[:, h, :]), reads=[cand], writes=[bh])
                                elif step == 1:
                                    f.op(dve, lambda e, h=h, bh=bh, ph=ph: e.max_index(out=ph[:, 0:8], in_max=bh[:, 0:8], in_values=cand[:, h, :]), reads=[cand, bh], writes=[ph])
                                elif step == 2:
                                    f.op(dve, lambda e, h=h, w_=w_, bh=bh: e.match_replace(out=w_[:], in_to_replace=bh[:, 0:8], in_values=cand[:, h, :], imm_value=NEG), reads=[cand, bh], writes=[w_])
                                elif step == 3:
                                    f.op(dve, lambda e, h=h, w_=w_, bh=bh: e.max(out=bh[:, 8:16], in_=w_[:]), reads=[w_], writes=[bh])
                                else:
                                    f.op(dve, lambda e, h=h, w_=w_, bh=bh, ph=ph: e.max_index(out=ph[:, 8:16], in_max=bh[:, 8:16], in_values=w_[:]), reads=[w_, bh], writes=[ph])
                            if step in (1, 3):
                                yield
                        yield
                    pflat = posu[:].rearrange("p h k -> p (h k)")
                    for (src_, off, dst) in ((xf, 0, IJG[0]), (xf, 1, IJG[1])):
                        if off == 0:
                            f.op(dve, lambda e: e.tensor_scalar(out=xu[:], in0=pflat, scalar1=4, scalar2=None, op0=ALU.logical_shift_right), reads=posuh, writes=[xu])
                        else:
                            f.op(dve, lambda e: e.tensor_scalar(out=xu[:], in0=pflat, scalar1=15, scalar2=None, op0=ALU.bitwise_and), reads=posuh, writes=[xu])
                        f.op(dve, lambda e: e.tensor_copy(out=xf[:], in_=xu[:]), reads=[xu], writes=[xf])
                        f.op(dve, lambda e, src_=src_: e.tensor_tensor(out=v4(eq), in0=APX(src_[:], [[16, 8], [1, 16], [0, 16]]), in1=iota16, op=ALU.is_equal), reads=[src_, iota_f], writes=[eq])
                        f.op(dve, lambda e, off=off: e.tensor_tensor(out=v4(eq), in0=v4(eq), in1=APX(topif[:, off, :], [[32, 8], [0, 16], [1, 16]]), op=ALU.mult), reads=[eq, topif], writes=[eq])
                        f.op(dve, lambda e, dst=dst: e.tensor_reduce(out=dst[:].rearrange("p (h k) -> p h k", h=8), in_=v4(eq), axis=AX.X, op=ALU.add), reads=[eq], writes=[dst])
                        yield
                    e3 = ebuf[:].rearrange("p (h k) -> p h k", h=8)
                    f.op(dve, lambda e: e.tensor_tensor(out=e3, in0=best[:], in1=APX(best[:], [[16, 8], [0, 16]]), op=ALU.subtract), reads=besth, writes=[ebuf])
                    f.op(act, lambda e: e.activation(out=ebuf[:], in_=ebuf[:], func=AF.Exp), reads=[ebuf], writes=[ebuf])
                    f.op(dve, lambda e: e.tensor_reduce(out=zs[:, 0:8], in_=e3, axis=AX.X, op=ALU.add), reads=[ebuf], writes=[zs])
                    f.op(dve, lambda e: e.reciprocal(out=zs[:, 8:16], in_=zs[:, 0:8]), reads=[zs], writes=[zs])
                    f.op(dve, lambda e: e.tensor_tensor(out=IJG[2][:].rearrange("p (h k) -> p h k", h=8), in0=e3, in1=APX(zs[:, 8:16], [[1, 8], [0, 16]]), op=ALU.mult), reads=[ebuf, zs], writes=[IJG[2]])
                    yield
                    if debug and b == 0 and blk_i == 0 and tt == 0:
                        for i_, nm_ in enumerate(("I", "J", "G")):
                            dbg("peer" + nm_, IJG[i_], IJG[i_][:], [128, 128])
                    for i_ in range(3):
                        f.op(pe, lambda e, i_=i_: e.transpose(PS[6][:, 0:128], IJG[i_][:], ident_f[:]), reads=[IJG[i_], ident_f], writes=[PS[6]])
                        f.op(act, lambda e, i_=i_, tt=tt: e.copy(out=IJGTs[blk_i % 2][tt][i_][:], in_=PS[6][:, 0:128]), writes=[IJGTs[blk_i % 2][tt][i_], PS[6]])
                    yield

            def build_W(blk_i, half):
                i0 = half * 64
                wbuf_ = WTh[half]
                iota_h = APX(iota_bf[:, i0:i0 + 64], [[0, TG], [1, 64]])

                def pe_part(tt, tg, R_, C_):
                    bank = PS[6] if tg % 2 == 0 else PS[7]
                    for tl in range(TG):
                        f.op(pe, lambda e, bank=bank, tl=tl: e.matmul(bank[:, tl * 64:(tl + 1) * 64], lhsT=C_[:, tl, :], rhs=R_[:, tl, :], start=True, stop=True), reads=[R_, C_], writes=[bank])
                    tb0 = tt * 128 + tg * TG
                    f.op(act, lambda e, bank=bank, tb0=tb0: e.copy(out=WT[:, tb0:tb0 + TG, i0:i0 + 64], in_=bank[:, :].rearrange("p (t i) -> p t i", i=64)), writes=[wbuf_, bank])

                prev = None
                n_ = 0
                for tt in range(NT):
                    IJGT_ = IJGTs[blk_i % 2][tt]
                    for tg in range(128 // TG):
                        if prev is not None:
                            pe_part(*prev)
                        R_, C_ = Rg[n_ % 2], Cg[n_ % 2]
                        n_ += 1
                        ts0 = tg * TG
                        f.op(dve, lambda e, ts0=ts0, R_=R_: e.tensor_tensor(out=R_[:], in0=iota_h, in1=APX(IJGT_[0][:, ts0:ts0 + TG], [[1, TG], [0, 64]]), op=ALU.is_equal), reads=[iota_bf, IJGT_[0]], writes=[R_])
                        f.op(pool, lambda e, ts0=ts0, R_=R_: e.tensor_tensor(out=R_[:], in0=R_[:], in1=APX(IJGT_[2][:, ts0:ts0 + TG], [[1, TG], [0, 64]]), op=ALU.mult), reads=[R_, IJGT_[2]], writes=[R_])
                        f.op(dve, lambda e, ts0=ts0, C_=C_: e.tensor_tensor(out=C_[:], in0=iota_g, in1=APX(IJGT_[1][:, ts0:ts0 + TG], [[1, TG], [0, 128]]), op=ALU.is_equal), reads=[iota_bf, IJGT_[1]], writes=[C_])
                        prev = (tt, tg, R_, C_)
                        yield
                pe_part(*prev)
                yield

            bgs = {"gen": None, "blk": -1, "next": 0, "done": set()}

            def bg_step():
                if bgs["gen"] is None:
                    if bgs["next"] >= NBLK:
                        return False
                    bgs["blk"] = bgs["next"]; bgs["next"] += 1
                    bgs["gen"] = topk_gen(bgs["blk"])
                try:
                    next(bgs["gen"])
                except StopIteration:
                    bgs["gen"] = None; bgs["done"].add(bgs["blk"])
                return True

            def bg_finish(blk):
                while blk < NBLK and blk not in bgs["done"]:
                    bg_step()

            def experts(blk_i, genB, genA_fn):
                strm = {}

                def load_grp(grp):
                    ub, vb = ustr[grp % 2], vstr[grp % 2]
                    _ap, _rd = s_u.rows(grp * GRP * 128, (grp + 1) * GRP * 128)
                    f.dma(sp, ub[:], _ap.rearrange("(c p) x -> p c x", p=128), reads=_rd, writes=[ub])
                    _ap, _rd = s_v.rows(grp * GRP * 128, (grp + 1) * GRP * 128)
                    f.dma(sp, vb[:], _ap.rearrange("(c p) x -> p c x", p=128), reads=_rd, writes=[vb])
                    strm[grp] = (ub, vb)

                def emit_A(i):
                    grp, ci = divmod(i, GRP)
                    if grp not in strm:
                        load_grp(grp)
                    ub = strm[grp][0]
                    bankA = PS[4 + (i % 2)]
                    g_, p_ = gl[i % 3], PT[i % 3]
                    for kc in range(8):
                        f.op(pe, lambda e, kc=kc, ub=ub, ci=ci, bankA=bankA: e.matmul(bankA[:, 0:TB], lhsT=ub[:, ci, kc * 128:(kc + 1) * 128], rhs=hfa(blk_i, kc), start=(kc == 0), stop=(kc == 7)), reads=[ub] + hfb(blk_i), writes=[bankA])
                    f.op(act, lambda e, g_=g_, bankA=bankA: e.activation(out=g_[:], in_=bankA[:, 0:TB], func=AF.Gelu), writes=[g_, bankA])
                    f.op(pool, lambda e, g_=g_, p_=p_, i=i: e.tensor_tensor(out=p_[:], in0=g_[:], in1=WT[:, :, i], op=ALU.mult), reads=[g_, WTh[i // 64]], writes=[p_])

                def emit_out(i):
                    grp, ci = divmod(i, GRP)
                    vb = strm[grp][1]
                    p_ = PT[i % 3]
                    for dc in range(8):
                        bo = PS[dc // 2]
                        f.op(pe, lambda e, dc=dc, bo=bo, vb=vb, ci=ci, p_=p_, i=i: e.matmul(bo[:, (dc % 2) * TB:(dc % 2 + 1) * TB], lhsT=vb[:, ci, dc * 128:(dc + 1) * 128], rhs=p_[:], start=(i == 0), stop=(i == 127)), reads=[vb, p_], writes=[bo])

                emit_A(0)
                genA = None
                for i in range(128):
                    if i + 1 < 128:
                        emit_A(i + 1)
                    emit_out(i)
                    if i < 64:
                        if genB is not None:
                            if i % 2 == 0 or i == 1:
                                next(genB, None)
                            if i == 62:
                                for _ in genB:
                                    pass
                        if i % 2 == 1 and (blk_i + 1) not in bgs["done"] and blk_i + 1 < NBLK:
                            bg_step()
                        if i == 61:
                            bg_finish(blk_i + 1)
                    else:
                        if i == 64 and genA_fn is not None:
                            genA = genA_fn()
                        if genA is not None and (i % 2 == 0 or i == 65):
                            next(genA, None)
                        if i % 2 == 1:
                            cur = bgs["blk"] if bgs["gen"] is not None else bgs["next"]
                            if cur == blk_i + 2 and cur < NBLK:
                                bg_step()
                if genA is not None:
                    for _ in genA:
                        pass
                tsl = slice(blk_i * TB, (blk_i + 1) * TB)
                for dc in range(8):
                    bo = PS[dc // 2]
                    f.op(dve, lambda e, dc=dc, bo=bo: e.scalar_tensor_tensor(out=x1T[:, dc, tsl], in0=bo[:, (dc % 2) * TB:(dc % 2 + 1) * TB], scalar=modv[:, 40 + dc, b:b + 1], in1=x1T[:, dc, tsl], op0=ALU.mult, op1=ALU.add), reads=[modv, x1T], writes=[x1T, bo])

            bg_finish(0)
            for _ in build_W(0, 0):
                pass
            for blk_i in range(NBLK):
                nxt = blk_i + 1 < NBLK
                if os.environ.get("KDBG_SERIAL"):
                    if blk_i > 0:
                        bg_finish(blk_i)
                        for _ in build_W(blk_i, 0):
                            pass
                    for _ in build_W(blk_i, 1):
                        pass
                    experts(blk_i, None, None)
                    continue
                experts(blk_i, build_W(blk_i, 1), (lambda bi=blk_i + 1: build_W(bi, 0)) if nxt else None)
            wstate["n"] = NW
            if b == 0:
                dbg("x2T0", x1T, x1T[:, 0, :], [128, L])
            f.barrier()
            if True:
                xs = [Buf(U2.t[:, i * L:(i + 1) * L], f"xo{i}") for i in range(2)]
                rstd3 = Buf(U2.t[:, 2 * L:3 * L], "rstd3")
                for kc in range(8):
                    sq_ = xs[kc % 2]
                    f.op(act, lambda e, sq_=sq_, kc=kc: e.activation(out=sq_[:], in_=x1T[:, kc, :], func=AF.Square), reads=[x1T], writes=[sq_])
                    for tb in range(4):
                        f.op(pe, lambda e, tb=tb, sq_=sq_, kc=kc: e.matmul(PS[tb][:, :], lhsT=ones_f[:], rhs=sq_[:, tb * 512:(tb + 1) * 512], start=(kc == 0), stop=(kc == 7)), reads=[ones_f, sq_], writes=[PS[tb]])
                for tb in range(4):
                    rstd_from(PS[tb][:, :], rstd3[:, tb * 512:(tb + 1) * 512], PS[tb], rstd3)
                for kc in range(8):
                    o_ = xs[kc % 2]
                    f.op(dve, lambda e, o_=o_, kc=kc: e.scalar_tensor_tensor(out=o_[:], in0=x1T[:, kc, :], scalar=gfin[:, kc:kc + 1], in1=rstd3[:], op0=ALU.mult, op1=ALU.mult), reads=[x1T, gfin, rstd3], writes=[o_])
                    f.dma(sp, out_d.ap()[b, kc], o_[:], reads=[o_])
    f.barrier()
    stats = {e.name: (e.n_instr, e.n_wait) for e in f.engs}
    f.root.close()
    return nc, stats


def _chunked(W):
    K, N = W.shape
    return np.ascontiguousarray(W.reshape(K // 128, 128, N // 128, 128).transpose(2, 1, 0, 3)).reshape(N, K)


def _vec(v):
    n = v.size // 128
    return np.ascontiguousarray(v.reshape(n, 128).T)


def _consts():
    t = np.arange(L)
    row = (t // 64).astype(np.float32); col = (t % 64).astype(np.float32)
    inv = (np.float32(10000.0) ** (-np.arange(32, dtype=np.float32) / np.float32(32))).astype(np.float32)
    ang = np.concatenate([row[:, None] * inv, col[:, None] * inv], axis=-1).astype(np.float32)
    cos = np.cos(ang).astype(np.float32).T; sin = np.sin(ang).astype(np.float32).T
    ropecos = np.concatenate([cos, cos], 0); ropesin = np.concatenate([-sin, sin], 0)
    ic = np.zeros((4, 128, L), np.float32)
    for gi, w in enumerate((2, 4, 8, 16)):
        lo = np.clip(t - w // 2, 0, L); hi = np.clip(t + w - w // 2, 0, L)
        ic[gi] = (1.0 / (hi - lo).astype(np.float32))[None, :]
    return np.ascontiguousarray(ropecos), np.ascontiguousarray(ropesin), ic


def prepare_inputs(x, c, ctx, c_ctx, ada_w, ada_b, norm_mix_g, norm_ffn_g, w_in, pool_w, pool_scale,
                   pool_out, ret_decay, ret_norm_g, ret_out, w_out, peer_wq, peer_keys, peer_u, peer_v, final_g):
    f32 = lambda a: np.asarray(a, dtype=np.float32)
    x, c, ctx, c_ctx = f32(x), f32(c), f32(ctx), f32(c_ctx)
    w = f32(w_in)[0]
    qk = w[:, 512:1536].reshape(1024, 8, 2, 64)[:, :, ::-1, :].reshape(1024, 1024)
    w_all = np.concatenate([w, qk], axis=1)
    ropecos, ropesin, ic = _consts()
    u = f32(peer_u)[0]
    shared = {
        "ada_b": _vec(f32(ada_b)[0]), "g_mix": _vec(f32(norm_mix_g)[0]), "g_ffn": _vec(f32(norm_ffn_g)[0]),
        "g_fin": _vec(f32(final_g)), "pool_scale": _vec(f32(pool_scale)[0]), "ret_norm_g": _vec(f32(ret_norm_g)[0]),
        "ret_decay": np.ascontiguousarray(f32(ret_decay)[0].reshape(1, 8)),
        "ropecos": ropecos, "ropesin": ropesin, "invcnt": ic,
        "ada_w_r": _chunked(f32(ada_w)[0]), "w_in_r": _chunked(w_all),
        "pool_w": np.ascontiguousarray(f32(pool_w)[0].transpose(1, 0, 2)).reshape(128, 512),
        "pool_out_r": _chunked(f32(pool_out)[0]), "ret_out_r": _chunked(f32(ret_out)[0]),
        "w_out_r": _chunked(f32(w_out)[0]), "wq_r": _chunked(f32(peer_wq)[0]),
        "keysT": np.ascontiguousarray(f32(peer_keys)[0].reshape(16, 128, 128).transpose(2, 0, 1)).reshape(128, 2048),
        "uT_r": np.ascontiguousarray(u.reshape(128, 128, 8, 128).transpose(0, 3, 2, 1)).reshape(128 * 128, 1024),
        "v": np.ascontiguousarray(f32(peer_v)[0]),
    }
    in_maps = []
    for core in range(NCORES):
        bs = slice(core * BPC, (core + 1) * BPC)
        cc = np.concatenate([c[bs], c_ctx[None, :]], 0)
        m = dict(shared)
        m["xT"] = np.ascontiguousarray(x[bs].transpose(0, 2, 1)).reshape(BPC, 8, 128, L)
        m["ctxT"] = np.ascontiguousarray(ctx[bs].transpose(0, 2, 1)).reshape(BPC, 8, 128, LC)
        m["cT"] = np.ascontiguousarray(cc.reshape(5, 8, 128).transpose(2, 1, 0)).reshape(128, 40)
        in_maps.append(m)
    return in_maps


def kernel(**inputs):
    in_maps = prepare_inputs(**inputs)
    nc, _ = build_program()
    res = run_bass_kernel_spmd(nc, in_maps, core_ids=list(range(NCORES)))
    out = np.empty((NCORES * BPC, L, D), np.float32)
    for core in range(NCORES):
        o = np.asarray(res.results[core]["outT"]).reshape(BPC, D, L)
        out[core * BPC:(core + 1) * BPC] = o.transpose(0, 2, 1)
    return out
```
